# Optimizing a Trainium2 kernel written in Bass

```python
import math
import jax, jax.numpy as jnp
from jax import lax
import numpy as np

D_MODEL = 1024
BATCH = 16
SEQ = 2048
DEPTH = 1

HG_HEADS = 4
HG_DK = 128
HG_DV = 128
HG_KEY_WIDTH = HG_HEADS * HG_DK
HG_WIDTH = HG_HEADS * HG_DV
HG_CHUNK = 64

MB_HEADS = 8
MB_DH = 64
MB_WIDTH = MB_HEADS * MB_DH
MB_BLOCK = 256
MB_TOPK = 3
MB_QCHUNK = 16

REL_BUCKETS = 32
REL_MAX_DIST = 128

N_GROUPS = 4
EXPERTS_PER_GROUP = 8
N_EXPERTS = N_GROUPS * EXPERTS_PER_GROUP
TOP_K = 2
EXPERT_HIDDEN = 512
MOE_BLOCK = 128

DN_ALPHA = (2.0 * DEPTH) ** 0.25
DN_BETA = (8.0 * DEPTH) ** -0.25
NORM_EPS = 1e-5

IN_SIZES = (HG_KEY_WIDTH, HG_KEY_WIDTH, HG_WIDTH, HG_WIDTH, MB_WIDTH, MB_WIDTH, MB_WIDTH, D_MODEL, D_MODEL)
IN_COLS = sum(IN_SIZES)

kernel_name = 'hybrid_hgrn2_moba_hmoe_block'


def layer_norm(x, g, b):
    xf = x.astype(jnp.float32)
    mu = jnp.mean(xf, axis=-1, keepdims=True)
    xc = xf - mu
    var = jnp.mean(xc * xc, axis=-1, keepdims=True)
    return (xc * lax.rsqrt(var + NORM_EPS) * g.astype(jnp.float32) + b.astype(jnp.float32)).astype(x.dtype)


def hgrn2_mixer(q, f_logit, i, g, lb, norm_g):
    B, S, _ = q.shape
    nc = S // HG_CHUNK
    f32 = jnp.float32
    lb = lb.astype(f32)
    z = f_logit.astype(f32)
    log_f = jnp.log(lb + (1.0 - lb) * jax.nn.sigmoid(z))
    k = (1.0 - lb) * jax.nn.sigmoid(-z)
    qf = jax.nn.silu(q.astype(f32))
    vf = i.astype(f32)

    def to_chunks(t, dh):
        return t.reshape(B, nc, HG_CHUNK, HG_HEADS, dh).transpose(1, 0, 3, 2, 4)

    causal = jnp.tril(jnp.ones((HG_CHUNK, HG_CHUNK), dtype=bool))

    def chunk_step(state, inp):
        qc, kc, vc, lfc = inp
        b = jnp.cumsum(lfc, axis=2)
        o_inter = jnp.einsum('bhtk,bhkv->bhtv', qc * jnp.exp(b), state)
        rel = b[:, :, :, None, :] - b[:, :, None, :, :]
        decay = jnp.exp(jnp.where(causal[:, :, None], rel, -jnp.inf))
        scores = jnp.einsum('bhtk,bhtsk,bhsk->bhts', qc, decay, kc)
        o_intra = jnp.einsum('bhts,bhsv->bhtv', scores, vc)
        b_end = b[:, :, -1:, :]
        state = (jnp.exp(b_end[:, :, 0, :])[..., None] * state
                 + jnp.einsum('bhsk,bhsv->bhkv', kc * jnp.exp(b_end - b), vc))
        return state, o_inter + o_intra

    s0 = jnp.zeros((B, HG_HEADS, HG_DK, HG_DV), f32)
    _, o = lax.scan(chunk_step, s0, (to_chunks(qf, HG_DK), to_chunks(k, HG_DK),
                                     to_chunks(vf, HG_DV), to_chunks(log_f, HG_DK)))
    o = o.transpose(1, 0, 3, 2, 4).reshape(B, S, HG_HEADS, HG_DV)
    o = o * lax.rsqrt(jnp.mean(o * o, axis=-1, keepdims=True) + NORM_EPS)
    o = o.reshape(B, S, HG_WIDTH) * norm_g.astype(f32) * jax.nn.silu(g.astype(f32))
    return o.astype(q.dtype)


def t5_bucket(dist):
    max_exact = REL_BUCKETS // 2
    d = jnp.maximum(dist, 1).astype(jnp.float32)
    log_part = max_exact + (jnp.log(d / max_exact) / math.log(REL_MAX_DIST / max_exact)
                            * (REL_BUCKETS - max_exact)).astype(jnp.int32)
    return jnp.where(dist < max_exact, dist, jnp.minimum(log_part, REL_BUCKETS - 1))


def moba_mixer(q, k, v, rel_bias):
    B, S, _ = q.shape
    f32 = jnp.float32
    nb = -(-S // MB_BLOCK)
    s_pad = nb * MB_BLOCK
    k_sel = min(MB_TOPK, nb)
    scale = MB_DH ** -0.5

    def to_heads(t):
        t = jnp.pad(t, ((0, 0), (0, s_pad - S), (0, 0)))
        return t.reshape(B, s_pad, MB_HEADS, MB_DH).transpose(0, 2, 1, 3)

    qh, kh, vh = to_heads(q), to_heads(k), to_heads(v)
    kb = kh.reshape(B, MB_HEADS, nb, MB_BLOCK, MB_DH)
    vb = vh.reshape(B, MB_HEADS, nb, MB_BLOCK, MB_DH)
    k_mean = jnp.mean(kb, axis=3)
    gate = jnp.einsum('bhsd,bhnd->bhsn', qh, k_mean).astype(f32)
    pos = jnp.arange(s_pad)
    fully_past = jnp.arange(nb)[None, :] < (pos // MB_BLOCK)[:, None]
    gate = jnp.where(fully_past, gate, -jnp.inf)
    top_val, top_idx = lax.top_k(gate, k_sel)
    top_ok = jnp.isfinite(top_val)

    rb = rel_bias.astype(f32).T
    b_ix = jnp.arange(B)[:, None, None, None]
    h_ix = jnp.arange(MB_HEADS)[None, :, None, None]

    def attend(c):
        t0 = c * MB_QCHUNK
        tq = t0 + jnp.arange(MB_QCHUNK)
        qc = lax.dynamic_slice_in_dim(qh, t0, MB_QCHUNK, axis=2)
        idx = lax.dynamic_slice_in_dim(top_idx, t0, MB_QCHUNK, axis=2)
        ok = lax.dynamic_slice_in_dim(top_ok, t0, MB_QCHUNK, axis=2)
        kg = kb[b_ix, h_ix, idx]
        vg = vb[b_ix, h_ix, idx]
        kpos = idx[..., None] * MB_BLOCK + jnp.arange(MB_BLOCK)
        bucket = t5_bucket(tq[None, None, :, None, None] - kpos)
        s_past = (jnp.einsum('bhqd,bhqjkd->bhqjk', qc, kg).astype(f32) * scale
                  + rb[h_ix[..., None], bucket])
        s_past = jnp.where(ok[..., None], s_past, -jnp.inf).reshape(B, MB_HEADS, MB_QCHUNK, k_sel * MB_BLOCK)
        j0 = (t0 // MB_BLOCK) * MB_BLOCK
        k_own = lax.dynamic_slice_in_dim(kh, j0, MB_BLOCK, axis=2)
        v_own = lax.dynamic_slice_in_dim(vh, j0, MB_BLOCK, axis=2)
        dist_own = tq[:, None] - (j0 + jnp.arange(MB_BLOCK))[None, :]
        s_own = (jnp.einsum('bhqd,bhkd->bhqk', qc, k_own).astype(f32) * scale
                 + rb[:, t5_bucket(jnp.maximum(dist_own, 0))])
        s_own = jnp.where(dist_own >= 0, s_own, -jnp.inf)
        p = jax.nn.softmax(jnp.concatenate([s_past, s_own], axis=-1), axis=-1)
        p_past = p[..., :k_sel * MB_BLOCK].reshape(B, MB_HEADS, MB_QCHUNK, k_sel, MB_BLOCK).astype(vg.dtype)
        p_own = p[..., k_sel * MB_BLOCK:].astype(v_own.dtype)
        return (jnp.einsum('bhqjk,bhqjkd->bhqd', p_past, vg)
                + jnp.einsum('bhqk,bhkd->bhqd', p_own, v_own))

    o = lax.map(attend, jnp.arange(s_pad // MB_QCHUNK))
    o = o.transpose(1, 0, 3, 2, 4).reshape(B, s_pad, MB_WIDTH)[:, :S]
    return o


def hier_moe(x, w_group, b_group, w_expert, b_expert, w_gate_up, w_down):
    B, S, D = x.shape
    n_tok = B * S
    n_asg = n_tok * TOP_K
    f32 = jnp.float32
    xt = x.reshape(n_tok, D)
    g_logits = (xt @ w_group + b_group).astype(f32)
    grp = jnp.argmax(g_logits, axis=-1)
    p_grp = jnp.take_along_axis(jax.nn.softmax(g_logits, axis=-1), grp[:, None], axis=1)
    e_logits = (xt @ w_expert + b_expert).astype(f32).reshape(n_tok, N_GROUPS, EXPERTS_PER_GROUP)
    e_in_grp = jnp.take_along_axis(e_logits, grp[:, None, None], axis=1)[:, 0]
    top_v, top_i = lax.top_k(e_in_grp, TOP_K)
    weights = (jax.nn.softmax(top_v, axis=-1) * p_grp).reshape(n_asg)
    expert = (grp[:, None] * EXPERTS_PER_GROUP + top_i).reshape(n_asg)
    token = jnp.repeat(jnp.arange(n_tok), TOP_K)
    order = jnp.argsort(expert)
    e_s, tok_s, w_s = expert[order], token[order], weights[order]
    counts = jax.ops.segment_sum(jnp.ones((n_asg,), jnp.int32), expert, num_segments=N_EXPERTS)
    starts = jnp.cumsum(counts) - counts
    padded = (counts + MOE_BLOCK - 1) // MOE_BLOCK * MOE_BLOCK
    pad_end = jnp.cumsum(padded)
    pad_start = pad_end - padded
    dest = pad_start[e_s] + jnp.arange(n_asg) - starts[e_s]
    n_blk = -(-n_asg // MOE_BLOCK) + N_EXPERTS
    n_rows = n_blk * MOE_BLOCK
    x_buf = jnp.zeros((n_rows, D), x.dtype).at[dest].set(xt[tok_s])
    blk_expert = jnp.minimum(jnp.searchsorted(pad_end, jnp.arange(n_blk) * MOE_BLOCK, side='right'),
                             N_EXPERTS - 1)

    def expert_block(args):
        xb, e = args
        gate, up = jnp.split(xb @ w_gate_up[e], 2, axis=-1)
        return (jax.nn.silu(gate) * up) @ w_down[e]

    y_buf = lax.map(expert_block, (x_buf.reshape(n_blk, MOE_BLOCK, D), blk_expert)).reshape(n_rows, D)
    y = jnp.zeros((n_tok, D), x.dtype).at[tok_s].add(y_buf[dest] * w_s[:, None].astype(x.dtype))
    return y.reshape(B, S, D)


def setup_inputs(seed: int = 0) -> dict:
    key = jax.random.key(seed)
    ks = jax.random.split(key, 20)
    f32 = jnp.float32
    L = DEPTH

    def normal(k, shape, std):
        return jax.random.normal(k, shape, f32) * std

    col_scale = jnp.concatenate([jnp.full((n,), s, f32) for n, s in
                                 zip(IN_SIZES, (1.0, 1.0, DN_BETA, 1.0, 1.0, 1.0, DN_BETA, 1.0, 1.0))])
    return {
        'x': normal(ks[0], (BATCH, SEQ, D_MODEL), 1.0),
        'w_in': normal(ks[1], (L, D_MODEL, IN_COLS), D_MODEL ** -0.5) * col_scale,
        'b_in': normal(ks[2], (L, IN_COLS), 0.02),
        'lb_logits': normal(ks[3], (L + 1, HG_KEY_WIDTH), 0.5),
        'hg_norm_g': 1.0 + normal(ks[4], (L, HG_WIDTH), 0.1),
        'rel_bias': normal(ks[5], (REL_BUCKETS, MB_HEADS), 0.5),
        'w_proj_a': normal(ks[6], (L, HG_WIDTH, D_MODEL), DN_BETA * HG_WIDTH ** -0.5),
        'w_proj_b': normal(ks[7], (L, MB_WIDTH, D_MODEL), DN_BETA * MB_WIDTH ** -0.5),
        'w_out': normal(ks[8], (L, D_MODEL, D_MODEL), DN_BETA * D_MODEL ** -0.5),
        'ln1_g': 1.0 + normal(ks[9], (L, D_MODEL), 0.1),
        'ln1_b': normal(ks[10], (L, D_MODEL), 0.02),
        'w_group': normal(ks[11], (L, D_MODEL, N_GROUPS), D_MODEL ** -0.5),
        'b_group': normal(ks[12], (L, N_GROUPS), 0.01),
        'w_expert': normal(ks[13], (L, D_MODEL, N_EXPERTS), D_MODEL ** -0.5),
        'b_expert': normal(ks[14], (L, N_EXPERTS), 0.01),
        'w_gate_up': normal(ks[15], (L, N_EXPERTS, D_MODEL, 2 * EXPERT_HIDDEN), D_MODEL ** -0.5),
        'w_down': normal(ks[16], (L, N_EXPERTS, EXPERT_HIDDEN, D_MODEL), DN_BETA * EXPERT_HIDDEN ** -0.5),
        'ln2_g': 1.0 + normal(ks[17], (L, D_MODEL), 0.1),
        'ln2_b': normal(ks[18], (L, D_MODEL), 0.02),
    }


def reference(x, w_in, b_in, lb_logits, hg_norm_g, rel_bias, w_proj_a, w_proj_b, w_out, ln1_g, ln1_b,
              w_group, b_group, w_expert, b_expert, w_gate_up, w_down, ln2_g, ln2_b):
    split_at = [int(s) for s in np.cumsum(IN_SIZES)[:-1]]
    lower_bounds = jnp.cumsum(jax.nn.softmax(lb_logits.astype(jnp.float32), axis=0), axis=0)
    for l in range(DEPTH):
        h = x
        proj = h @ w_in[l] + b_in[l]
        hg_q, hg_f, hg_i, hg_g, mb_q, mb_k, mb_v, gate_a, gate_b = jnp.split(proj, split_at, axis=-1)
        y_a = hgrn2_mixer(hg_q, hg_f, hg_i, hg_g, lower_bounds[l], hg_norm_g[l]) @ w_proj_a[l]
        y_b = moba_mixer(mb_q, mb_k, mb_v, rel_bias) @ w_proj_b[l]
        mixed = (jax.nn.sigmoid(gate_a) * y_a + jax.nn.sigmoid(gate_b) * y_b) @ w_out[l]
        x = layer_norm(DN_ALPHA * h + mixed, ln1_g[l], ln1_b[l])
        ffn = hier_moe(x, w_group[l], b_group[l], w_expert[l], b_expert[l], w_gate_up[l], w_down[l])
        x = layer_norm(DN_ALPHA * x + ffn, ln2_g[l], ln2_b[l])
    return x
```

```python
import os
import numpy as np
import concourse.bass as bass
import concourse.mybir as mybir
from concourse.bass_utils import run_bass_kernel_spmd

F32 = mybir.dt.float32
BF16 = mybir.dt.bfloat16
I32 = mybir.dt.int32
AF = mybir.ActivationFunctionType
ALU = mybir.AluOpType
AX = mybir.AxisListType

NCORES = 8
D = 1024
S = 2048
NTOK = 4096
INC = 5632
ALPHA = 2.0 ** 0.25
EPS = 1e-5
NEG = -30000.0
CAP = 384
NE = 32
ENGS = ("pe", "act", "dve", "pool", "sp")

C_ID, C_M2, C_ANTI, C_LTRI, C_PAST, C_OWN, C_EOFF, C_E, C_OHG, NCST = 0, 128, 256, 384, 512, 1024, 1536, 1568, 2592, 3104


class Op:
    __slots__ = ("eng", "fn", "deps", "dma_key", "dma_val", "sig", "idx", "waits")

    def __init__(self, eng, fn):
        self.eng = eng
        self.fn = fn
        self.deps = set()
        self.dma_key = None
        self.dma_val = 0
        self.sig = 0
        self.waits = []


class Prog:
    def __init__(self, nc):
        self.nc = nc
        self.ops = []
        self.last_w = {}
        self.readers = {}
        self.dma_cnt = {}
        self.last_eng = {}
        self.last_dma = {}
        self.bg_keys = set()

    def op(self, eng, fn, r=(), w=(), dma=None, extra=()):
        o = Op(eng, fn)
        o.idx = len(self.ops)
        deps = set(extra)
        for x in r:
            lw = self.last_w.get(x)
            if lw is not None:
                deps.add(lw)
        for x in w:
            lw = self.last_w.get(x)
            if lw is not None:
                deps.add(lw)
            for rd in self.readers.get(x, ()):
                deps.add(rd)
        deps.discard(o.idx)
        o.deps = deps
        for x in r:
            self.readers.setdefault(x, []).append(o.idx)
        for x in w:
            self.last_w[x] = o.idx
            self.readers[x] = []
        if dma is not None:
            o.dma_key = dma
            self.dma_cnt[dma] = self.dma_cnt.get(dma, 0) + 1
            o.dma_val = 16 * self.dma_cnt[dma]
            self.last_dma[dma] = o.idx
        else:
            self.last_eng[eng] = o.idx
        self.ops.append(o)
        return o

    def barrier(self):
        deps = set(self.last_eng.values()) | set(v for k, v in self.last_dma.items() if k not in self.bg_keys)
        for e in ENGS:
            self.op(e, lambda eng: None, extra=deps)
        self.last_w = {}
        self.readers = {}

    def finalize(self):
        ops = self.ops

        def skip(D, o):
            return D.eng == "pe" and o.eng == "pe" and o.dma_key is None and D.dma_key is None

        needs_sig = set()
        for o in ops:
            for d in o.deps:
                Dd = ops[d]
                if Dd.dma_key is not None or skip(Dd, o):
                    continue
                needs_sig.add(d)
        cnt = {e: 0 for e in ENGS}
        for o in ops:
            if o.idx in needs_sig:
                cnt[o.eng] += 1
                o.sig = cnt[o.eng]
        waited = {e: {} for e in ENGS}
        for o in ops:
            wl = {}
            for d in o.deps:
                Dd = ops[d]
                if Dd.dma_key is not None:
                    k, v = ("dma", Dd.dma_key), Dd.dma_val
                else:
                    if skip(Dd, o):
                        continue
                    k, v = ("eng", Dd.eng), Dd.sig
                if v > wl.get(k, 0):
                    wl[k] = v
            for k, v in wl.items():
                if waited[o.eng].get(k, 0) >= v:
                    continue
                waited[o.eng][k] = v
                o.waits.append((k, v))

    def emit(self):
        nc = self.nc
        self.finalize()
        sems = {}
        for e in ENGS:
            sems[("eng", e)] = nc.alloc_semaphore(name=f"s_{e}")
        for k in self.dma_cnt:
            sems[("dma", k)] = nc.alloc_semaphore(name=f"d_{k}")
        by_eng = {e: [o for o in self.ops if o.eng == e] for e in ENGS}
        prog = self

        def run(engname, eng):
            for o in by_eng[engname]:
                for k, v in o.waits:
                    eng.wait_ge(sems[k], v)
                ins = o.fn(eng)
                if ins is None:
                    if o.sig:
                        eng.nop().then_inc(sems[("eng", engname)], 1)
                    continue
                if o.dma_key is not None:
                    ins.then_inc(sems[("dma", o.dma_key)], 16)
                elif o.sig:
                    ins.then_inc(sems[("eng", engname)], 1)
            if engname == "sp":
                for k, n in prog.dma_cnt.items():
                    eng.wait_ge(sems[("dma", k)], 16 * n)

        with nc.Block() as block:
            @block.tensor
            def _(e):
                run("pe", e)

            @block.scalar
            def _(e):
                run("act", e)

            @block.vector
            def _(e):
                run("dve", e)

            @block.gpsimd
            def _(e):
                run("pool", e)

            @block.sync
            def _(e):
                run("sp", e)


class Arena:
    def __init__(self, nc, words):
        self.t = nc.alloc_sbuf_tensor("arena", [128, words], F32)
        self.top = 0
        self.W = words
        self.peak = 0

    def alloc_at(self, off, shape, dt=F32):
        assert off >= self.top, (off, self.top)
        save = self.top
        self.top = off
        v = self.alloc(shape, dt)
        self.top = save
        return v

    def alloc(self, shape, dt=F32):
        shape = list(shape)
        n = int(np.prod(shape))
        words = n if dt in (F32, I32) else (n + 1) // 2
        words = (words + 7) // 8 * 8
        off = self.top
        self.top += words
        self.peak = max(self.peak, self.top)
        assert self.top <= self.W, f"arena overflow {self.top} > {self.W}"
        v = self.t[:, off:off + words]
        self.last_raw = v
        if dt != F32:
            v = v.bitcast(dt)
        v = v[:, 0:n]
        if len(shape) == 2:
            v = v.rearrange("p (a b) -> p a b", a=shape[0], b=shape[1])
        elif len(shape) == 3:
            v = v.rearrange("p (a b c) -> p a b c", a=shape[0], b=shape[1], c=shape[2])
        return v


def dap(t, off, pat):
    return bass.AP(tensor=t, offset=off, ap=[list(x) for x in pat])


def _t5_bucket(dist):
    dist = np.asarray(dist, dtype=np.int64)
    d = np.maximum(dist, 1).astype(np.float32)
    lp = 16 + (np.log(d / np.float32(16.0)) / np.float32(np.log(128.0 / 16.0)) * np.float32(16.0)).astype(np.int32)
    return np.where(dist < 16, dist, np.minimum(lp, 31))


def make_consts():
    c = np.zeros((128, NCST), np.float32)
    i = np.arange(128)
    c[:, C_ID:C_ID + 128] = np.eye(128)
    s_, t_ = np.meshgrid(i, i, indexing="ij")
    c[:, C_M2:C_M2 + 128] = ((s_ // 64 == t_ // 64) & (s_ <= t_)).astype(np.float32)
    c[:, C_ANTI:C_ANTI + 128] = np.eye(128)[::-1]
    c[:, C_LTRI:C_LTRI + 128] = (s_ < t_).astype(np.float32)
    past = np.zeros((8, 8, 8), np.float32)
    own = np.zeros((8, 8, 8), np.float32)
    for qb in range(8):
        for n in range(8):
            past[qb, :, n] = 0.0 if n < qb else -1e30
            own[qb, :, n] = 1.0 if n == qb else 0.0
    c[:, C_PAST:C_PAST + 512] = past.reshape(1, 512)
    c[:, C_OWN:C_OWN + 512] = own.reshape(1, 512)
    c[:, C_EOFF:C_EOFF + 32] = (np.arange(32) * CAP).astype(np.float32)[None, :]
    E = np.zeros((8, 8, 128), np.float32)
    for n in range(8):
        E[n, n, :] = 1.0
    c[0:8, C_E:C_E + 1024] = E.reshape(8, 1024)
    ohg = np.zeros((33, 2, 256), np.float32)
    for u in range(255):
        d = u - 127
        if d >= 0:
            ohg[_t5_bucket(d), 0, u] += 1.0
            ohg[31, 0, u] -= 1.0
        else:
            ohg[32, 0, u] = NEG
        dist = d + 128
        ohg[_t5_bucket(dist), 1, u] += 1.0
        ohg[31, 1, u] -= 1.0
    c[0:33, C_OHG:C_OHG + 512] = ohg.reshape(33, 512)
    return c


def build(debug=0, stage=99, maxphase=99):
    nc = bass.Bass("TRN2", target_bir_lowering=False)

    def din(name, shape, dt=F32):
        return nc.dram_tensor(name, list(shape), dt, kind="ExternalInput")

    x_d = din("x", [NTOK, D])
    win_d = din("w_in", [D, INC])
    bin_d = din("b_in", [INC])
    lbl_d = din("lb_logits", [2, 512])
    ng_d = din("hg_norm_g", [512])
    rb_d = din("rel_bias", [32, 8])
    wpa_d = din("w_proj_a", [512, D])
    wpb_d = din("w_proj_b", [512, D])
    wout_d = din("w_out", [D, D])
    ln1g_d = din("ln1_g", [D])
    ln1b_d = din("ln1_b", [D])
    wr_d = din("w_router", [D, 36])
    br_d = din("b_router", [36])
    wgu_d = din("w_gate_up", [NE, D, 1024])
    wdn_d = din("w_down", [NE, 512, D])
    ln2g_d = din("ln2_g", [D])
    ln2b_d = din("ln2_b", [D])
    cst_d = din("consts", [128, NCST])
    eind_d = din("eind", [8, S])
    y_d = nc.dram_tensor("y", [NTOK, D], F32, kind="ExternalOutput")

    def dscr(name, shape, dt):
        return nc.dram_tensor(name, list(shape), dt, kind="Internal")

    gd_d = dscr("gd", [8, 512], BF16)
    ohg_d = dscr("ohgT", [4, 128, S], BF16)
    omb_d = dscr("ombT", [8, 64, S], BF16)
    x1_d = dscr("x1s", [NTOK, D], F32)
    xbuf_d = dscr("xbuf", [NE * CAP + 128, D], BF16)
    ybuf_d = dscr("ybuf", [NE * CAP, D], F32)
    dbg = {}
    if debug:
        dbg["ohg"] = nc.dram_tensor("dbg_ohg", [4, 128, S], BF16, kind="ExternalOutput")
        dbg["omb"] = nc.dram_tensor("dbg_omb", [8, 64, S], BF16, kind="ExternalOutput")
        dbg["x1"] = nc.dram_tensor("dbg_x1", [NTOK, D], F32, kind="ExternalOutput")

    P = Prog(nc)
    A = Arena(nc, 47 * 1024)
    ps = [nc.alloc_psum_tensor(f"ps{i}", [128, 512], F32)[:, :] for i in range(8)]
    psb = [p.bitcast(BF16) for p in ps]
    PSN = [f"ps{i}" for i in range(8)]

    def dma(eng, out, in_, r=(), w=(), key=None, nonc=False):
        if nonc:
            return P.op(eng, lambda e: e.dma_start(out=out, in_=in_, allow_slow_non_contiguous=True), r=r, w=w, dma=key)
        return P.op(eng, lambda e: e.dma_start(out=out, in_=in_), r=r, w=w, dma=key)

    def mm(out, lhsT, rhs, start, stop, r, w):
        return P.op("pe", lambda e: e.matmul(out, lhsT=lhsT, rhs=rhs, start=start, stop=stop), r=r, w=w)

    def tr(out, in_, ident, r, w):
        return P.op("pe", lambda e: e.transpose(out=out, in_=in_, identity=ident), r=r, w=w)

    def act(out, in_, func, r, w, bias=None, scale=None, accum=None):
        kw = {}
        if bias is not None:
            kw["bias"] = bias
        if scale is not None:
            kw["scale"] = scale
        if accum is not None:
            kw["accum_out"] = accum
        return P.op("act", lambda e: e.activation(out=out, in_=in_, func=func, **kw), r=r, w=w)

    def tcopy(eng, out, in_, r, w):
        if eng == "act":
            return P.op(eng, lambda e: e.activation(out=out, in_=in_, func=AF.Copy), r=r, w=w)
        return P.op(eng, lambda e: e.tensor_copy(out=out, in_=in_), r=r, w=w)

    def tt(eng, out, in0, in1, op, r, w):
        return P.op(eng, lambda e: e.tensor_tensor(out=out, in0=in0, in1=in1, op=op), r=r, w=w)

    def ts(eng, out, in0, s1, s2, op0, op1, r, w):
        if op1 is None:
            return P.op(eng, lambda e: e.tensor_scalar(out=out, in0=in0, scalar1=s1, scalar2=None, op0=op0), r=r, w=w)
        return P.op(eng, lambda e: e.tensor_scalar(out=out, in0=in0, scalar1=s1, scalar2=s2, op0=op0, op1=op1), r=r, w=w)

    def stt(out, in0, scalar, in1, op0, op1, r, w):
        return P.op("dve", lambda e: e.scalar_tensor_tensor(out=out, in0=in0, scalar=scalar, in1=in1, op0=op0, op1=op1), r=r, w=w)

    def memset(eng, ap, val, w):
        return P.op(eng, lambda e: e.memset(ap, val), w=w)

    cst = A.alloc([NCST])
    dma("sp", cst, cst_d.ap(), w=["cst"], key="cst")
    identf = cst[:, C_ID:C_ID + 128]
    cb = A.alloc([4, 128], BF16)
    identb, antib, ltrib, onesb = cb[:, 0, :], cb[:, 1, :], cb[:, 2, :], cb[:, 3, :]
    tcopy("dve", identb, cst[:, C_ID:C_ID + 128], ["cst"], ["identb"])
    tcopy("dve", antib, cst[:, C_ANTI:C_ANTI + 128], ["cst"], ["antib"])
    tcopy("dve", ltrib, cst[:, C_LTRI:C_LTRI + 128], ["cst"], ["ltrib"])
    memset("dve", onesb, 1.0, ["onesb"])
    Eb = A.alloc([8, 128], BF16)
    memset("dve", Eb.rearrange("p a b -> p (a b)"), 0.0, ["Eb"])
    tcopy("dve", Eb[0:8], cst[0:8, C_E:C_E + 1024].rearrange("p (a b) -> p a b", a=8, b=128), ["cst"], ["Eb"])
    mask2x4 = A.alloc([4, 128])
    for h in range(4):
        tcopy("dve", mask2x4[:, h, :], cst[:, C_M2:C_M2 + 128], ["cst"], ["mask2x4"])
    resetm = A.alloc([512], BF16)
    memset("dve", resetm, 1.0, ["resetm"])
    memset("dve", resetm[:, 0:512:64], 0.0, ["resetm"])
    pastm = cst[:, C_PAST:C_PAST + 512].rearrange("p (q h n) -> p q h n", q=8, h=8, n=8)
    ownm = cst[:, C_OWN:C_OWN + 512].rearrange("p (q h n) -> p q h n", q=8, h=8, n=8)

    bcol = A.alloc([44])
    dma("sp", bcol, dap(bin_d, 0, [[1, 128], [128, 44]]), w=["bcol"], key="bcol", nonc=True)
    bcolq = A.alloc([4])
    ts("dve", bcolq, bcol[:, 16:20], 0.125, None, ALU.mult, None, ["bcol"], ["bcolq"])
    brow = A.alloc([1536], BF16)
    dma("pool", brow[0:1, 0:1024], dap(bin_d, 1024, [[0, 1], [1, 1024]]), w=["brow"], key="brow")
    dma("pool", brow[0:1, 1024:1536], dap(bin_d, 3072, [[0, 1], [1, 512]]), w=["brow"], key="brow")
    onesr = A.alloc([128], BF16)
    memset("dve", onesr[0:1, :], 1.0, ["onesr"])
    lbl = A.alloc([2, 4])
    dma("sp", lbl, dap(lbl_d, 0, [[1, 128], [512, 2], [128, 4]]), w=["lbl"], key="lbl", nonc=True)
    lb = A.alloc([4])
    oml = A.alloc([4])
    tt("dve", lb, lbl[:, 0, :], lbl[:, 1, :], ALU.subtract, ["lbl"], ["lb"])
    act(lb, lb, AF.Sigmoid, ["lb"], ["lb"])
    ts("dve", oml, lb, -1.0, 1.0, ALU.mult, ALU.add, ["lb"], ["oml"])
    ng_bc = A.alloc([512])
    dma("sp", ng_bc, dap(ng_d, 0, [[0, 128], [1, 512]]), w=["ng_bc"], key="ng_bc")
    rb31 = A.alloc([8])
    dma("sp", rb31, dap(rb_d, 31 * 8, [[0, 128], [1, 8]]), w=["rb31"], key="rb31")
    rbm = A.alloc([8, 8])
    tcopy("dve", rbm, rb31.unsqueeze(2).to_broadcast([128, 8, 8]), ["rb31"], ["rbm"])
    rbl = A.alloc([8])
    dma("sp", rbl[0:32, :], rb_d.ap(), w=["rbl"], key="rbl")
    memset("dve", rbl[32:33, :], 1.0, ["rbl1"])
    mm(ps[0][0:8, 0:512], rbl[0:33, 0:8], cst[0:33, C_OHG:C_OHG + 512], True, True, ["rbl", "rbl1", "cst"], ["ps0"])
    Gs = A.alloc([512], BF16)
    tcopy("dve", Gs[0:8, :], ps[0][0:8, 0:512], ["ps0"], ["Gs"])
    dma("sp", gd_d.ap(), Gs[0:8, :], r=["Gs"], w=["gd_d"], key="gd_w")
    Yc = A.alloc([16, 128], BF16)
    dma("sp", Yc, dap(gd_d, 0, [[1, 128], [256, 16], [1, 128]]), r=["gd_d"], w=["Yc"], key="Yc")
    wr = A.alloc([8, 36])
    dma("sp", wr, dap(wr_d, 0, [[36, 128], [128 * 36, 8], [1, 36]]), w=["wr"], key="wr")
    br_bc = A.alloc([36])
    dma("sp", br_bc, dap(br_d, 0, [[0, 128], [1, 36]]), w=["br_bc"], key="br_bc")
    carry = A.alloc([32])
    tcopy("dve", carry, cst[:, C_EOFF:C_EOFF + 32], ["cst"], ["carry"])
    dest_i = A.alloc([32, 2], I32)
    wts = A.alloc([32, 2])
    zrow = A.alloc([1024], BF16)
    memset("dve", A.last_raw, 0.0, ["zrow"])
    P.barrier()
    persist_top = A.top
    def start_zero_fill():
        P.bg_keys.add("zfill")
        zfill_ops = []
        nrow = NE * CAP + 128
        for r0 in range(0, nrow, 1024):
            nr = min(1024, nrow - r0)
            zfill_ops.append(dma("sp", dap(xbuf_d, r0 * D, [[D, 128], [128 * D, nr // 128], [1, D]]),
                                 zrow.unsqueeze(1).to_broadcast([128, nr // 128, 1024]), key="zfill").idx)
        return zfill_ops

    def load_w(dst, src_t, row0, col0, ncols, nk, rowstride, key, rname, kp=128):
        dma("pool", dst, dap(src_t, row0 * rowstride + col0, [[rowstride, kp], [kp * rowstride, nk], [1, ncols]]),
            w=[rname], key=key)

    for s in range(2 if stage >= 1 else 0):
        A.top = persist_top
        xT = A.alloc([8, S], BF16)
        seq_top = A.top
        xb = [A.alloc([1024], BF16) for _ in range(2)]
        wq = A.alloc_at(seq_top + 8320, [8, 512], BF16)
        wf = A.alloc_at(seq_top + 8320 + 2048, [8, 512], BF16)
        for j in range(16):
            b = j % 2
            if j == 4:
                load_w(wq, win_d, 0, 0, 512, 8, INC, "wq", "wq")
                load_w(wf, win_d, 0, 512, 512, 8, INC, "wf", "wf")
            dma("pool", xb[b], dap(x_d, (s * 16 + j) * 128 * D, [[D, 128], [1, D]]), w=[f"xb{b}"], key=f"xb{b}")
            for dk in range(8):
                tr(psb[b][:, dk * 128:(dk + 1) * 128], xb[b][:, dk * 128:(dk + 1) * 128], identb,
                   [f"xb{b}", "identb"], [PSN[b]])
            tcopy("act" if b else "dve", xT[:, :, j * 128:(j + 1) * 128],
                  psb[b].rearrange("p (a b) -> p a b", a=8, b=128), [PSN[b]], [f"xT{j}"])
        XT = lambda j0, n: [f"xT{j}" for j in range(j0, j0 + n)]
        P.barrier()
        if s == 0:
            zfill_ops = start_zero_fill()
        if maxphase <= 1:
            break
        A.top = seq_top

        qtT = A.alloc([4, S], BF16)
        ktT = A.alloc([4, S], BF16)
        dec = A.alloc([4, 32])
        h_top = A.top
        assert A.top == seq_top + 8320, A.top - seq_top
        A.top += 4096
        wi = A.alloc_at(seq_top + 19712, [8, 512], BF16)
        wg = A.alloc_at(seq_top + 19712 + 2048, [8, 512], BF16)
        load_w(wi, win_d, 0, 1024, 512, 8, INC, "wi", "wi")
        load_w(wg, win_d, 0, 1536, 512, 8, INC, "wg", "wg")
        sq2 = [A.alloc([512], BF16) for _ in range(2)]
        fb2 = [A.alloc([512]) for _ in range(2)]
        lfb2 = [A.alloc([512]) for _ in range(2)]
        bb2 = [A.alloc([512]) for _ in range(2)]
        kk2 = [A.alloc([512]) for _ in range(2)]
        eb2 = [A.alloc([512]) for _ in range(2)]
        enb2 = [A.alloc([512]) for _ in range(2)]
        its = [(h, tg) for h in range(4) for tg in range(4)]
        for r0 in range(0, 16, 2):
            rnd = []
            for u in range(2):
                h, tg = its[r0 + u]
                rnd.append((u, h, tg, slice(tg * 512, (tg + 1) * 512), 2 + u * 2, 3 + u * 2,
                            sq2[u], fb2[u], lfb2[u], bb2[u], kk2[u], eb2[u], enb2[u]))
            for (u, h, tg, tsl, bq, bf_, sq, fb, lfb, bb, kk, eb, enb) in rnd:
                for dk in range(8):
                    mm(ps[bq], wq[:, dk, h * 128:(h + 1) * 128], xT[:, dk, tsl], dk == 0, dk == 7,
                       ["wq"] + XT(tg * 4, 4), [PSN[bq]])
                for dk in range(8):
                    mm(ps[bf_], wf[:, dk, h * 128:(h + 1) * 128], xT[:, dk, tsl], dk == 0, dk == 7,
                       ["wf"] + XT(tg * 4, 4), [PSN[bf_]])
            for (u, h, tg, tsl, bq, bf_, sq, fb, lfb, bb, kk, eb, enb) in rnd:
                act(sq, ps[bq], AF.Silu, [PSN[bq], "bcol"], [f"sq{u}"], bias=bcol[:, h:h + 1])
                act(fb, ps[bf_], AF.Sigmoid, [PSN[bf_], "bcol"], [f"fb{u}"], bias=bcol[:, 4 + h:5 + h])
                ts("dve", fb, fb, oml[:, h:h + 1], lb[:, h:h + 1], ALU.mult, ALU.add, [f"fb{u}", "oml", "lb"], [f"fb{u}"])
            for (u, h, tg, tsl, bq, bf_, sq, fb, lfb, bb, kk, eb, enb) in rnd:
                act(lfb, fb, AF.Ln, [f"fb{u}"], [f"lfb{u}"])
                P.op("dve", lambda e, bb=bb, lfb=lfb: e.tensor_tensor_scan(out=bb, data0=resetm, data1=lfb, initial=0.0,
                                                                         op0=ALU.mult, op1=ALU.add),
                     r=["resetm", f"lfb{u}"], w=[f"bb{u}"])
                ts("pool", kk, fb, -1.0, 1.0, ALU.mult, ALU.add, [f"fb{u}"], [f"kk{u}"])
            for (u, h, tg, tsl, bq, bf_, sq, fb, lfb, bb, kk, eb, enb) in rnd:
                act(eb, bb, AF.Exp, [f"bb{u}"], [f"eb{u}"])
                act(enb, bb, AF.Exp, [f"bb{u}"], [f"enb{u}"], scale=-1.0)
                tt("dve", qtT[:, h, tsl], sq, eb, ALU.mult, [f"sq{u}", f"eb{u}"], [f"qt{h}"])
                tt("pool", ktT[:, h, tsl], kk, enb, ALU.mult, [f"kk{u}", f"enb{u}"], [f"kt{h}"])
                tcopy("dve", dec[:, h, tg * 8:(tg + 1) * 8], eb[:, 63:512:64], [f"eb{u}"], ["dec"])
        P.barrier()
        if maxphase <= 2:
            break
        A.top = h_top

        wmq = A.alloc_at(seq_top + 24576, [8, 512], BF16)
        wmk = A.alloc_at(seq_top + 24576 + 2048, [8, 512], BF16)
        wmv = A.alloc_at(seq_top + 24576 + 4096, [8, 512], BF16)
        load_w(wmq, win_d, 0, 2048, 512, 8, INC, "wmq", "wmq")
        load_w(wmk, win_d, 0, 2560, 512, 8, INC, "wmk", "wmk")
        load_w(wmv, win_d, 0, 3072, 512, 8, INC, "wmv", "wmv")
        St = A.alloc([4, 128])
        Sb = A.alloc([4, 128], BF16)
        Sbb = A.alloc([4, 128], BF16)
        memset("dve", St, 0.0, ["St"])
        memset("dve", Sb, 0.0, ["Sb"])
        vt = [A.alloc([512], BF16) for _ in range(2)]
        gt = [A.alloc([512], BF16) for _ in range(2)]
        gs = A.alloc([512])
        ktk = [A.alloc([4, 128], BF16) for _ in range(2)]
        scm = A.alloc([4, 128], BF16)
        Tb = A.alloc([4, 128])
        sqo = A.alloc([512])
        ssq = A.alloc([4])
        rs = A.alloc([4])
        otmp = A.alloc([4, 128])
        otok = A.alloc([4, 128], BF16)
        ohs = [A.alloc([4, 512], BF16) for _ in range(2)]
        def f_v(j):
            b = j % 2
            tl = slice(j * 128, (j + 1) * 128)
            mm(ps[0], onesr[0:1, :], brow[0:1, 0:512], True, False, ["onesr", "brow"], ["ps0"])
            for dk in range(8):
                mm(ps[0], xT[:, dk, tl], wi[:, dk, :], False, dk == 7, [f"xT{j}", "wi"], ["ps0"])
            tcopy("act", vt[b], ps[0], ["ps0"], [f"vt{b}"])

        def f_g(j):
            b = j % 2
            tl = slice(j * 128, (j + 1) * 128)
            mm(ps[1], onesr[0:1, :], brow[0:1, 512:1024], True, False, ["onesr", "brow"], ["ps1"])
            for dk in range(8):
                mm(ps[1], xT[:, dk, tl], wg[:, dk, :], False, dk == 7, [f"xT{j}", "wg"], ["ps1"])
            act(gs, ps[1], AF.Silu, ["ps1"], ["gs"])
            tt("pool", gt[b], gs, ng_bc, ALU.mult, ["gs", "ng_bc"], [f"gt{b}"])

        def f_k(j):
            b = j % 2
            tl = slice(j * 128, (j + 1) * 128)
            for h in range(4):
                tr(psb[6][:, h * 128:(h + 1) * 128], ktT[:, h, tl], identb, [f"kt{h}", "identb"], ["ps6"])
            tcopy("act", ktk[b], psb[6][:, 0:512].rearrange("p (a b) -> p a b", a=4, b=128), ["ps6"], [f"ktk{b}"])

        def b_a(j):
            b = j % 2
            tl = slice(j * 128, (j + 1) * 128)
            for h in range(4):
                mm(ps[2][:, h * 128:(h + 1) * 128], ktT[:, h, tl], qtT[:, h, tl], True, True,
                   [f"kt{h}", f"qt{h}"], ["ps2"])
            tt("dve", scm, ps[2].rearrange("p (a b) -> p a b", a=4, b=128), mask2x4, ALU.mult,
               ["ps2", "mask2x4"], ["scm"])
            for h in range(4):
                hs = slice(h * 128, (h + 1) * 128)
                mm(ps[4][:, hs], ktk[b][0:64, h, :], vt[b][0:64, hs], True, True, [f"ktk{b}", f"vt{b}"], ["ps4"])
            for h in range(4):
                hs = slice(h * 128, (h + 1) * 128)
                mm(ps[5][:, hs], ktk[b][64:128, h, :], vt[b][64:128, hs], True, True, [f"ktk{b}", f"vt{b}"], ["ps5"])
            dec0 = dec[:, :, 2 * j:2 * j + 1].to_broadcast([128, 4, 128])
            tt("dve", Tb, ps[4].rearrange("p (a b) -> p a b", a=4, b=128), St, ALU.add, ["ps4", "St"], ["Tb"])
            tt("dve", Sbb, Tb, dec0, ALU.mult, ["Tb", "dec"], ["Sbb"])
            tt("pool", St, Tb, dec0, ALU.mult, ["Tb", "dec"], ["St"])

        def b_b(j):
            b = j % 2
            t0 = j * 128
            dec1 = dec[:, :, 2 * j + 1:2 * j + 2].to_broadcast([128, 4, 128])
            for h in range(4):
                hs = slice(h * 128, (h + 1) * 128)
                mm(ps[3][:, hs], scm[:, h, :], vt[b][:, hs], True, False, ["scm", f"vt{b}"], ["ps3"])
                mm(ps[3][0:64, hs], qtT[:, h, t0:t0 + 64], Sb[:, h, :], False, True, [f"qt{h}", "Sb"], ["ps3"])
                mm(ps[3][64:128, hs], qtT[:, h, t0 + 64:t0 + 128], Sbb[:, h, :], False, True, [f"qt{h}", "Sbb"], ["ps3"])
            tt("dve", Tb, ps[5].rearrange("p (a b) -> p a b", a=4, b=128), St, ALU.add, ["ps5", "St"], ["Tb"])
            tt("dve", Sb, Tb, dec1, ALU.mult, ["Tb", "dec"], ["Sb"])
            tt("pool", St, Tb, dec1, ALU.mult, ["Tb", "dec"], ["St"])

        def b_c(j):
            b = j % 2
            act(sqo, ps[3], AF.Square, ["ps3"], ["sqo"])
            P.op("dve", lambda e: e.tensor_reduce(out=ssq, in_=sqo.rearrange("p (a b) -> p a b", a=4, b=128),
                                                  axis=AX.X, op=ALU.add), r=["sqo"], w=["ssq"])
            ts("dve", ssq, ssq, 1.0 / 128.0, EPS, ALU.mult, ALU.add, ["ssq"], ["ssq"])
            act(rs, ssq, AF.Ln, ["ssq"], ["rs"])
            act(rs, rs, AF.Exp, ["rs"], ["rs"], scale=-0.5)
            tt("dve", otmp, ps[3].rearrange("p (a b) -> p a b", a=4, b=128),
               rs.unsqueeze(2).to_broadcast([128, 4, 128]), ALU.mult, ["ps3", "rs"], ["otmp"])
            tt("pool", otok, otmp, gt[b].rearrange("p (a b) -> p a b", a=4, b=128), ALU.mult,
               ["otmp", f"gt{b}"], ["otok"])

        def b_d(j):
            for h in range(4):
                tr(psb[7][:, h * 128:(h + 1) * 128], otok[:, h, :], identb, ["otok", "identb"], ["ps7"])
            g4 = (j // 4) % 2
            tcopy("act", ohs[g4][:, :, (j % 4) * 128:(j % 4 + 1) * 128],
                  psb[7][:, 0:512].rearrange("p (a b) -> p a b", a=4, b=128), ["ps7"], [f"ohs{g4}"])
            if j % 4 == 3:
                tg = j // 4
                dma("sp", dap(ohg_d, tg * 512, [[S, 128], [128 * S, 4], [1, 512]]), ohs[g4],
                    r=[f"ohs{g4}"], key=f"ohs{g4}")
                if debug and s == 0:
                    dma("sp", dap(dbg["ohg"], tg * 512, [[S, 128], [128 * S, 4], [1, 512]]), ohs[g4],
                        r=[f"ohs{g4}"], key=f"dbgohs{g4}")

        f_v(0)
        f_g(0)
        f_k(0)
        for j in range(16):
            nx = j + 1 < 16
            b_a(j)
            if nx:
                f_v(j + 1)
                f_k(j + 1)
            if j >= 1:
                b_d(j - 1)
            b_b(j)
            if nx:
                f_g(j + 1)
            b_c(j)
        b_d(15)
        P.barrier()
        if maxphase <= 3:
            break
        A.top = seq_top

        mqE = A.alloc([4, S], BF16)
        memset("dve", A.last_raw[64:128], 0.0, ["mqz"])
        mqO = A.alloc([4, S], BF16)
        memset("dve", A.last_raw[0:64], 0.0, ["mqz"])
        mkE = A.alloc([4, S], BF16)
        memset("dve", A.last_raw[64:128], 0.0, ["mkz"])
        mkO = A.alloc([4, S], BF16)
        memset("dve", A.last_raw[0:64], 0.0, ["mkz"])
        for p in range(4):
            dma("pool", mkE[64:72, p, :], eind_d.ap(), r=["mkz"], w=[f"mkiE{p}"], key="mkzE")
            dma("pool", mkO[0:8, p, :], eind_d.ap(), r=["mkz"], w=[f"mkiO{p}"], key="mkzO")
        vaug = A.alloc([16, 8, 65], BF16)
        kmE = A.alloc([4, 8], BF16)
        kmO = A.alloc([4, 8], BF16)
        memset("dve", kmE.rearrange("p a b -> p (a b)"), 0.0, ["kmT"])
        memset("dve", kmO.rearrange("p a b -> p (a b)"), 0.0, ["kmT"])
        km32 = A.alloc([4, 8])
        m_top = A.top
        assert A.top <= seq_top + 24576, A.top - seq_top
        memset("dve", vaug.rearrange("p a b c -> p (a b c)"), 1.0, ["vaug"])
        it = 0
        for p in range(4):
            for tg in range(4):
                bq, bk = (it % 2) * 2, (it % 2) * 2 + 1
                it += 1
                tsl = slice(tg * 512, (tg + 1) * 512)
                for dk in range(8):
                    mm(ps[bq], wmq[:, dk, p * 128:(p + 1) * 128], xT[:, dk, tsl], dk == 0, dk == 7,
                       ["wmq"] + XT(tg * 4, 4), [PSN[bq]])
                for dk in range(8):
                    mm(ps[bk], wmk[:, dk, p * 128:(p + 1) * 128], xT[:, dk, tsl], dk == 0, dk == 7,
                       ["wmk"] + XT(tg * 4, 4), [PSN[bk]])
                act(mqE[0:64, p, tsl], ps[bq][0:64], AF.Identity, [PSN[bq], "bcolq", "mqz"], [f"mq{p}"], bias=bcolq[0:64, p:p + 1], scale=0.125)
                act(mqO[64:128, p, tsl], ps[bq][64:128], AF.Identity, [PSN[bq], "bcolq", "mqz"], [f"mq{p}"], bias=bcolq[64:128, p:p + 1], scale=0.125)
                ts("dve", mkE[0:64, p, tsl], ps[bk][0:64], bcol[0:64, 20 + p:21 + p], None, ALU.add, None, [PSN[bk], "bcol", "mkz"], [f"mk{p}"])
                ts("dve", mkO[64:128, p, tsl], ps[bk][64:128], bcol[64:128, 20 + p:21 + p], None, ALU.add, None, [PSN[bk], "bcol", "mkz"], [f"mk{p}"])
            P.op("dve", lambda e, p=p: e.tensor_reduce(out=km32[0:64, p, :], in_=mkE[0:64, p, :].rearrange("p (a b) -> p a b", a=8, b=256),
                                                       axis=AX.X, op=ALU.add), r=[f"mk{p}"], w=["km32"])
            P.op("dve", lambda e, p=p: e.tensor_reduce(out=km32[64:128, p, :], in_=mkO[64:128, p, :].rearrange("p (a b) -> p a b", a=8, b=256),
                                                       axis=AX.X, op=ALU.add), r=[f"mk{p}"], w=["km32"])
        ts("dve", kmE[0:64], km32[0:64], 1.0 / 256.0, None, ALU.mult, None, ["km32", "kmT"], ["kmT"])
        ts("dve", kmO[64:128], km32[64:128], 1.0 / 256.0, None, ALU.mult, None, ["km32", "kmT"], ["kmT"])
        gm = [A.alloc([8, 8]) for _ in range(2)]
        top8 = [A.alloc([8, 8]) for _ in range(2)]
        thr = [A.alloc([8]) for _ in range(2)]
        sel = [A.alloc([8, 8]) for _ in range(2)]
        Mtok = [A.alloc([8, 8], BF16) for _ in range(2)]
        PT = [A.alloc([2, 256], BF16) for _ in range(3)]
        osb = A.alloc([256])
        rden = A.alloc([256])
        onesf = A.alloc([64])
        memset("dve", onesf[64:65, :], 1.0, ["onesf"])
        oms = [A.alloc([8, 256], BF16) for _ in range(2)]

        def gate_front(qb, jls=(0, 1)):
            for jl in jls:
                jt = 2 * qb + jl
                tl = slice(jt * 128, (jt + 1) * 128)
                for h in range(8):
                    p_ = h // 2
                    mm(ps[6][:, jl * 64 + h * 8:jl * 64 + (h + 1) * 8], (mqO if h % 2 else mqE)[:, p_, tl], (kmO if h % 2 else kmE)[:, p_, :],
                       True, True, [f"mq{p_}", "kmT"], ["ps6"])
                tt("dve", gm[jl], ps[6][:, jl * 64:(jl + 1) * 64].rearrange("p (a b) -> p a b", a=8, b=8), pastm[:, qb], ALU.add,
                   ["ps6", "cst"], [f"gm{jl}"])
                for h in range(8):
                    P.op("dve", lambda e, h=h, jl=jl: e.max(out=top8[jl][:, h, :], in_=gm[jl][:, h, :]), r=[f"gm{jl}"], w=[f"top8{jl}"])
                ts("dve", thr[jl], top8[jl][:, :, 2], -1e29, None, ALU.max, None, [f"top8{jl}"], [f"thr{jl}"])
                tt("dve", sel[jl], gm[jl], thr[jl].unsqueeze(2).to_broadcast([128, 8, 8]), ALU.is_ge, [f"gm{jl}", f"thr{jl}"], [f"sel{jl}"])
                tt("dve", sel[jl], sel[jl], ownm[:, qb], ALU.max, [f"sel{jl}", "cst"], [f"sel{jl}"])
                ts("dve", sel[jl], sel[jl], -NEG, NEG, ALU.mult, ALU.add, [f"sel{jl}"], [f"sel{jl}"])
                tt("dve", Mtok[jl], sel[jl], rbm, ALU.add, [f"sel{jl}", "rbm"], [f"Mtok{jl}"])

        def gate_back(qb):
            for jl in range(2):
                qc = slice(qb * 256 + jl * 128, qb * 256 + (jl + 1) * 128)
                for h in range(8):
                    r0 = 0 if h % 2 else 64
                    tr(psb[7][r0:r0 + 8, h * 128:(h + 1) * 128], Mtok[jl][:, h, :], identb, [f"Mtok{jl}", "identb"], ["ps7"])
                p7 = psb[7].rearrange("p (a b c) -> p a b c", a=4, b=2, c=128)
                act(mqE[64:72, :, qc], p7[64:72, :, 0, :], AF.Copy, ["ps7"], [f"mq{p}" for p in range(4)])
                act(mqO[0:8, :, qc], p7[0:8, :, 1, :], AF.Copy, ["ps7"], [f"mq{p}" for p in range(4)])

        sc_state = {"i": 0}
        MKI = [f"mkiE{p}" for p in range(4)] + [f"mkiO{p}" for p in range(4)]

        def scores(qb, h, n):
            mb = qb % 2
            p_ = h // 2
            sb_ = sc_state["i"] % 3
            sc_state["i"] += 1
            psc = ps[sb_]
            qsel = mqO if h % 2 else mqE
            q_all = qsel[:, p_, qb * 256:(qb + 1) * 256]
            q_hi = qsel[:, p_, qb * 256 + 128:(qb + 1) * 256]
            own = (n == qb)
            for jj in range(2):
                kt_ = 2 * n + jj
                ktile = (mkO if h % 2 else mkE)[:, p_, kt_ * 128:(kt_ + 1) * 128]
                if own and jj == 1:
                    osl = slice(jj * 256 + 128, jj * 256 + 256)
                    mm(psc[:, osl], ktile, q_hi, True, False, [f"mk{p_}", f"mq{p_}"] + MKI, [PSN[sb_]])
                    mm(psc[:, osl], antib, Yc[:, h * 2 + 0, :], False, True, ["antib", "Yc"], [PSN[sb_]])
                else:
                    osl = slice(jj * 256, jj * 256 + 256)
                    corr = own or (n == qb - 1 and jj == 1)
                    mm(psc[:, osl], ktile, q_all, True, not corr, [f"mk{p_}", f"mq{p_}"] + MKI, [PSN[sb_]])
                    if own:
                        mm(psc[:, jj * 256:jj * 256 + 128], antib, Yc[:, h * 2 + 0, :], False, False, ["antib", "Yc"], [PSN[sb_]])
                        mm(psc[:, jj * 256 + 128:jj * 256 + 256], antib, Yc[:, h * 2 + 1, :], False, True, ["antib", "Yc"], [PSN[sb_]])
                    elif corr:
                        mm(psc[:, jj * 256:jj * 256 + 128], antib, Yc[:, h * 2 + 1, :], False, True, ["antib", "Yc"], [PSN[sb_]])
            return sb_

        def exp_pv(qb, h, n, sb_):
            po = 4 + h % 2
            psc = ps[sb_]
            pt = PT[sb_]
            first = (n == 0)
            if n == qb:
                act(pt[:, 0, :], psc[:, 0:256], AF.Exp, [PSN[sb_]], [f"PT{sb_}"])
                act(pt[:, 1, 128:256], psc[:, 384:512], AF.Exp, [PSN[sb_]], [f"PT{sb_}"])
                mm(ps[po][0:65, 0:256], vaug[:, 2 * n, h, :], pt[:, 0, :], first, False, [f"va{2 * n}", f"PT{sb_}"], [PSN[po]])
                mm(ps[po][0:65, 128:256], vaug[:, 2 * n + 1, h, :], pt[:, 1, 128:256], False, True,
                   [f"va{2 * n + 1}", f"PT{sb_}"], [PSN[po]])
            else:
                act(pt.rearrange("p a b -> p (a b)"), psc, AF.Exp, [PSN[sb_]], [f"PT{sb_}"])
                mm(ps[po][0:65, 0:256], vaug[:, 2 * n, h, :], pt[:, 0, :], first, False, [f"va{2 * n}", f"PT{sb_}"], [PSN[po]])
                mm(ps[po][0:65, 0:256], vaug[:, 2 * n + 1, h, :], pt[:, 1, :], False, False,
                   [f"va{2 * n + 1}", f"PT{sb_}"], [PSN[po]])

        def norm1(h):
            po = 4 + h % 2
            tcopy("act", osb[0:64, :], ps[po][0:64, 0:256], [PSN[po]], ["osb"])
            P.op("dve", lambda e, po=po: e.reciprocal(out=rden[64:65, :], in_=ps[po][64:65, 0:256]), r=[PSN[po]], w=["rden"])

        def norm2(qb, h):
            mb = qb % 2
            mm(ps[3][0:64, 0:256], onesf[64:65, 0:64], rden[64:65, :], True, True, ["onesf", "rden"], ["ps3"])
            tt("dve", oms[mb][0:64, h, :], osb[0:64, :], ps[3][0:64, 0:256], ALU.mult, ["osb", "ps3"], [f"oms{mb}"])

        gate_front(0)
        gate_back(0)
        for j in range(16):
            b = 4 + j % 2
            tl = slice(j * 128, (j + 1) * 128)
            mm(ps[b], onesr[0:1, :], brow[0:1, 1024:1536], True, False, ["onesr", "brow"], [PSN[b]])
            for dk in range(8):
                mm(ps[b], xT[:, dk, tl], wmv[:, dk, :], False, dk == 7, [f"xT{j}", "wmv"], [PSN[b]])
            tcopy("act" if j % 2 else "dve", vaug[:, j, :, 0:64], ps[b].rearrange("p (a b) -> p a b", a=8, b=64),
                  [PSN[b]], [f"va{j}", "vaug"])
        assert A.top <= seq_top + 24576, A.top - seq_top
        for qb in range(8):
            mb = qb % 2
            blocks = [(h, n) for h in range(8) for n in range(qb + 1)]
            pend = None
            sbs = {0: scores(qb, *blocks[0]), 1: scores(qb, *blocks[1])}
            for i, (h, n) in enumerate(blocks):
                if i + 2 < len(blocks):
                    sbs[i + 2] = scores(qb, *blocks[i + 2])
                exp_pv(qb, h, n, sbs[i])
                if pend is not None:
                    norm2(qb, pend)
                    pend = None
                if n == qb:
                    norm1(h)
                    pend = h
                    if qb + 1 < 8 and h in (1, 4):
                        gate_front(qb + 1, (0,) if h == 1 else (1,))
            norm2(qb, pend)
            if qb + 1 < 8:
                gate_back(qb + 1)
            dma("sp", dap(omb_d, qb * 256, [[S, 64], [64 * S, 8], [1, 256]]), oms[mb][0:64],
                r=[f"oms{mb}"], key=f"oms{mb}")
            if debug and s == 0:
                dma("sp", dap(dbg["omb"], qb * 256, [[S, 64], [64 * S, 8], [1, 256]]), oms[mb][0:64],
                    r=[f"oms{mb}"], key=f"dbgoms{mb}")
        P.barrier()
        if maxphase <= 4:
            break
        A.top = seq_top

        mT = A.alloc([8, S], BF16)
        xa_top = A.top
        wga = A.alloc([8, 1024], BF16)
        wgb = A.alloc([8, 1024], BF16)
        wpa = A.alloc([4, 1024], BF16)
        wpb = A.alloc([4, 1024], BF16)
        load_w(wga, win_d, 0, 3584, 1024, 8, INC, "wA", "wga")
        load_w(wgb, win_d, 0, 4608, 1024, 8, INC, "wB", "wgb")
        load_w(wpa, wpa_d, 0, 0, 1024, 4, D, "wC", "wpa")
        load_w(wpb, wpb_d, 0, 0, 1024, 4, D, "wD", "wpb")
        ohg_g = [A.alloc([4, 512], BF16) for _ in range(2)]
        omb_g = [A.alloc([4, 512], BF16) for _ in range(2)]
        sga = A.alloc([512])
        sgb = A.alloc([512])
        t1 = A.alloc([512])
        t2 = A.alloc([512])
        it = 0
        for tg in range(4):
            gb_ = tg % 2
            tsl = slice(tg * 512, (tg + 1) * 512)
            dma("sp", ohg_g[gb_], dap(ohg_d, tg * 512, [[S, 128], [128 * S, 4], [1, 512]]), w=[f"ohg_g{gb_}"], key=f"ohg_g{gb_}")
            dma("sp", omb_g[gb_], dap(omb_d, tg * 512, [[S, 128], [128 * S, 4], [1, 512]]), w=[f"omb_g{gb_}"], key=f"omb_g{gb_}")
            for c in range(8):
                o4 = (it % 2) * 4
                it += 1
                csl = slice(c * 128, (c + 1) * 128)
                for dk in range(8):
                    mm(ps[o4], wga[:, dk, csl], xT[:, dk, tsl], dk == 0, dk == 7, ["wga"] + XT(tg * 4, 4), [PSN[o4]])
                for dk in range(8):
                    mm(ps[o4 + 1], wgb[:, dk, csl], xT[:, dk, tsl], dk == 0, dk == 7, ["wgb"] + XT(tg * 4, 4), [PSN[o4 + 1]])
                for h in range(4):
                    mm(ps[o4 + 2], wpa[:, h, csl], ohg_g[gb_][:, h, :], h == 0, h == 3, ["wpa", f"ohg_g{gb_}"], [PSN[o4 + 2]])
                for h in range(4):
                    mm(ps[o4 + 3], wpb[:, h, csl], omb_g[gb_][:, h, :], h == 0, h == 3, ["wpb", f"omb_g{gb_}"], [PSN[o4 + 3]])
                act(sga, ps[o4], AF.Sigmoid, [PSN[o4], "bcol"], ["sga"], bias=bcol[:, 28 + c:29 + c])
                act(sgb, ps[o4 + 1], AF.Sigmoid, [PSN[o4 + 1], "bcol"], ["sgb"], bias=bcol[:, 36 + c:37 + c])
                tt("dve", t1, sga, ps[o4 + 2], ALU.mult, ["sga", PSN[o4 + 2]], ["t1"])
                tt("dve", t2, sgb, ps[o4 + 3], ALU.mult, ["sgb", PSN[o4 + 3]], ["t2"])
                tt("pool", mT[:, c, tsl], t1, t2, ALU.add, ["t1", "t2"], [f"mT{tg}"])
        P.barrier()
        if maxphase <= 5:
            break
        A.top = xa_top

        wo = A.alloc([8, 1024], BF16)
        load_w(wo, wout_d, 0, 0, 1024, 8, D, "wA", "wo")
        if s == 1:
            wgu0 = A.alloc_at(A.W - 6144, [8, 1024], BF16)
            wdn0 = A.alloc_at(A.W - 2048, [4, 1024], BF16)
            load_w(wgu0, wgu_d, 0, 0, 1024, 8, 1024, "wgu0", "wgu0")
            load_w(wdn0, wdn_d, 0, 0, 1024, 4, D, "wdn0", "wdn0")
        l1g = A.alloc([1024])
        l1b = A.alloc([1024])
        dma("sp", l1g, dap(ln1g_d, 0, [[0, 128], [1, D]]), w=["l1g"], key="l1g")
        dma("sp", l1b, dap(ln1b_d, 0, [[0, 128], [1, D]]), w=["l1b"], key="l1b")
        xin = [A.alloc([1024]) for _ in range(2)]
        rr = A.alloc([1024])
        st = A.alloc([12])
        mv = A.alloc([2])
        rsd = A.alloc([1])
        nmr = A.alloc([1])
        x1 = [A.alloc([1024]) for _ in range(3)]
        x1b = [A.alloc([1024], BF16) for _ in range(4)]
        x1T_ = [A.alloc([8, 128]) for _ in range(2)]
        lg = A.alloc([36])
        sm = A.alloc([16])
        ohg4 = A.alloc([4])
        pen = A.alloc([4])
        em = A.alloc([4, 8])
        t8 = A.alloc([8])
        oh1_ = [A.alloc([32]) for _ in range(2)]
        oh2_ = [A.alloc([32]) for _ in range(2)]
        cnt_ = [A.alloc([32], BF16) for _ in range(2)]
        slot = A.alloc([32])
        tmp32 = A.alloc([32])
        dfl = A.alloc([2])
        def xb_A_mm(j):
            b = j % 2
            tile = s * 16 + j
            tl = slice(j * 128, (j + 1) * 128)
            dma("sp", xin[b], dap(x_d, tile * 128 * D, [[D, 128], [1, D]]), w=[f"xin{b}"], key=f"xin{b}")
            for half in range(2):
                for dk in range(8):
                    mm(ps[half], mT[:, dk, tl], wo[:, dk, half * 512:(half + 1) * 512], dk == 0, dk == 7,
                       [f"mT{j // 4}", "wo"], [PSN[half]])

        def xb_A_rest(j):
            b = j % 2
            tile = s * 16 + j
            for half in range(2):
                hsl = slice(half * 512, (half + 1) * 512)
                stt(rr[:, hsl], xin[b][:, hsl], ALPHA, ps[half], ALU.mult, ALU.add, [f"xin{b}", PSN[half]], ["rr"])
            P.op("dve", lambda e: e.bn_stats(out=st[:, 0:6], in_=rr[:, 0:512]), r=["rr"], w=["st"])
            P.op("dve", lambda e: e.bn_stats(out=st[:, 6:12], in_=rr[:, 512:1024]), r=["rr"], w=["st"])
            P.op("dve", lambda e: e.bn_aggr(out=mv, in_=st), r=["st"], w=["mv"])
            ts("dve", rsd, mv[:, 1:2], EPS, None, ALU.add, None, ["mv"], ["rsd"])
            act(rsd, rsd, AF.Ln, ["rsd"], ["rsd"])
            act(rsd, rsd, AF.Exp, ["rsd"], ["rsd"], scale=-0.5)
            stt(nmr, mv[:, 0:1], -1.0, rsd[:, 0:1], ALU.mult, ALU.mult, ["mv", "rsd"], ["nmr"])
            act(rr, rr, AF.Identity, ["rr", "rsd", "nmr"], ["rr"], bias=nmr[:, 0:1], scale=rsd[:, 0:1])
            tt("pool", rr, rr, l1g, ALU.mult, ["rr", "l1g"], ["rr"])
            tt("pool", x1[j % 3], rr, l1b, ALU.add, ["rr", "l1b"], [f"x1{j % 3}"])
            dma("sp", dap(x1_d, tile * 128 * D, [[D, 128], [1, D]]), x1[j % 3], r=[f"x1{j % 3}"], key=f"x1o{j % 3}")
            if debug:
                dma("sp", dap(dbg["x1"], tile * 128 * D, [[D, 128], [1, D]]), x1[j % 3], r=[f"x1{j % 3}"], key=f"dbgx1{j % 3}")
            tcopy("act", x1b[j % 4], x1[j % 3], [f"x1{j % 3}"], [f"x1b{j % 4}"])

        def xb_B0(j):
            b = j % 2
            x1T = x1T_[b]
            for dk in range(8):
                tr(ps[2 + dk // 4][:, (dk % 4) * 128:(dk % 4 + 1) * 128], x1[j % 3][:, dk * 128:(dk + 1) * 128], identf,
                   [f"x1{j % 3}", "cst"], [PSN[2 + dk // 4]])
            tcopy("act", x1T[:, 0:4, :], ps[2].rearrange("p (a b) -> p a b", a=4, b=128), ["ps2"], [f"x1T{b}"])
            tcopy("act", x1T[:, 4:8, :], ps[3].rearrange("p (a b) -> p a b", a=4, b=128), ["ps3"], [f"x1T{b}"])

        def xb_B1(j):
            b = j % 2
            tile = s * 16 + j
            x1T = x1T_[b]
            oh1, oh2, cnt = oh1_[b], oh2_[b], cnt_[b]
            for dk in range(8):
                mm(ps[4][:, 0:36], x1T[:, dk, :], wr[:, dk, :], dk == 0, dk == 7, [f"x1T{b}", "wr"], ["ps4"])
            tt("dve", lg, ps[4][:, 0:36], br_bc, ALU.add, ["ps4", "br_bc"], ["lg"])
            gmax, ngmax, sg_, pg_, v21, e21, den_, w1_, w2_ = (sm[:, i:i + 1] for i in range(9))
            P.op("dve", lambda e, gmax=gmax: e.tensor_reduce(out=gmax, in_=lg[:, 0:4], axis=AX.X, op=ALU.max), r=["lg"], w=["sm0"])
            ts("dve", ohg4, lg[:, 0:4], gmax, None, ALU.is_equal, None, ["lg", "sm0"], ["ohg4"])
            ts("dve", ngmax, gmax, -1.0, None, ALU.mult, None, ["sm0"], ["sm1"])
            act(pen, lg[:, 0:4], AF.Exp, ["lg", "sm1"], ["pen", "sm2"], bias=ngmax, accum=sg_)
            P.op("dve", lambda e, pg_=pg_, sg_=sg_: e.reciprocal(out=pg_, in_=sg_), r=["sm2"], w=["sm3"])
            ts("dve", pen, ohg4, 1e30, -1e30, ALU.mult, ALU.add, ["ohg4", "pen"], ["pen"])
            tt("dve", em, lg[:, 4:36].rearrange("p (a b) -> p a b", a=4, b=8), pen.unsqueeze(2).to_broadcast([128, 4, 8]),
               ALU.add, ["lg", "pen"], ["em"])
            emf = em.rearrange("p a b -> p (a b)")
            P.op("dve", lambda e, emf=emf: e.max(out=t8, in_=emf), r=["em"], w=["t8"])
            ts("dve", oh1, emf, t8[:, 0:1], None, ALU.is_equal, None, ["em", "t8"], [f"oh1{b}"])
            ts("dve", oh2, emf, t8[:, 1:2], None, ALU.is_equal, None, ["em", "t8"], [f"oh2{b}"])
            tt("dve", v21, t8[:, 1:2], t8[:, 0:1], ALU.subtract, ["t8"], ["sm4"])
            act(e21, v21, AF.Exp, ["sm4"], ["sm5"])
            ts("dve", den_, e21, 1.0, None, ALU.add, None, ["sm5"], ["sm6"])
            P.op("dve", lambda e, w1_=w1_, den_=den_: e.reciprocal(out=w1_, in_=den_), r=["sm6"], w=["sm7"])
            tt("dve", w2_, e21, w1_, ALU.mult, ["sm5", "sm7"], ["sm8"])
            tt("dve", wts[:, tile, 0:1], w1_, pg_, ALU.mult, ["sm7", "sm3"], [f"wts{tile}a"])
            tt("dve", wts[:, tile, 1:2], w2_, pg_, ALU.mult, ["sm8", "sm3"], [f"wts{tile}b"])
            tt("dve", cnt, oh1, oh2, ALU.add, [f"oh1{b}", f"oh2{b}"], [f"cnt{b}"])

        def xb_B2(j):
            b = j % 2
            tile = s * 16 + j
            oh1, oh2, cnt = oh1_[b], oh2_[b], cnt_[b]
            mm(ps[5][:, 0:32], ltrib, cnt, True, True, ["ltrib", f"cnt{b}"], ["ps5"])
            mm(ps[5][:, 32:64], onesb, cnt, True, True, ["onesb", f"cnt{b}"], ["ps5"])
            tt("dve", slot, ps[5][:, 0:32], carry, ALU.add, ["ps5", "carry"], ["slot"])
            tt("dve", carry, ps[5][:, 32:64], carry, ALU.add, ["ps5", "carry"], ["carry"])
            for k, oh in ((0, oh1), (1, oh2)):
                tt("dve", tmp32, oh, slot, ALU.mult, [f"oh{k + 1}{b}", "slot"], ["tmp32"])
                P.op("dve", lambda e, k=k: e.tensor_reduce(out=dfl[:, k:k + 1], in_=tmp32, axis=AX.X, op=ALU.add),
                     r=["tmp32"], w=["dfl"])
            tcopy("dve", dest_i[:, tile, :], dfl, ["dfl"], [f"dest{tile}"])
            for k in range(2):
                P.op("pool", lambda e, k=k, tile=tile, b=b: e.indirect_dma_start(
                    out=xbuf_d.ap(), out_offset=bass.IndirectOffsetOnAxis(ap=dest_i[:, tile, k:k + 1], axis=0),
                    in_=x1b[j % 4], in_offset=None), r=[f"x1b{j % 4}", f"dest{tile}"], dma=f"scat{j % 4}", extra=zfill_ops)

        for j0 in range(2):
            xb_A_mm(j0)
            xb_A_rest(j0)
        xb_B0(0)
        for j in range(16):
            xb_B1(j)
            if j + 2 < 16:
                xb_A_mm(j + 2)
            if j + 1 < 16:
                xb_B0(j + 1)
            if j + 2 < 16:
                xb_A_rest(j + 2)
            if j >= 1:
                xb_B2(j - 1)
        xb_B2(15)
        P.barrier()

    if stage >= 2:
        A.top = persist_top
        wgu = [wgu0, A.alloc([8, 1024], BF16)]
        wdn = [wdn0, A.alloc([4, 1024], BF16)]
        xe = [A.alloc([3, 1024], BF16) for _ in range(2)]
        xTe = [A.alloc([8, CAP], BF16) for _ in range(2)]
        hact = A.alloc([4, CAP], BF16)
        sgs = [A.alloc([CAP]) for _ in range(2)]
        ysb = [A.alloc([1024]) for _ in range(4)]
        yi = 0
        dma("sp", xe[0], dap(xbuf_d, 0, [[D, 128], [128 * D, 3], [1, D]]), w=["xe0"], key="xe0")
        for e_ in range(NE):
            b = e_ % 2
            if e_ > 0:
                load_w(wgu[b], wgu_d, e_ * D, 0, 1024, 8, 1024, f"wgu{b}", f"wgu{b}")
                load_w(wdn[b], wdn_d, e_ * 512, 0, 1024, 4, D, f"wdn{b}", f"wdn{b}")
            if e_ + 1 < NE:
                dma("sp", xe[1 - b], dap(xbuf_d, (e_ + 1) * CAP * D, [[D, 128], [128 * D, 3], [1, D]]),
                    w=[f"xe{1 - b}"], key=f"xe{1 - b}")
            for r_ in range(3):
                tb = r_ % 2
                for dk in range(8):
                    tr(psb[tb][:, dk * 128:(dk + 1) * 128], xe[b][:, r_, dk * 128:(dk + 1) * 128], identb,
                       [f"xe{b}", "identb"], [PSN[tb]])
                tcopy("act" if r_ % 2 else "dve", xTe[b][:, :, r_ * 128:(r_ + 1) * 128],
                      psb[tb].rearrange("p (a b) -> p a b", a=8, b=128), [PSN[tb]], [f"xTe{b}"])
            for fc in range(4):
                pg_b, pu_b = 2 + (fc % 2) * 2, 3 + (fc % 2) * 2
                for dk in range(8):
                    mm(ps[pg_b][:, 0:CAP], wgu[b][:, dk, fc * 128:(fc + 1) * 128], xTe[b][:, dk, :], dk == 0, dk == 7,
                       [f"wgu{b}", f"xTe{b}"], [PSN[pg_b]])
                for dk in range(8):
                    mm(ps[pu_b][:, 0:CAP], wgu[b][:, dk, 512 + fc * 128:512 + (fc + 1) * 128], xTe[b][:, dk, :], dk == 0, dk == 7,
                       [f"wgu{b}", f"xTe{b}"], [PSN[pu_b]])
                act(sgs[fc % 2], ps[pg_b][:, 0:CAP], AF.Silu, [PSN[pg_b]], [f"sgs{fc % 2}"])
                tt("dve", hact[:, fc, :], sgs[fc % 2], ps[pu_b][:, 0:CAP], ALU.mult, [f"sgs{fc % 2}", PSN[pu_b]], ["hact"])
            for r_ in range(3):
                yb = yi % 4
                yi += 1
                for half in range(2):
                    pb = 6 + half
                    for fk in range(4):
                        mm(ps[pb], hact[:, fk, r_ * 128:(r_ + 1) * 128], wdn[b][:, fk, half * 512:(half + 1) * 512],
                           fk == 0, fk == 3, ["hact", f"wdn{b}"], [PSN[pb]])
                tcopy("act", ysb[yb][:, 0:512], ps[6], ["ps6"], [f"ysb{yb}"])
                tcopy("dve", ysb[yb][:, 512:1024], ps[7], ["ps7"], [f"ysb{yb}"])
                dma("sp", dap(ybuf_d, (e_ * CAP + r_ * 128) * D, [[D, 128], [1, D]]), ysb[yb],
                    r=[f"ysb{yb}"], key=f"ysb{yb}")
        P.barrier()

        A.top = persist_top
        l2g = A.alloc([1024])
        l2b = A.alloc([1024])
        dma("sp", l2g, dap(ln2g_d, 0, [[0, 128], [1, D]]), w=["l2g"], key="l2g")
        dma("sp", l2b, dap(ln2b_d, 0, [[0, 128], [1, D]]), w=["l2b"], key="l2b")
        y1 = [A.alloc([1024]) for _ in range(3)]
        y2 = [A.alloc([1024]) for _ in range(3)]
        xs = [A.alloc([1024]) for _ in range(3)]
        zz = A.alloc([1024])
        zn = [A.alloc([1024]) for _ in range(2)]
        ot = [A.alloc([1024]) for _ in range(2)]
        st2 = A.alloc([12])
        mv2 = A.alloc([2])
        rsd2 = A.alloc([1])
        nmr2 = A.alloc([1])
        def c_issue(tile):
            b = tile % 3
            for k, yk in ((0, y1[b]), (1, y2[b])):
                P.op("pool", lambda e, k=k, tile=tile, yk=yk: e.indirect_dma_start(
                    out=yk, out_offset=None, in_=ybuf_d.ap(),
                    in_offset=bass.IndirectOffsetOnAxis(ap=dest_i[:, tile, k:k + 1], axis=0)),
                    r=[f"dest{tile}"], w=[f"y{k}{b}"], dma=f"gy{k}{b}")
            dma("sp", xs[b], dap(x1_d, tile * 128 * D, [[D, 128], [1, D]]), w=[f"xs{b}"], key=f"xs{b}")

        c_issue(0)
        c_issue(1)
        for tile in range(32):
            b = tile % 2
            c = tile % 3
            if tile + 2 < 32:
                c_issue(tile + 2)
            act(xs[c], xs[c], AF.Copy, [f"xs{c}"], [f"xs{c}"], scale=ALPHA)
            stt(zz, y1[c], wts[:, tile, 0:1], xs[c], ALU.mult, ALU.add, [f"y0{c}", f"xs{c}"], ["zz"])
            stt(zz, y2[c], wts[:, tile, 1:2], zz, ALU.mult, ALU.add, [f"y1{c}", "zz"], ["zz"])
            P.op("dve", lambda e: e.bn_stats(out=st2[:, 0:6], in_=zz[:, 0:512]), r=["zz"], w=["st"])
            P.op("dve", lambda e: e.bn_stats(out=st2[:, 6:12], in_=zz[:, 512:1024]), r=["zz"], w=["st"])
            P.op("dve", lambda e: e.bn_aggr(out=mv2, in_=st2), r=["st"], w=["mv"])
            ts("dve", rsd2, mv2[:, 1:2], EPS, None, ALU.add, None, ["mv"], ["rsd"])
            act(rsd2, rsd2, AF.Ln, ["rsd"], ["rsd"])
            act(rsd2, rsd2, AF.Exp, ["rsd"], ["rsd"], scale=-0.5)
            stt(nmr2, mv2[:, 0:1], -1.0, rsd2[:, 0:1], ALU.mult, ALU.mult, ["mv", "rsd"], ["nmr"])
            act(zn[b], zz, AF.Identity, ["zz", "rsd", "nmr"], [f"zn{b}"], bias=nmr2[:, 0:1], scale=rsd2[:, 0:1])
            tt("pool", zn[b], zn[b], l2g, ALU.mult, [f"zn{b}", "l2g"], [f"zn{b}"])
            tt("dve", ot[b], zn[b], l2b, ALU.add, [f"zn{b}", "l2b"], [f"ot{b}"])
            dma("sp", dap(y_d, tile * 128 * D, [[D, 128], [1, D]]), ot[b], r=[f"ot{b}"], key=f"ot{b}")
    P.emit()
    return nc, A.peak


_CACHE = {}


def kernel(x, w_in, b_in, lb_logits, hg_norm_g, rel_bias, w_proj_a, w_proj_b, w_out, ln1_g, ln1_b,
           w_group, b_group, w_expert, b_expert, w_gate_up, w_down, ln2_g, ln2_b):
    f = lambda a: np.ascontiguousarray(np.asarray(a, dtype=np.float32))
    if "nc" not in _CACHE:
        _CACHE["nc"] = build()[0]
    nc = _CACHE["nc"]
    x = f(x)
    shared = {
        "w_in": f(w_in[0]), "b_in": f(b_in[0]), "lb_logits": f(lb_logits), "hg_norm_g": f(hg_norm_g[0]),
        "rel_bias": f(rel_bias), "w_proj_a": f(w_proj_a[0]), "w_proj_b": f(w_proj_b[0]), "w_out": f(w_out[0]),
        "ln1_g": f(ln1_g[0]), "ln1_b": f(ln1_b[0]),
        "w_router": f(np.concatenate([np.asarray(w_group[0]), np.asarray(w_expert[0])], axis=1)),
        "b_router": f(np.concatenate([np.asarray(b_group[0]), np.asarray(b_expert[0])], axis=0)),
        "w_gate_up": f(w_gate_up[0]), "w_down": f(w_down[0]), "ln2_g": f(ln2_g[0]), "ln2_b": f(ln2_b[0]),
        "consts": make_consts(),
        "eind": np.ascontiguousarray((np.arange(S)[None, :] // 256 == np.arange(8)[:, None]).astype(np.float32)),
    }
    in_maps = []
    for c in range(NCORES):
        m = dict(shared)
        m["x"] = np.ascontiguousarray(x[2 * c:2 * c + 2].reshape(NTOK, D))
        in_maps.append(m)
    res = run_bass_kernel_spmd(nc, in_maps, core_ids=list(range(NCORES)))
    out = np.concatenate([np.asarray(r["y"]).reshape(2, S, D) for r in res.results], axis=0)
    return out.astype(np.float32)
```

```python
import os
import numpy as np
import concourse.bass as bass
import concourse.mybir as mybir
from concourse.bass_utils import run_bass_kernel_spmd

F32 = mybir.dt.float32
BF16 = mybir.dt.bfloat16
I32 = mybir.dt.int32
AF = mybir.ActivationFunctionType
ALU = mybir.AluOpType
AX = mybir.AxisListType

NCORES = 8
D = 1024
S = 2048
NTOK = 4096
INC = 5632
ALPHA = 2.0 ** 0.25
EPS = 1e-5
NEG = -30000.0
CAP = 384
NE = 32
ENGS = ("pe", "act", "dve", "pool", "sp")

C_ID, C_M2, C_ANTI, C_LTRI, C_PAST, C_OWN, C_EOFF, C_E, C_OHG, NCST = 0, 128, 256, 384, 512, 1024, 1536, 1568, 2592, 3104


class Op:
    __slots__ = ("eng", "fn", "deps", "dma_key", "dma_val", "sig", "idx", "waits")

    def __init__(self, eng, fn):
        self.eng = eng
        self.fn = fn
        self.deps = set()
        self.dma_key = None
        self.dma_val = 0
        self.sig = 0
        self.waits = []


class Prog:
    def __init__(self, nc):
        self.nc = nc
        self.ops = []
        self.last_w = {}
        self.readers = {}
        self.dma_cnt = {}
        self.last_eng = {}
        self.last_dma = {}
        self.bg_keys = set()

    def op(self, eng, fn, r=(), w=(), dma=None, extra=()):
        o = Op(eng, fn)
        o.idx = len(self.ops)
        deps = set(extra)
        for x in r:
            lw = self.last_w.get(x)
            if lw is not None:
                deps.add(lw)
        for x in w:
            lw = self.last_w.get(x)
            if lw is not None:
                deps.add(lw)
            for rd in self.readers.get(x, ()):
                deps.add(rd)
        deps.discard(o.idx)
        o.deps = deps
        for x in r:
            self.readers.setdefault(x, []).append(o.idx)
        for x in w:
            self.last_w[x] = o.idx
            self.readers[x] = []
        if dma is not None:
            o.dma_key = dma
            self.dma_cnt[dma] = self.dma_cnt.get(dma, 0) + 1
            o.dma_val = 16 * self.dma_cnt[dma]
            self.last_dma[dma] = o.idx
        else:
            self.last_eng[eng] = o.idx
        self.ops.append(o)
        return o

    def barrier(self):
        deps = set(self.last_eng.values()) | set(v for k, v in self.last_dma.items() if k not in self.bg_keys)
        for e in ENGS:
            self.op(e, lambda eng: None, extra=deps)
        self.last_w = {}
        self.readers = {}

    def finalize(self):
        ops = self.ops

        def skip(D, o):
            return D.eng == "pe" and o.eng == "pe" and o.dma_key is None and D.dma_key is None

        needs_sig = set()
        for o in ops:
            for d in o.deps:
                Dd = ops[d]
                if Dd.dma_key is not None or skip(Dd, o):
                    continue
                needs_sig.add(d)
        cnt = {e: 0 for e in ENGS}
        for o in ops:
            if o.idx in needs_sig:
                cnt[o.eng] += 1
                o.sig = cnt[o.eng]
        waited = {e: {} for e in ENGS}
        for o in ops:
            wl = {}
            for d in o.deps:
                Dd = ops[d]
                if Dd.dma_key is not None:
                    k, v = ("dma", Dd.dma_key), Dd.dma_val
                else:
                    if skip(Dd, o):
                        continue
                    k, v = ("eng", Dd.eng), Dd.sig
                if v > wl.get(k, 0):
                    wl[k] = v
            for k, v in wl.items():
                if waited[o.eng].get(k, 0) >= v:
                    continue
                waited[o.eng][k] = v
                o.waits.append((k, v))

    def emit(self):
        nc = self.nc
        self.finalize()
        sems = {}
        for e in ENGS:
            sems[("eng", e)] = nc.alloc_semaphore(name=f"s_{e}")
        for k in self.dma_cnt:
            sems[("dma", k)] = nc.alloc_semaphore(name=f"d_{k}")
        by_eng = {e: [o for o in self.ops if o.eng == e] for e in ENGS}
        prog = self

        def run(engname, eng):
            for o in by_eng[engname]:
                for k, v in o.waits:
                    eng.wait_ge(sems[k], v)
                ins = o.fn(eng)
                if ins is None:
                    if o.sig:
                        eng.nop().then_inc(sems[("eng", engname)], 1)
                    continue
                if o.dma_key is not None:
                    ins.then_inc(sems[("dma", o.dma_key)], 16)
                elif o.sig:
                    ins.then_inc(sems[("eng", engname)], 1)
            if engname == "sp":
                for k, n in prog.dma_cnt.items():
                    eng.wait_ge(sems[("dma", k)], 16 * n)

        with nc.Block() as block:
            @block.tensor
            def _(e):
                run("pe", e)

            @block.scalar
            def _(e):
                run("act", e)

            @block.vector
            def _(e):
                run("dve", e)

            @block.gpsimd
            def _(e):
                run("pool", e)

            @block.sync
            def _(e):
                run("sp", e)


class Arena:
    def __init__(self, nc, words):
        self.t = nc.alloc_sbuf_tensor("arena", [128, words], F32)
        self.top = 0
        self.W = words
        self.peak = 0

    def alloc_at(self, off, shape, dt=F32):
        assert off >= self.top, (off, self.top)
        save = self.top
        self.top = off
        v = self.alloc(shape, dt)
        self.top = save
        return v

    def alloc(self, shape, dt=F32):
        shape = list(shape)
        n = int(np.prod(shape))
        words = n if dt in (F32, I32) else (n + 1) // 2
        words = (words + 7) // 8 * 8
        off = self.top
        self.top += words
        self.peak = max(self.peak, self.top)
        assert self.top <= self.W, f"arena overflow {self.top} > {self.W}"
        v = self.t[:, off:off + words]
        self.last_raw = v
        if dt != F32:
            v = v.bitcast(dt)
        v = v[:, 0:n]
        if len(shape) == 2:
            v = v.rearrange("p (a b) -> p a b", a=shape[0], b=shape[1])
        elif len(shape) == 3:
            v = v.rearrange("p (a b c) -> p a b c", a=shape[0], b=shape[1], c=shape[2])
        return v


def dap(t, off, pat):
    return bass.AP(tensor=t, offset=off, ap=[list(x) for x in pat])


def _t5_bucket(dist):
    dist = np.asarray(dist, dtype=np.int64)
    d = np.maximum(dist, 1).astype(np.float32)
    lp = 16 + (np.log(d / np.float32(16.0)) / np.float32(np.log(128.0 / 16.0)) * np.float32(16.0)).astype(np.int32)
    return np.where(dist < 16, dist, np.minimum(lp, 31))


def make_consts():
    c = np.zeros((128, NCST), np.float32)
    i = np.arange(128)
    c[:, C_ID:C_ID + 128] = np.eye(128)
    s_, t_ = np.meshgrid(i, i, indexing="ij")
    c[:, C_M2:C_M2 + 128] = ((s_ // 64 == t_ // 64) & (s_ <= t_)).astype(np.float32)
    c[:, C_ANTI:C_ANTI + 128] = np.eye(128)[::-1]
    c[:, C_LTRI:C_LTRI + 128] = (s_ < t_).astype(np.float32)
    past = np.zeros((8, 8, 8), np.float32)
    own = np.zeros((8, 8, 8), np.float32)
    for qb in range(8):
        for n in range(8):
            past[qb, :, n] = 0.0 if n < qb else -1e30
            own[qb, :, n] = 1.0 if n == qb else 0.0
    c[:, C_PAST:C_PAST + 512] = past.reshape(1, 512)
    c[:, C_OWN:C_OWN + 512] = own.reshape(1, 512)
    c[:, C_EOFF:C_EOFF + 32] = (np.arange(32) * CAP).astype(np.float32)[None, :]
    E = np.zeros((8, 8, 128), np.float32)
    for n in range(8):
        E[n, n, :] = 1.0
    c[0:8, C_E:C_E + 1024] = E.reshape(8, 1024)
    ohg = np.zeros((33, 2, 256), np.float32)
    for u in range(255):
        d = u - 127
        if d >= 0:
            ohg[_t5_bucket(d), 0, u] += 1.0
            ohg[31, 0, u] -= 1.0
        else:
            ohg[32, 0, u] = NEG
        dist = d + 128
        ohg[_t5_bucket(dist), 1, u] += 1.0
        ohg[31, 1, u] -= 1.0
    c[0:33, C_OHG:C_OHG + 512] = ohg.reshape(33, 512)
    return c


def build(debug=0, stage=99, maxphase=99):
    nc = bass.Bass("TRN2", target_bir_lowering=False)

    def din(name, shape, dt=F32):
        return nc.dram_tensor(name, list(shape), dt, kind="ExternalInput")

    x_d = din("x", [NTOK, D])
    win_d = din("w_in", [D, INC])
    bin_d = din("b_in", [INC])
    lbl_d = din("lb_logits", [2, 512])
    ng_d = din("hg_norm_g", [512])
    rb_d = din("rel_bias", [32, 8])
    wpa_d = din("w_proj_a", [512, D])
    wpb_d = din("w_proj_b", [512, D])
    wout_d = din("w_out", [D, D])
    ln1g_d = din("ln1_g", [D])
    ln1b_d = din("ln1_b", [D])
    wr_d = din("w_router", [D, 36])
    br_d = din("b_router", [36])
    wgu_d = din("w_gate_up", [NE, D, 1024])
    wdn_d = din("w_down", [NE, 512, D])
    ln2g_d = din("ln2_g", [D])
    ln2b_d = din("ln2_b", [D])
    cst_d = din("consts", [128, NCST])
    eind_d = din("eind", [8, S])
    y_d = nc.dram_tensor("y", [NTOK, D], F32, kind="ExternalOutput")

    def dscr(name, shape, dt):
        return nc.dram_tensor(name, list(shape), dt, kind="Internal")

    gd_d = dscr("gd", [8, 512], BF16)
    ohg_d = dscr("ohgT", [4, 128, S], BF16)
    omb_d = dscr("ombT", [8, 64, S], BF16)
    x1_d = dscr("x1s", [NTOK, D], F32)
    xbuf_d = dscr("xbuf", [NE * CAP + 128, D], BF16)
    ybuf_d = dscr("ybuf", [NE * CAP, D], F32)
    dbg = {}
    if debug:
        dbg["ohg"] = nc.dram_tensor("dbg_ohg", [4, 128, S], BF16, kind="ExternalOutput")
        dbg["omb"] = nc.dram_tensor("dbg_omb", [8, 64, S], BF16, kind="ExternalOutput")
        dbg["x1"] = nc.dram_tensor("dbg_x1", [NTOK, D], F32, kind="ExternalOutput")

    P = Prog(nc)
    A = Arena(nc, 47 * 1024)
    ps = [nc.alloc_psum_tensor(f"ps{i}", [128, 512], F32)[:, :] for i in range(8)]
    psb = [p.bitcast(BF16) for p in ps]
    PSN = [f"ps{i}" for i in range(8)]

    def dma(eng, out, in_, r=(), w=(), key=None, nonc=False):
        if nonc:
            return P.op(eng, lambda e: e.dma_start(out=out, in_=in_, allow_slow_non_contiguous=True), r=r, w=w, dma=key)
        return P.op(eng, lambda e: e.dma_start(out=out, in_=in_), r=r, w=w, dma=key)

    def mm(out, lhsT, rhs, start, stop, r, w):
        return P.op("pe", lambda e: e.matmul(out, lhsT=lhsT, rhs=rhs, start=start, stop=stop), r=r, w=w)

    def tr(out, in_, ident, r, w):
        return P.op("pe", lambda e: e.transpose(out=out, in_=in_, identity=ident), r=r, w=w)

    def act(out, in_, func, r, w, bias=None, scale=None, accum=None):
        kw = {}
        if bias is not None:
            kw["bias"] = bias
        if scale is not None:
            kw["scale"] = scale
        if accum is not None:
            kw["accum_out"] = accum
        return P.op("act", lambda e: e.activation(out=out, in_=in_, func=func, **kw), r=r, w=w)

    def tcopy(eng, out, in_, r, w):
        if eng == "act":
            return P.op(eng, lambda e: e.activation(out=out, in_=in_, func=AF.Copy), r=r, w=w)
        return P.op(eng, lambda e: e.tensor_copy(out=out, in_=in_), r=r, w=w)

    def tt(eng, out, in0, in1, op, r, w):
        return P.op(eng, lambda e: e.tensor_tensor(out=out, in0=in0, in1=in1, op=op), r=r, w=w)

    def ts(eng, out, in0, s1, s2, op0, op1, r, w):
        if op1 is None:
            return P.op(eng, lambda e: e.tensor_scalar(out=out, in0=in0, scalar1=s1, scalar2=None, op0=op0), r=r, w=w)
        return P.op(eng, lambda e: e.tensor_scalar(out=out, in0=in0, scalar1=s1, scalar2=s2, op0=op0, op1=op1), r=r, w=w)

    def stt(out, in0, scalar, in1, op0, op1, r, w):
        return P.op("dve", lambda e: e.scalar_tensor_tensor(out=out, in0=in0, scalar=scalar, in1=in1, op0=op0, op1=op1), r=r, w=w)

    def memset(eng, ap, val, w):
        return P.op(eng, lambda e: e.memset(ap, val), w=w)

    cst = A.alloc([NCST])
    dma("sp", cst, cst_d.ap(), w=["cst"], key="cst")
    identf = cst[:, C_ID:C_ID + 128]
    cb = A.alloc([4, 128], BF16)
    identb, antib, ltrib, onesb = cb[:, 0, :], cb[:, 1, :], cb[:, 2, :], cb[:, 3, :]
    tcopy("dve", identb, cst[:, C_ID:C_ID + 128], ["cst"], ["identb"])
    tcopy("dve", antib, cst[:, C_ANTI:C_ANTI + 128], ["cst"], ["antib"])
    tcopy("dve", ltrib, cst[:, C_LTRI:C_LTRI + 128], ["cst"], ["ltrib"])
    memset("dve", onesb, 1.0, ["onesb"])
    Eb = A.alloc([8, 128], BF16)
    memset("dve", Eb.rearrange("p a b -> p (a b)"), 0.0, ["Eb"])
    tcopy("dve", Eb[0:8], cst[0:8, C_E:C_E + 1024].rearrange("p (a b) -> p a b", a=8, b=128), ["cst"], ["Eb"])
    mask2x4 = A.alloc([4, 128])
    for h in range(4):
        tcopy("dve", mask2x4[:, h, :], cst[:, C_M2:C_M2 + 128], ["cst"], ["mask2x4"])
    resetm = A.alloc([512], BF16)
    memset("dve", resetm, 1.0, ["resetm"])
    memset("dve", resetm[:, 0:512:64], 0.0, ["resetm"])
    pastm = cst[:, C_PAST:C_PAST + 512].rearrange("p (q h n) -> p q h n", q=8, h=8, n=8)
    ownm = cst[:, C_OWN:C_OWN + 512].rearrange("p (q h n) -> p q h n", q=8, h=8, n=8)

    bcol = A.alloc([44])
    dma("sp", bcol, dap(bin_d, 0, [[1, 128], [128, 44]]), w=["bcol"], key="bcol", nonc=True)
    bcolq = A.alloc([4])
    ts("dve", bcolq, bcol[:, 16:20], 0.125, None, ALU.mult, None, ["bcol"], ["bcolq"])
    brow = A.alloc([1536], BF16)
    dma("pool", brow[0:1, 0:1024], dap(bin_d, 1024, [[0, 1], [1, 1024]]), w=["brow"], key="brow")
    dma("pool", brow[0:1, 1024:1536], dap(bin_d, 3072, [[0, 1], [1, 512]]), w=["brow"], key="brow")
    onesr = A.alloc([128], BF16)
    memset("dve", onesr[0:1, :], 1.0, ["onesr"])
    lbl = A.alloc([2, 4])
    dma("sp", lbl, dap(lbl_d, 0, [[1, 128], [512, 2], [128, 4]]), w=["lbl"], key="lbl", nonc=True)
    lb = A.alloc([4])
    oml = A.alloc([4])
    tt("dve", lb, lbl[:, 0, :], lbl[:, 1, :], ALU.subtract, ["lbl"], ["lb"])
    act(lb, lb, AF.Sigmoid, ["lb"], ["lb"])
    ts("dve", oml, lb, -1.0, 1.0, ALU.mult, ALU.add, ["lb"], ["oml"])
    ng_bc = A.alloc([512])
    dma("sp", ng_bc, dap(ng_d, 0, [[0, 128], [1, 512]]), w=["ng_bc"], key="ng_bc")
    rb31 = A.alloc([8])
    dma("sp", rb31, dap(rb_d, 31 * 8, [[0, 128], [1, 8]]), w=["rb31"], key="rb31")
    rbm = A.alloc([8, 8])
    tcopy("dve", rbm, rb31.unsqueeze(2).to_broadcast([128, 8, 8]), ["rb31"], ["rbm"])
    rbl = A.alloc([8])
    dma("sp", rbl[0:32, :], rb_d.ap(), w=["rbl"], key="rbl")
    memset("dve", rbl[32:33, :], 1.0, ["rbl1"])
    mm(ps[0][0:8, 0:512], rbl[0:33, 0:8], cst[0:33, C_OHG:C_OHG + 512], True, True, ["rbl", "rbl1", "cst"], ["ps0"])
    Gs = A.alloc([512], BF16)
    tcopy("dve", Gs[0:8, :], ps[0][0:8, 0:512], ["ps0"], ["Gs"])
    dma("sp", gd_d.ap(), Gs[0:8, :], r=["Gs"], w=["gd_d"], key="gd_w")
    Yc = A.alloc([16, 128], BF16)
    dma("sp", Yc, dap(gd_d, 0, [[1, 128], [256, 16], [1, 128]]), r=["gd_d"], w=["Yc"], key="Yc")
    wr = A.alloc([8, 36])
    dma("sp", wr, dap(wr_d, 0, [[36, 128], [128 * 36, 8], [1, 36]]), w=["wr"], key="wr")
    br_bc = A.alloc([36])
    dma("sp", br_bc, dap(br_d, 0, [[0, 128], [1, 36]]), w=["br_bc"], key="br_bc")
    carry = A.alloc([32])
    tcopy("dve", carry, cst[:, C_EOFF:C_EOFF + 32], ["cst"], ["carry"])
    dest_i = A.alloc([32, 2], I32)
    wts = A.alloc([32, 2])
    zrow = A.alloc([1024], BF16)
    memset("dve", A.last_raw, 0.0, ["zrow"])
    P.barrier()
    persist_top = A.top
    def start_zero_fill():
        P.bg_keys.add("zfill")
        zfill_ops = []
        nrow = NE * CAP + 128
        for r0 in range(0, nrow, 1024):
            nr = min(1024, nrow - r0)
            zfill_ops.append(dma("sp", dap(xbuf_d, r0 * D, [[D, 128], [128 * D, nr // 128], [1, D]]),
                                 zrow.unsqueeze(1).to_broadcast([128, nr // 128, 1024]), key="zfill").idx)
        return zfill_ops

    def load_w(dst, src_t, row0, col0, ncols, nk, rowstride, key, rname, kp=128):
        dma("pool", dst, dap(src_t, row0 * rowstride + col0, [[rowstride, kp], [kp * rowstride, nk], [1, ncols]]),
            w=[rname], key=key)

    for s in range(2 if stage >= 1 else 0):
        A.top = persist_top
        xT = A.alloc([8, S], BF16)
        seq_top = A.top
        xb = [A.alloc([1024], BF16) for _ in range(2)]
        wq = A.alloc_at(seq_top + 8320, [8, 512], BF16)
        wf = A.alloc_at(seq_top + 8320 + 2048, [8, 512], BF16)
        for j in range(16):
            b = j % 2
            if j == 4:
                load_w(wq, win_d, 0, 0, 512, 8, INC, "wq", "wq")
                load_w(wf, win_d, 0, 512, 512, 8, INC, "wf", "wf")
            dma("pool", xb[b], dap(x_d, (s * 16 + j) * 128 * D, [[D, 128], [1, D]]), w=[f"xb{b}"], key=f"xb{b}")
            for dk in range(8):
                tr(psb[b][:, dk * 128:(dk + 1) * 128], xb[b][:, dk * 128:(dk + 1) * 128], identb,
                   [f"xb{b}", "identb"], [PSN[b]])
            tcopy("act" if b else "dve", xT[:, :, j * 128:(j + 1) * 128],
                  psb[b].rearrange("p (a b) -> p a b", a=8, b=128), [PSN[b]], [f"xT{j}"])
        XT = lambda j0, n: [f"xT{j}" for j in range(j0, j0 + n)]
        P.barrier()
        if s == 0:
            zfill_ops = start_zero_fill()
        if maxphase <= 1:
            break
        A.top = seq_top

        qtT = A.alloc([4, S], BF16)
        ktT = A.alloc([4, S], BF16)
        dec = A.alloc([4, 32])
        h_top = A.top
        assert A.top == seq_top + 8320, A.top - seq_top
        A.top += 4096
        wi = A.alloc_at(seq_top + 19712, [8, 512], BF16)
        wg = A.alloc_at(seq_top + 19712 + 2048, [8, 512], BF16)
        load_w(wi, win_d, 0, 1024, 512, 8, INC, "wi", "wi")
        load_w(wg, win_d, 0, 1536, 512, 8, INC, "wg", "wg")
        sq2 = [A.alloc([512], BF16) for _ in range(2)]
        fb2 = [A.alloc([512]) for _ in range(2)]
        lfb2 = [A.alloc([512]) for _ in range(2)]
        bb2 = [A.alloc([512]) for _ in range(2)]
        kk2 = [A.alloc([512]) for _ in range(2)]
        eb2 = [A.alloc([512]) for _ in range(2)]
        enb2 = [A.alloc([512]) for _ in range(2)]
        its = [(h, tg) for h in range(4) for tg in range(4)]
        for r0 in range(0, 16, 2):
            rnd = []
            for u in range(2):
                h, tg = its[r0 + u]
                rnd.append((u, h, tg, slice(tg * 512, (tg + 1) * 512), 2 + u * 2, 3 + u * 2,
                            sq2[u], fb2[u], lfb2[u], bb2[u], kk2[u], eb2[u], enb2[u]))
            for (u, h, tg, tsl, bq, bf_, sq, fb, lfb, bb, kk, eb, enb) in rnd:
                for dk in range(8):
                    mm(ps[bq], wq[:, dk, h * 128:(h + 1) * 128], xT[:, dk, tsl], dk == 0, dk == 7,
                       ["wq"] + XT(tg * 4, 4), [PSN[bq]])
                for dk in range(8):
                    mm(ps[bf_], wf[:, dk, h * 128:(h + 1) * 128], xT[:, dk, tsl], dk == 0, dk == 7,
                       ["wf"] + XT(tg * 4, 4), [PSN[bf_]])
            for (u, h, tg, tsl, bq, bf_, sq, fb, lfb, bb, kk, eb, enb) in rnd:
                act(sq, ps[bq], AF.Silu, [PSN[bq], "bcol"], [f"sq{u}"], bias=bcol[:, h:h + 1])
                act(fb, ps[bf_], AF.Sigmoid, [PSN[bf_], "bcol"], [f"fb{u}"], bias=bcol[:, 4 + h:5 + h])
                ts("dve", fb, fb, oml[:, h:h + 1], lb[:, h:h + 1], ALU.mult, ALU.add, [f"fb{u}", "oml", "lb"], [f"fb{u}"])
            for (u, h, tg, tsl, bq, bf_, sq, fb, lfb, bb, kk, eb, enb) in rnd:
                act(lfb, fb, AF.Ln, [f"fb{u}"], [f"lfb{u}"])
                P.op("dve", lambda e, bb=bb, lfb=lfb: e.tensor_tensor_scan(out=bb, data0=resetm, data1=lfb, initial=0.0,
                                                                         op0=ALU.mult, op1=ALU.add),
                     r=["resetm", f"lfb{u}"], w=[f"bb{u}"])
                ts("pool", kk, fb, -1.0, 1.0, ALU.mult, ALU.add, [f"fb{u}"], [f"kk{u}"])
            for (u, h, tg, tsl, bq, bf_, sq, fb, lfb, bb, kk, eb, enb) in rnd:
                act(eb, bb, AF.Exp, [f"bb{u}"], [f"eb{u}"])
                act(enb, bb, AF.Exp, [f"bb{u}"], [f"enb{u}"], scale=-1.0)
                tt("dve", qtT[:, h, tsl], sq, eb, ALU.mult, [f"sq{u}", f"eb{u}"], [f"qt{h}"])
                tt("pool", ktT[:, h, tsl], kk, enb, ALU.mult, [f"kk{u}", f"enb{u}"], [f"kt{h}"])
                tcopy("dve", dec[:, h, tg * 8:(tg + 1) * 8], eb[:, 63:512:64], [f"eb{u}"], ["dec"])
        P.barrier()
        if maxphase <= 2:
            break
        A.top = h_top

        wmq = A.alloc_at(seq_top + 24576, [8, 512], BF16)
        wmk = A.alloc_at(seq_top + 24576 + 2048, [8, 512], BF16)
        wmv = A.alloc_at(seq_top + 24576 + 4096, [8, 512], BF16)
        load_w(wmq, win_d, 0, 2048, 512, 8, INC, "wmq", "wmq")
        load_w(wmk, win_d, 0, 2560, 512, 8, INC, "wmk", "wmk")
        load_w(wmv, win_d, 0, 3072, 512, 8, INC, "wmv", "wmv")
        St = A.alloc([4, 128])
        Sb = A.alloc([4, 128], BF16)
        Sbb = A.alloc([4, 128], BF16)
        memset("dve", St, 0.0, ["St"])
        memset("dve", Sb, 0.0, ["Sb"])
        vt = [A.alloc([512], BF16) for _ in range(2)]
        gt = [A.alloc([512], BF16) for _ in range(2)]
        gs = A.alloc([512])
        ktk = [A.alloc([4, 128], BF16) for _ in range(2)]
        scm = A.alloc([4, 128], BF16)
        Tb = A.alloc([4, 128])
        sqo = A.alloc([512])
        ssq = A.alloc([4])
        rs = A.alloc([4])
        otmp = A.alloc([4, 128])
        otok = A.alloc([4, 128], BF16)
        ohs = [A.alloc([4, 512], BF16) for _ in range(2)]
        def f_v(j):
            b = j % 2
            tl = slice(j * 128, (j + 1) * 128)
            mm(ps[0], onesr[0:1, :], brow[0:1, 0:512], True, False, ["onesr", "brow"], ["ps0"])
            for dk in range(8):
                mm(ps[0], xT[:, dk, tl], wi[:, dk, :], False, dk == 7, [f"xT{j}", "wi"], ["ps0"])
            tcopy("act", vt[b], ps[0], ["ps0"], [f"vt{b}"])

        def f_g(j):
            b = j % 2
            tl = slice(j * 128, (j + 1) * 128)
            mm(ps[1], onesr[0:1, :], brow[0:1, 512:1024], True, False, ["onesr", "brow"], ["ps1"])
            for dk in range(8):
                mm(ps[1], xT[:, dk, tl], wg[:, dk, :], False, dk == 7, [f"xT{j}", "wg"], ["ps1"])
            act(gs, ps[1], AF.Silu, ["ps1"], ["gs"])
            tt("pool", gt[b], gs, ng_bc, ALU.mult, ["gs", "ng_bc"], [f"gt{b}"])

        def f_k(j):
            b = j % 2
            tl = slice(j * 128, (j + 1) * 128)
            for h in range(4):
                tr(psb[6][:, h * 128:(h + 1) * 128], ktT[:, h, tl], identb, [f"kt{h}", "identb"], ["ps6"])
            tcopy("act", ktk[b], psb[6][:, 0:512].rearrange("p (a b) -> p a b", a=4, b=128), ["ps6"], [f"ktk{b}"])

        def b_a(j):
            b = j % 2
            tl = slice(j * 128, (j + 1) * 128)
            for h in range(4):
                mm(ps[2][:, h * 128:(h + 1) * 128], ktT[:, h, tl], qtT[:, h, tl], True, True,
                   [f"kt{h}", f"qt{h}"], ["ps2"])
            tt("dve", scm, ps[2].rearrange("p (a b) -> p a b", a=4, b=128), mask2x4, ALU.mult,
               ["ps2", "mask2x4"], ["scm"])
            for h in range(4):
                hs = slice(h * 128, (h + 1) * 128)
                mm(ps[4][:, hs], ktk[b][0:64, h, :], vt[b][0:64, hs], True, True, [f"ktk{b}", f"vt{b}"], ["ps4"])
            for h in range(4):
                hs = slice(h * 128, (h + 1) * 128)
                mm(ps[5][:, hs], ktk[b][64:128, h, :], vt[b][64:128, hs], True, True, [f"ktk{b}", f"vt{b}"], ["ps5"])
            dec0 = dec[:, :, 2 * j:2 * j + 1].to_broadcast([128, 4, 128])
            tt("dve", Tb, ps[4].rearrange("p (a b) -> p a b", a=4, b=128), St, ALU.add, ["ps4", "St"], ["Tb"])
            tt("dve", Sbb, Tb, dec0, ALU.mult, ["Tb", "dec"], ["Sbb"])
            tt("pool", St, Tb, dec0, ALU.mult, ["Tb", "dec"], ["St"])

        def b_b(j):
            b = j % 2
            t0 = j * 128
            dec1 = dec[:, :, 2 * j + 1:2 * j + 2].to_broadcast([128, 4, 128])
            for h in range(4):
                hs = slice(h * 128, (h + 1) * 128)
                mm(ps[3][:, hs], scm[:, h, :], vt[b][:, hs], True, False, ["scm", f"vt{b}"], ["ps3"])
                mm(ps[3][0:64, hs], qtT[:, h, t0:t0 + 64], Sb[:, h, :], False, True, [f"qt{h}", "Sb"], ["ps3"])
                mm(ps[3][64:128, hs], qtT[:, h, t0 + 64:t0 + 128], Sbb[:, h, :], False, True, [f"qt{h}", "Sbb"], ["ps3"])
            tt("dve", Tb, ps[5].rearrange("p (a b) -> p a b", a=4, b=128), St, ALU.add, ["ps5", "St"], ["Tb"])
            tt("dve", Sb, Tb, dec1, ALU.mult, ["Tb", "dec"], ["Sb"])
            tt("pool", St, Tb, dec1, ALU.mult, ["Tb", "dec"], ["St"])

        def b_c(j):
            b = j % 2
            act(sqo, ps[3], AF.Square, ["ps3"], ["sqo"])
            P.op("dve", lambda e: e.tensor_reduce(out=ssq, in_=sqo.rearrange("p (a b) -> p a b", a=4, b=128),
                                                  axis=AX.X, op=ALU.add), r=["sqo"], w=["ssq"])
            ts("dve", ssq, ssq, 1.0 / 128.0, EPS, ALU.mult, ALU.add, ["ssq"], ["ssq"])
            act(rs, ssq, AF.Ln, ["ssq"], ["rs"])
            act(rs, rs, AF.Exp, ["rs"], ["rs"], scale=-0.5)
            tt("dve", otmp, ps[3].rearrange("p (a b) -> p a b", a=4, b=128),
               rs.unsqueeze(2).to_broadcast([128, 4, 128]), ALU.mult, ["ps3", "rs"], ["otmp"])
            tt("pool", otok, otmp, gt[b].rearrange("p (a b) -> p a b", a=4, b=128), ALU.mult,
               ["otmp", f"gt{b}"], ["otok"])

        def b_d(j):
            for h in range(4):
                tr(psb[7][:, h * 128:(h + 1) * 128], otok[:, h, :], identb, ["otok", "identb"], ["ps7"])
            g4 = (j // 4) % 2
            tcopy("act", ohs[g4][:, :, (j % 4) * 128:(j % 4 + 1) * 128],
                  psb[7][:, 0:512].rearrange("p (a b) -> p a b", a=4, b=128), ["ps7"], [f"ohs{g4}"])
            if j % 4 == 3:
                tg = j // 4
                dma("sp", dap(ohg_d, tg * 512, [[S, 128], [128 * S, 4], [1, 512]]), ohs[g4],
                    r=[f"ohs{g4}"], key=f"ohs{g4}")
                if debug and s == 0:
                    dma("sp", dap(dbg["ohg"], tg * 512, [[S, 128], [128 * S, 4], [1, 512]]), ohs[g4],
                        r=[f"ohs{g4}"], key=f"dbgohs{g4}")

        f_v(0)
        f_g(0)
        f_k(0)
        for j in range(16):
            nx = j + 1 < 16
            b_a(j)
            if nx:
                f_v(j + 1)
                f_k(j + 1)
            if j >= 1:
                b_d(j - 1)
            b_b(j)
            if nx:
                f_g(j + 1)
            b_c(j)
        b_d(15)
        P.barrier()
        if maxphase <= 3:
            break
        A.top = seq_top

        mqE = A.alloc([4, S], BF16)
        memset("dve", A.last_raw[64:128], 0.0, ["mqz"])
        mqO = A.alloc([4, S], BF16)
        memset("dve", A.last_raw[0:64], 0.0, ["mqz"])
        mkE = A.alloc([4, S], BF16)
        memset("dve", A.last_raw[64:128], 0.0, ["mkz"])
        mkO = A.alloc([4, S], BF16)
        memset("dve", A.last_raw[0:64], 0.0, ["mkz"])
        for p in range(4):
            dma("pool", mkE[64:72, p, :], eind_d.ap(), r=["mkz"], w=[f"mkiE{p}"], key="mkzE")
            dma("pool", mkO[0:8, p, :], eind_d.ap(), r=["mkz"], w=[f"mkiO{p}"], key="mkzO")
        vaug = A.alloc([16, 8, 65], BF16)
        kmE = A.alloc([4, 8], BF16)
        kmO = A.alloc([4, 8], BF16)
        memset("dve", kmE.rearrange("p a b -> p (a b)"), 0.0, ["kmT"])
        memset("dve", kmO.rearrange("p a b -> p (a b)"), 0.0, ["kmT"])
        km32 = A.alloc([4, 8])
        m_top = A.top
        assert A.top <= seq_top + 24576, A.top - seq_top
        memset("dve", vaug.rearrange("p a b c -> p (a b c)"), 1.0, ["vaug"])
        it = 0
        for p in range(4):
            for tg in range(4):
                bq, bk = (it % 2) * 2, (it % 2) * 2 + 1
                it += 1
                tsl = slice(tg * 512, (tg + 1) * 512)
                for dk in range(8):
                    mm(ps[bq], wmq[:, dk, p * 128:(p + 1) * 128], xT[:, dk, tsl], dk == 0, dk == 7,
                       ["wmq"] + XT(tg * 4, 4), [PSN[bq]])
                for dk in range(8):
                    mm(ps[bk], wmk[:, dk, p * 128:(p + 1) * 128], xT[:, dk, tsl], dk == 0, dk == 7,
                       ["wmk"] + XT(tg * 4, 4), [PSN[bk]])
                act(mqE[0:64, p, tsl], ps[bq][0:64], AF.Identity, [PSN[bq], "bcolq", "mqz"], [f"mq{p}"], bias=bcolq[0:64, p:p + 1], scale=0.125)
                act(mqO[64:128, p, tsl], ps[bq][64:128], AF.Identity, [PSN[bq], "bcolq", "mqz"], [f"mq{p}"], bias=bcolq[64:128, p:p + 1], scale=0.125)
                ts("dve", mkE[0:64, p, tsl], ps[bk][0:64], bcol[0:64, 20 + p:21 + p], None, ALU.add, None, [PSN[bk], "bcol", "mkz"], [f"mk{p}"])
                ts("dve", mkO[64:128, p, tsl], ps[bk][64:128], bcol[64:128, 20 + p:21 + p], None, ALU.add, None, [PSN[bk], "bcol", "mkz"], [f"mk{p}"])
            P.op("dve", lambda e, p=p: e.tensor_reduce(out=km32[0:64, p, :], in_=mkE[0:64, p, :].rearrange("p (a b) -> p a b", a=8, b=256),
                                                       axis=AX.X, op=ALU.add), r=[f"mk{p}"], w=["km32"])
            P.op("dve", lambda e, p=p: e.tensor_reduce(out=km32[64:128, p, :], in_=mkO[64:128, p, :].rearrange("p (a b) -> p a b", a=8, b=256),
                                                       axis=AX.X, op=ALU.add), r=[f"mk{p}"], w=["km32"])
        ts("dve", kmE[0:64], km32[0:64], 1.0 / 256.0, None, ALU.mult, None, ["km32", "kmT"], ["kmT"])
        ts("dve", kmO[64:128], km32[64:128], 1.0 / 256.0, None, ALU.mult, None, ["km32", "kmT"], ["kmT"])
        gm = [A.alloc([8, 8]) for _ in range(2)]
        top8 = [A.alloc([8, 8]) for _ in range(2)]
        thr = [A.alloc([8]) for _ in range(2)]
        sel = [A.alloc([8, 8]) for _ in range(2)]
        Mtok = [A.alloc([8, 8], BF16) for _ in range(2)]
        PT = [A.alloc([2, 256], BF16) for _ in range(3)]
        osb = A.alloc([256])
        rden = A.alloc([256])
        onesf = A.alloc([64])
        memset("dve", onesf[64:65, :], 1.0, ["onesf"])
        oms = [A.alloc([8, 256], BF16) for _ in range(2)]

        def gate_front(qb, jls=(0, 1)):
            for jl in jls:
                jt = 2 * qb + jl
                tl = slice(jt * 128, (jt + 1) * 128)
                for h in range(8):
                    p_ = h // 2
                    mm(ps[6][:, jl * 64 + h * 8:jl * 64 + (h + 1) * 8], (mqO if h % 2 else mqE)[:, p_, tl], (kmO if h % 2 else kmE)[:, p_, :],
                       True, True, [f"mq{p_}", "kmT"], ["ps6"])
                tt("dve", gm[jl], ps[6][:, jl * 64:(jl + 1) * 64].rearrange("p (a b) -> p a b", a=8, b=8), pastm[:, qb], ALU.add,
                   ["ps6", "cst"], [f"gm{jl}"])
                for h in range(8):
                    P.op("dve", lambda e, h=h, jl=jl: e.max(out=top8[jl][:, h, :], in_=gm[jl][:, h, :]), r=[f"gm{jl}"], w=[f"top8{jl}"])
                ts("dve", thr[jl], top8[jl][:, :, 2], -1e29, None, ALU.max, None, [f"top8{jl}"], [f"thr{jl}"])
                tt("dve", sel[jl], gm[jl], thr[jl].unsqueeze(2).to_broadcast([128, 8, 8]), ALU.is_ge, [f"gm{jl}", f"thr{jl}"], [f"sel{jl}"])
                tt("dve", sel[jl], sel[jl], ownm[:, qb], ALU.max, [f"sel{jl}", "cst"], [f"sel{jl}"])
                ts("dve", sel[jl], sel[jl], -NEG, NEG, ALU.mult, ALU.add, [f"sel{jl}"], [f"sel{jl}"])
                tt("dve", Mtok[jl], sel[jl], rbm, ALU.add, [f"sel{jl}", "rbm"], [f"Mtok{jl}"])

        def gate_back(qb):
            for jl in range(2):
                qc = slice(qb * 256 + jl * 128, qb * 256 + (jl + 1) * 128)
                for h in range(8):
                    r0 = 0 if h % 2 else 64
                    tr(psb[7][r0:r0 + 8, h * 128:(h + 1) * 128], Mtok[jl][:, h, :], identb, [f"Mtok{jl}", "identb"], ["ps7"])
                p7 = psb[7].rearrange("p (a b c) -> p a b c", a=4, b=2, c=128)
                act(mqE[64:72, :, qc], p7[64:72, :, 0, :], AF.Copy, ["ps7"], [f"mq{p}" for p in range(4)])
                act(mqO[0:8, :, qc], p7[0:8, :, 1, :], AF.Copy, ["ps7"], [f"mq{p}" for p in range(4)])

        sc_state = {"i": 0}
        MKI = [f"mkiE{p}" for p in range(4)] + [f"mkiO{p}" for p in range(4)]

        def scores(qb, h, n):
            mb = qb % 2
            p_ = h // 2
            sb_ = sc_state["i"] % 3
            sc_state["i"] += 1
            psc = ps[sb_]
            qsel = mqO if h % 2 else mqE
            q_all = qsel[:, p_, qb * 256:(qb + 1) * 256]
            q_hi = qsel[:, p_, qb * 256 + 128:(qb + 1) * 256]
            own = (n == qb)
            for jj in range(2):
                kt_ = 2 * n + jj
                ktile = (mkO if h % 2 else mkE)[:, p_, kt_ * 128:(kt_ + 1) * 128]
                if own and jj == 1:
                    osl = slice(jj * 256 + 128, jj * 256 + 256)
                    mm(psc[:, osl], ktile, q_hi, True, False, [f"mk{p_}", f"mq{p_}"] + MKI, [PSN[sb_]])
                    mm(psc[:, osl], antib, Yc[:, h * 2 + 0, :], False, True, ["antib", "Yc"], [PSN[sb_]])
                else:
                    osl = slice(jj * 256, jj * 256 + 256)
                    corr = own or (n == qb - 1 and jj == 1)
                    mm(psc[:, osl], ktile, q_all, True, not corr, [f"mk{p_}", f"mq{p_}"] + MKI, [PSN[sb_]])
                    if own:
                        mm(psc[:, jj * 256:jj * 256 + 128], antib, Yc[:, h * 2 + 0, :], False, False, ["antib", "Yc"], [PSN[sb_]])
                        mm(psc[:, jj * 256 + 128:jj * 256 + 256], antib, Yc[:, h * 2 + 1, :], False, True, ["antib", "Yc"], [PSN[sb_]])
                    elif corr:
                        mm(psc[:, jj * 256:jj * 256 + 128], antib, Yc[:, h * 2 + 1, :], False, True, ["antib", "Yc"], [PSN[sb_]])
            return sb_

        def exp_pv(qb, h, n, sb_):
            po = 4 + h % 2
            psc = ps[sb_]
            pt = PT[sb_]
            first = (n == 0)
            if n == qb:
                act(pt[:, 0, :], psc[:, 0:256], AF.Exp, [PSN[sb_]], [f"PT{sb_}"])
                act(pt[:, 1, 128:256], psc[:, 384:512], AF.Exp, [PSN[sb_]], [f"PT{sb_}"])
                mm(ps[po][0:65, 0:256], vaug[:, 2 * n, h, :], pt[:, 0, :], first, False, [f"va{2 * n}", f"PT{sb_}"], [PSN[po]])
                mm(ps[po][0:65, 128:256], vaug[:, 2 * n + 1, h, :], pt[:, 1, 128:256], False, True,
                   [f"va{2 * n + 1}", f"PT{sb_}"], [PSN[po]])
            else:
                act(pt.rearrange("p a b -> p (a b)"), psc, AF.Exp, [PSN[sb_]], [f"PT{sb_}"])
                mm(ps[po][0:65, 0:256], vaug[:, 2 * n, h, :], pt[:, 0, :], first, False, [f"va{2 * n}", f"PT{sb_}"], [PSN[po]])
                mm(ps[po][0:65, 0:256], vaug[:, 2 * n + 1, h, :], pt[:, 1, :], False, False,
                   [f"va{2 * n + 1}", f"PT{sb_}"], [PSN[po]])

        def norm1(h):
            po = 4 + h % 2
            tcopy("act", osb[0:64, :], ps[po][0:64, 0:256], [PSN[po]], ["osb"])
            P.op("dve", lambda e, po=po: e.reciprocal(out=rden[64:65, :], in_=ps[po][64:65, 0:256]), r=[PSN[po]], w=["rden"])

        def norm2(qb, h):
            mb = qb % 2
            mm(ps[3][0:64, 0:256], onesf[64:65, 0:64], rden[64:65, :], True, True, ["onesf", "rden"], ["ps3"])
            tt("dve", oms[mb][0:64, h, :], osb[0:64, :], ps[3][0:64, 0:256], ALU.mult, ["osb", "ps3"], [f"oms{mb}"])

        gate_front(0)
        gate_back(0)
        for j in range(16):
            b = 4 + j % 2
            tl = slice(j * 128, (j + 1) * 128)
            mm(ps[b], onesr[0:1, :], brow[0:1, 1024:1536], True, False, ["onesr", "brow"], [PSN[b]])
            for dk in range(8):
                mm(ps[b], xT[:, dk, tl], wmv[:, dk, :], False, dk == 7, [f"xT{j}", "wmv"], [PSN[b]])
            tcopy("act" if j % 2 else "dve", vaug[:, j, :, 0:64], ps[b].rearrange("p (a b) -> p a b", a=8, b=64),
                  [PSN[b]], [f"va{j}", "vaug"])
        assert A.top <= seq_top + 24576, A.top - seq_top
        for qb in range(8):
            mb = qb % 2
            blocks = [(h, n) for h in range(8) for n in range(qb + 1)]
            pend = None
            sbs = {0: scores(qb, *blocks[0]), 1: scores(qb, *blocks[1])}
            for i, (h, n) in enumerate(blocks):
                if i + 2 < len(blocks):
                    sbs[i + 2] = scores(qb, *blocks[i + 2])
                exp_pv(qb, h, n, sbs[i])
                if pend is not None:
                    norm2(qb, pend)
                    pend = None
                if n == qb:
                    norm1(h)
                    pend = h
                    if qb + 1 < 8 and h in (1, 4):
                        gate_front(qb + 1, (0,) if h == 1 else (1,))
            norm2(qb, pend)
            if qb + 1 < 8:
                gate_back(qb + 1)
            dma("sp", dap(omb_d, qb * 256, [[S, 64], [64 * S, 8], [1, 256]]), oms[mb][0:64],
                r=[f"oms{mb}"], key=f"oms{mb}")
            if debug and s == 0:
                dma("sp", dap(dbg["omb"], qb * 256, [[S, 64], [64 * S, 8], [1, 256]]), oms[mb][0:64],
                    r=[f"oms{mb}"], key=f"dbgoms{mb}")
        P.barrier()
        if maxphase <= 4:
            break
        A.top = seq_top

        mT = A.alloc([8, S], BF16)
        xa_top = A.top
        wga = A.alloc([8, 1024], BF16)
        wgb = A.alloc([8, 1024], BF16)
        wpa = A.alloc([4, 1024], BF16)
        wpb = A.alloc([4, 1024], BF16)
        load_w(wga, win_d, 0, 3584, 1024, 8, INC, "wA", "wga")
        load_w(wgb, win_d, 0, 4608, 1024, 8, INC, "wB", "wgb")
        load_w(wpa, wpa_d, 0, 0, 1024, 4, D, "wC", "wpa")
        load_w(wpb, wpb_d, 0, 0, 1024, 4, D, "wD", "wpb")
        ohg_g = [A.alloc([4, 512], BF16) for _ in range(2)]
        omb_g = [A.alloc([4, 512], BF16) for _ in range(2)]
        sga = A.alloc([512])
        sgb = A.alloc([512])
        t1 = A.alloc([512])
        t2 = A.alloc([512])
        it = 0
        for tg in range(4):
            gb_ = tg % 2
            tsl = slice(tg * 512, (tg + 1) * 512)
            dma("sp", ohg_g[gb_], dap(ohg_d, tg * 512, [[S, 128], [128 * S, 4], [1, 512]]), w=[f"ohg_g{gb_}"], key=f"ohg_g{gb_}")
            dma("sp", omb_g[gb_], dap(omb_d, tg * 512, [[S, 128], [128 * S, 4], [1, 512]]), w=[f"omb_g{gb_}"], key=f"omb_g{gb_}")
            for c in range(8):
                o4 = (it % 2) * 4
                it += 1
                csl = slice(c * 128, (c + 1) * 128)
                for dk in range(8):
                    mm(ps[o4], wga[:, dk, csl], xT[:, dk, tsl], dk == 0, dk == 7, ["wga"] + XT(tg * 4, 4), [PSN[o4]])
                for dk in range(8):
                    mm(ps[o4 + 1], wgb[:, dk, csl], xT[:, dk, tsl], dk == 0, dk == 7, ["wgb"] + XT(tg * 4, 4), [PSN[o4 + 1]])
                for h in range(4):
                    mm(ps[o4 + 2], wpa[:, h, csl], ohg_g[gb_][:, h, :], h == 0, h == 3, ["wpa", f"ohg_g{gb_}"], [PSN[o4 + 2]])
                for h in range(4):
                    mm(ps[o4 + 3], wpb[:, h, csl], omb_g[gb_][:, h, :], h == 0, h == 3, ["wpb", f"omb_g{gb_}"], [PSN[o4 + 3]])
                act(sga, ps[o4], AF.Sigmoid, [PSN[o4], "bcol"], ["sga"], bias=bcol[:, 28 + c:29 + c])
                act(sgb, ps[o4 + 1], AF.Sigmoid, [PSN[o4 + 1], "bcol"], ["sgb"], bias=bcol[:, 36 + c:37 + c])
                tt("dve", t1, sga, ps[o4 + 2], ALU.mult, ["sga", PSN[o4 + 2]], ["t1"])
                tt("dve", t2, sgb, ps[o4 + 3], ALU.mult, ["sgb", PSN[o4 + 3]], ["t2"])
                tt("pool", mT[:, c, tsl], t1, t2, ALU.add, ["t1", "t2"], [f"mT{tg}"])
        P.barrier()
        if maxphase <= 5:
            break
        A.top = xa_top

        wo = A.alloc([8, 1024], BF16)
        load_w(wo, wout_d, 0, 0, 1024, 8, D, "wA", "wo")
        if s == 1:
            wgu0 = A.alloc_at(A.W - 6144, [8, 1024], BF16)
            wdn0 = A.alloc_at(A.W - 2048, [4, 1024], BF16)
            load_w(wgu0, wgu_d, 0, 0, 1024, 8, 1024, "wgu0", "wgu0")
            load_w(wdn0, wdn_d, 0, 0, 1024, 4, D, "wdn0", "wdn0")
        l1g = A.alloc([1024])
        l1b = A.alloc([1024])
        dma("sp", l1g, dap(ln1g_d, 0, [[0, 128], [1, D]]), w=["l1g"], key="l1g")
        dma("sp", l1b, dap(ln1b_d, 0, [[0, 128], [1, D]]), w=["l1b"], key="l1b")
        xin = [A.alloc([1024]) for _ in range(2)]
        rr = A.alloc([1024])
        st = A.alloc([12])
        mv = A.alloc([2])
        rsd = A.alloc([1])
        nmr = A.alloc([1])
        x1 = [A.alloc([1024]) for _ in range(3)]
        x1b = [A.alloc([1024], BF16) for _ in range(4)]
        x1T_ = [A.alloc([8, 128]) for _ in range(2)]
        lg = A.alloc([36])
        sm = A.alloc([16])
        ohg4 = A.alloc([4])
        pen = A.alloc([4])
        em = A.alloc([4, 8])
        t8 = A.alloc([8])
        oh1_ = [A.alloc([32]) for _ in range(2)]
        oh2_ = [A.alloc([32]) for _ in range(2)]
        cnt_ = [A.alloc([32], BF16) for _ in range(2)]
        slot = A.alloc([32])
        tmp32 = A.alloc([32])
        dfl = A.alloc([2])
        def xb_A_mm(j):
            b = j % 2
            tile = s * 16 + j
            tl = slice(j * 128, (j + 1) * 128)
            dma("sp", xin[b], dap(x_d, tile * 128 * D, [[D, 128], [1, D]]), w=[f"xin{b}"], key=f"xin{b}")
            for half in range(2):
                for dk in range(8):
                    mm(ps[half], mT[:, dk, tl], wo[:, dk, half * 512:(half + 1) * 512], dk == 0, dk == 7,
                       [f"mT{j // 4}", "wo"], [PSN[half]])

        def xb_A_rest(j):
            b = j % 2
            tile = s * 16 + j
            for half in range(2):
                hsl = slice(half * 512, (half + 1) * 512)
                stt(rr[:, hsl], xin[b][:, hsl], ALPHA, ps[half], ALU.mult, ALU.add, [f"xin{b}", PSN[half]], ["rr"])
            P.op("dve", lambda e: e.bn_stats(out=st[:, 0:6], in_=rr[:, 0:512]), r=["rr"], w=["st"])
            P.op("dve", lambda e: e.bn_stats(out=st[:, 6:12], in_=rr[:, 512:1024]), r=["rr"], w=["st"])
            P.op("dve", lambda e: e.bn_aggr(out=mv, in_=st), r=["st"], w=["mv"])
            ts("dve", rsd, mv[:, 1:2], EPS, None, ALU.add, None, ["mv"], ["rsd"])
            act(rsd, rsd, AF.Ln, ["rsd"], ["rsd"])
            act(rsd, rsd, AF.Exp, ["rsd"], ["rsd"], scale=-0.5)
            stt(nmr, mv[:, 0:1], -1.0, rsd[:, 0:1], ALU.mult, ALU.mult, ["mv", "rsd"], ["nmr"])
            act(rr, rr, AF.Identity, ["rr", "rsd", "nmr"], ["rr"], bias=nmr[:, 0:1], scale=rsd[:, 0:1])
            tt("pool", rr, rr, l1g, ALU.mult, ["rr", "l1g"], ["rr"])
            tt("pool", x1[j % 3], rr, l1b, ALU.add, ["rr", "l1b"], [f"x1{j % 3}"])
            dma("sp", dap(x1_d, tile * 128 * D, [[D, 128], [1, D]]), x1[j % 3], r=[f"x1{j % 3}"], key=f"x1o{j % 3}")
            if debug:
                dma("sp", dap(dbg["x1"], tile * 128 * D, [[D, 128], [1, D]]), x1[j % 3], r=[f"x1{j % 3}"], key=f"dbgx1{j % 3}")
            tcopy("act", x1b[j % 4], x1[j % 3], [f"x1{j % 3}"], [f"x1b{j % 4}"])

        def xb_B0(j):
            b = j % 2
            x1T = x1T_[b]
            for dk in range(8):
                tr(ps[2 + dk // 4][:, (dk % 4) * 128:(dk % 4 + 1) * 128], x1[j % 3][:, dk * 128:(dk + 1) * 128], identf,
                   [f"x1{j % 3}", "cst"], [PSN[2 + dk // 4]])
            tcopy("act", x1T[:, 0:4, :], ps[2].rearrange("p (a b) -> p a b", a=4, b=128), ["ps2"], [f"x1T{b}"])
            tcopy("act", x1T[:, 4:8, :], ps[3].rearrange("p (a b) -> p a b", a=4, b=128), ["ps3"], [f"x1T{b}"])

        def xb_B1(j):
            b = j % 2
            tile = s * 16 + j
            x1T = x1T_[b]
            oh1, oh2, cnt = oh1_[b], oh2_[b], cnt_[b]
            for dk in range(8):
                mm(ps[4][:, 0:36], x1T[:, dk, :], wr[:, dk, :], dk == 0, dk == 7, [f"x1T{b}", "wr"], ["ps4"])
            tt("dve", lg, ps[4][:, 0:36], br_bc, ALU.add, ["ps4", "br_bc"], ["lg"])
            gmax, ngmax, sg_, pg_, v21, e21, den_, w1_, w2_ = (sm[:, i:i + 1] for i in range(9))
            P.op("dve", lambda e, gmax=gmax: e.tensor_reduce(out=gmax, in_=lg[:, 0:4], axis=AX.X, op=ALU.max), r=["lg"], w=["sm0"])
            ts("dve", ohg4, lg[:, 0:4], gmax, None, ALU.is_equal, None, ["lg", "sm0"], ["ohg4"])
            ts("dve", ngmax, gmax, -1.0, None, ALU.mult, None, ["sm0"], ["sm1"])
            act(pen, lg[:, 0:4], AF.Exp, ["lg", "sm1"], ["pen", "sm2"], bias=ngmax, accum=sg_)
            P.op("dve", lambda e, pg_=pg_, sg_=sg_: e.reciprocal(out=pg_, in_=sg_), r=["sm2"], w=["sm3"])
            ts("dve", pen, ohg4, 1e30, -1e30, ALU.mult, ALU.add, ["ohg4", "pen"], ["pen"])
            tt("dve", em, lg[:, 4:36].rearrange("p (a b) -> p a b", a=4, b=8), pen.unsqueeze(2).to_broadcast([128, 4, 8]),
               ALU.add, ["lg", "pen"], ["em"])
            emf = em.rearrange("p a b -> p (a b)")
            P.op("dve", lambda e, emf=emf: e.max(out=t8, in_=emf), r=["em"], w=["t8"])
            ts("dve", oh1, emf, t8[:, 0:1], None, ALU.is_equal, None, ["em", "t8"], [f"oh1{b}"])
            ts("dve", oh2, emf, t8[:, 1:2], None, ALU.is_equal, None, ["em", "t8"], [f"oh2{b}"])
            tt("dve", v21, t8[:, 1:2], t8[:, 0:1], ALU.subtract, ["t8"], ["sm4"])
            act(e21, v21, AF.Exp, ["sm4"], ["sm5"])
            ts("dve", den_, e21, 1.0, None, ALU.add, None, ["sm5"], ["sm6"])
            P.op("dve", lambda e, w1_=w1_, den_=den_: e.reciprocal(out=w1_, in_=den_), r=["sm6"], w=["sm7"])
            tt("dve", w2_, e21, w1_, ALU.mult, ["sm5", "sm7"], ["sm8"])
            tt("dve", wts[:, tile, 0:1], w1_, pg_, ALU.mult, ["sm7", "sm3"], [f"wts{tile}a"])
            tt("dve", wts[:, tile, 1:2], w2_, pg_, ALU.mult, ["sm8", "sm3"], [f"wts{tile}b"])
            tt("dve", cnt, oh1, oh2, ALU.add, [f"oh1{b}", f"oh2{b}"], [f"cnt{b}"])

        def xb_B2(j):
            b = j % 2
            tile = s * 16 + j
            oh1, oh2, cnt = oh1_[b], oh2_[b], cnt_[b]
            mm(ps[5][:, 0:32], ltrib, cnt, True, True, ["ltrib", f"cnt{b}"], ["ps5"])
            mm(ps[5][:, 32:64], onesb, cnt, True, True, ["onesb", f"cnt{b}"], ["ps5"])
            tt("dve", slot, ps[5][:, 0:32], carry, ALU.add, ["ps5", "carry"], ["slot"])
            tt("dve", carry, ps[5][:, 32:64], carry, ALU.add, ["ps5", "carry"], ["carry"])
            for k, oh in ((0, oh1), (1, oh2)):
                tt("dve", tmp32, oh, slot, ALU.mult, [f"oh{k + 1}{b}", "slot"], ["tmp32"])
                P.op("dve", lambda e, k=k: e.tensor_reduce(out=dfl[:, k:k + 1], in_=tmp32, axis=AX.X, op=ALU.add),
                     r=["tmp32"], w=["dfl"])
            tcopy("dve", dest_i[:, tile, :], dfl, ["dfl"], [f"dest{tile}"])
            for k in range(2):
                P.op("pool", lambda e, k=k, tile=tile, b=b: e.indirect_dma_start(
                    out=xbuf_d.ap(), out_offset=bass.IndirectOffsetOnAxis(ap=dest_i[:, tile, k:k + 1], axis=0),
                    in_=x1b[j % 4], in_offset=None), r=[f"x1b{j % 4}", f"dest{tile}"], dma=f"scat{j % 4}", extra=zfill_ops)

        for j0 in range(2):
            xb_A_mm(j0)
            xb_A_rest(j0)
        xb_B0(0)
        for j in range(16):
            xb_B1(j)
            if j + 2 < 16:
                xb_A_mm(j + 2)
            if j + 1 < 16:
                xb_B0(j + 1)
            if j + 2 < 16:
                xb_A_rest(j + 2)
            if j >= 1:
                xb_B2(j - 1)
        xb_B2(15)
        P.barrier()

    if stage >= 2:
        A.top = persist_top
        wgu = [wgu0, A.alloc([8, 1024], BF16)]
        wdn = [wdn0, A.alloc([4, 1024], BF16)]
        xe = [A.alloc([3, 1024], BF16) for _ in range(2)]
        xTe = [A.alloc([8, CAP], BF16) for _ in range(2)]
        hact = A.alloc([4, CAP], BF16)
        sgs = [A.alloc([CAP]) for _ in range(2)]
        ysb = [A.alloc([1024]) for _ in range(4)]
        yi = 0
        dma("sp", xe[0], dap(xbuf_d, 0, [[D, 128], [128 * D, 3], [1, D]]), w=["xe0"], key="xe0")
        for e_ in range(NE):
            b = e_ % 2
            if e_ > 0:
                load_w(wgu[b], wgu_d, e_ * D, 0, 1024, 8, 1024, f"wgu{b}", f"wgu{b}")
                load_w(wdn[b], wdn_d, e_ * 512, 0, 1024, 4, D, f"wdn{b}", f"wdn{b}")
            if e_ + 1 < NE:
                dma("sp", xe[1 - b], dap(xbuf_d, (e_ + 1) * CAP * D, [[D, 128], [128 * D, 3], [1, D]]),
                    w=[f"xe{1 - b}"], key=f"xe{1 - b}")
            for r_ in range(3):
                tb = r_ % 2
                for dk in range(8):
                    tr(psb[tb][:, dk * 128:(dk + 1) * 128], xe[b][:, r_, dk * 128:(dk + 1) * 128], identb,
                       [f"xe{b}", "identb"], [PSN[tb]])
                tcopy("act" if r_ % 2 else "dve", xTe[b][:, :, r_ * 128:(r_ + 1) * 128],
                      psb[tb].rearrange("p (a b) -> p a b", a=8, b=128), [PSN[tb]], [f"xTe{b}"])
            for fc in range(4):
                pg_b, pu_b = 2 + (fc % 2) * 2, 3 + (fc % 2) * 2
                for dk in range(8):
                    mm(ps[pg_b][:, 0:CAP], wgu[b][:, dk, fc * 128:(fc + 1) * 128], xTe[b][:, dk, :], dk == 0, dk == 7,
                       [f"wgu{b}", f"xTe{b}"], [PSN[pg_b]])
                for dk in range(8):
                    mm(ps[pu_b][:, 0:CAP], wgu[b][:, dk, 512 + fc * 128:512 + (fc + 1) * 128], xTe[b][:, dk, :], dk == 0, dk == 7,
                       [f"wgu{b}", f"xTe{b}"], [PSN[pu_b]])
                act(sgs[fc % 2], ps[pg_b][:, 0:CAP], AF.Silu, [PSN[pg_b]], [f"sgs{fc % 2}"])
                tt("dve", hact[:, fc, :], sgs[fc % 2], ps[pu_b][:, 0:CAP], ALU.mult, [f"sgs{fc % 2}", PSN[pu_b]], ["hact"])
            for r_ in range(3):
                yb = yi % 4
                yi += 1
                for half in range(2):
                    pb = 6 + half
                    for fk in range(4):
                        mm(ps[pb], hact[:, fk, r_ * 128:(r_ + 1) * 128], wdn[b][:, fk, half * 512:(half + 1) * 512],
                           fk == 0, fk == 3, ["hact", f"wdn{b}"], [PSN[pb]])
                tcopy("act", ysb[yb][:, 0:512], ps[6], ["ps6"], [f"ysb{yb}"])
                tcopy("dve", ysb[yb][:, 512:1024], ps[7], ["ps7"], [f"ysb{yb}"])
                dma("sp", dap(ybuf_d, (e_ * CAP + r_ * 128) * D, [[D, 128], [1, D]]), ysb[yb],
                    r=[f"ysb{yb}"], key=f"ysb{yb}")
        P.barrier()

        A.top = persist_top
        l2g = A.alloc([1024])
        l2b = A.alloc([1024])
        dma("sp", l2g, dap(ln2g_d, 0, [[0, 128], [1, D]]), w=["l2g"], key="l2g")
        dma("sp", l2b, dap(ln2b_d, 0, [[0, 128], [1, D]]), w=["l2b"], key="l2b")
        y1 = [A.alloc([1024]) for _ in range(3)]
        y2 = [A.alloc([1024]) for _ in range(3)]
        xs = [A.alloc([1024]) for _ in range(3)]
        zz = A.alloc([1024])
        zn = [A.alloc([1024]) for _ in range(2)]
        ot = [A.alloc([1024]) for _ in range(2)]
        st2 = A.alloc([12])
        mv2 = A.alloc([2])
        rsd2 = A.alloc([1])
        nmr2 = A.alloc([1])
        def c_issue(tile):
            b = tile % 3
            for k, yk in ((0, y1[b]), (1, y2[b])):
                P.op("pool", lambda e, k=k, tile=tile, yk=yk: e.indirect_dma_start(
                    out=yk, out_offset=None, in_=ybuf_d.ap(),
                    in_offset=bass.IndirectOffsetOnAxis(ap=dest_i[:, tile, k:k + 1], axis=0)),
                    r=[f"dest{tile}"], w=[f"y{k}{b}"], dma=f"gy{k}{b}")
            dma("sp", xs[b], dap(x1_d, tile * 128 * D, [[D, 128], [1, D]]), w=[f"xs{b}"], key=f"xs{b}")

        zz2 = [zz, A.alloc([1024])]
        st3 = [st2, A.alloc([12])]
        mv3 = [mv2, A.alloc([2])]
        rsd3 = [rsd2, A.alloc([1])]
        nmr3 = [nmr2, A.alloc([1])]

        def c_front(tile):
            b = tile % 2
            c = tile % 3
            z_, st_, mv_, rs_, nm_ = zz2[b], st3[b], mv3[b], rsd3[b], nmr3[b]
            act(xs[c], xs[c], AF.Copy, [f"xs{c}"], [f"xs{c}"], scale=ALPHA)
            stt(z_, y1[c], wts[:, tile, 0:1], xs[c], ALU.mult, ALU.add, [f"y0{c}", f"xs{c}"], [f"zz{b}"])
            stt(z_, y2[c], wts[:, tile, 1:2], z_, ALU.mult, ALU.add, [f"y1{c}", f"zz{b}"], [f"zz{b}"])
            P.op("dve", lambda e, z_=z_, st_=st_: e.bn_stats(out=st_[:, 0:6], in_=z_[:, 0:512]), r=[f"zz{b}"], w=[f"st{b}"])
            P.op("dve", lambda e, z_=z_, st_=st_: e.bn_stats(out=st_[:, 6:12], in_=z_[:, 512:1024]), r=[f"zz{b}"], w=[f"st{b}"])
            P.op("dve", lambda e, st_=st_, mv_=mv_: e.bn_aggr(out=mv_, in_=st_), r=[f"st{b}"], w=[f"mv{b}"])
            ts("dve", rs_, mv_[:, 1:2], EPS, None, ALU.add, None, [f"mv{b}"], [f"rsd{b}"])
            act(rs_, rs_, AF.Ln, [f"rsd{b}"], [f"rsd{b}"])
            act(rs_, rs_, AF.Exp, [f"rsd{b}"], [f"rsd{b}"], scale=-0.5)
            stt(nm_, mv_[:, 0:1], -1.0, rs_[:, 0:1], ALU.mult, ALU.mult, [f"mv{b}", f"rsd{b}"], [f"nmr{b}"])
            act(zn[b], z_, AF.Identity, [f"zz{b}", f"rsd{b}", f"nmr{b}"], [f"zn{b}"], bias=nm_[:, 0:1], scale=rs_[:, 0:1])
            tt("pool", zn[b], zn[b], l2g, ALU.mult, [f"zn{b}", "l2g"], [f"zn{b}"])

        def c_back(tile):
            b = tile % 2
            tt("dve", ot[b], zn[b], l2b, ALU.add, [f"zn{b}", "l2b"], [f"ot{b}"])
            dma("sp", dap(y_d, tile * 128 * D, [[D, 128], [1, D]]), ot[b], r=[f"ot{b}"], key=f"ot{b}")

        c_issue(0)
        c_issue(1)
        c_issue(2)
        c_front(0)
        for tile in range(32):
            if tile + 3 < 32:
                c_issue(tile + 3)
            if tile + 1 < 32:
                c_front(tile + 1)
            c_back(tile)
    P.emit()
    return nc, A.peak


_CACHE = {}


def kernel(x, w_in, b_in, lb_logits, hg_norm_g, rel_bias, w_proj_a, w_proj_b, w_out, ln1_g, ln1_b,
           w_group, b_group, w_expert, b_expert, w_gate_up, w_down, ln2_g, ln2_b):
    f = lambda a: np.ascontiguousarray(np.asarray(a, dtype=np.float32))
    if "nc" not in _CACHE:
        _CACHE["nc"] = build()[0]
    nc = _CACHE["nc"]
    x = f(x)
    shared = {
        "w_in": f(w_in[0]), "b_in": f(b_in[0]), "lb_logits": f(lb_logits), "hg_norm_g": f(hg_norm_g[0]),
        "rel_bias": f(rel_bias), "w_proj_a": f(w_proj_a[0]), "w_proj_b": f(w_proj_b[0]), "w_out": f(w_out[0]),
        "ln1_g": f(ln1_g[0]), "ln1_b": f(ln1_b[0]),
        "w_router": f(np.concatenate([np.asarray(w_group[0]), np.asarray(w_expert[0])], axis=1)),
        "b_router": f(np.concatenate([np.asarray(b_group[0]), np.asarray(b_expert[0])], axis=0)),
        "w_gate_up": f(w_gate_up[0]), "w_down": f(w_down[0]), "ln2_g": f(ln2_g[0]), "ln2_b": f(ln2_b[0]),
        "consts": make_consts(),
        "eind": np.ascontiguousarray((np.arange(S)[None, :] // 256 == np.arange(8)[:, None]).astype(np.float32)),
    }
    in_maps = []
    for c in range(NCORES):
        m = dict(shared)
        m["x"] = np.ascontiguousarray(x[2 * c:2 * c + 2].reshape(NTOK, D))
        in_maps.append(m)
    res = run_bass_kernel_spmd(nc, in_maps, core_ids=list(range(NCORES)))
    out = np.concatenate([np.asarray(r["y"]).reshape(2, S, D) for r in res.results], axis=0)
    return out.astype(np.float32)
```

```python
import os
import numpy as np
import concourse.bass as bass
import concourse.mybir as mybir
from concourse.bass_utils import run_bass_kernel_spmd

F32 = mybir.dt.float32
BF16 = mybir.dt.bfloat16
I32 = mybir.dt.int32
AF = mybir.ActivationFunctionType
ALU = mybir.AluOpType
AX = mybir.AxisListType

NCORES = 8
D = 1024
S = 2048
NTOK = 4096
INC = 5632
ALPHA = 2.0 ** 0.25
EPS = 1e-5
NEG = -30000.0
CAP = 384
NE = 32
ENGS = ("pe", "act", "dve", "pool", "sp")

C_ID, C_M2, C_ANTI, C_LTRI, C_PAST, C_OWN, C_EOFF, C_E, C_OHG, NCST = 0, 128, 256, 384, 512, 1024, 1536, 1568, 2592, 3104


class Op:
    __slots__ = ("eng", "fn", "deps", "dma_key", "dma_val", "sig", "idx", "waits")

    def __init__(self, eng, fn):
        self.eng = eng
        self.fn = fn
        self.deps = set()
        self.dma_key = None
        self.dma_val = 0
        self.sig = 0
        self.waits = []


class Prog:
    def __init__(self, nc):
        self.nc = nc
        self.ops = []
        self.last_w = {}
        self.readers = {}
        self.dma_cnt = {}
        self.last_eng = {}
        self.last_dma = {}
        self.bg_keys = set()

    def op(self, eng, fn, r=(), w=(), dma=None, extra=()):
        o = Op(eng, fn)
        o.idx = len(self.ops)
        deps = set(extra)
        for x in r:
            lw = self.last_w.get(x)
            if lw is not None:
                deps.add(lw)
        for x in w:
            lw = self.last_w.get(x)
            if lw is not None:
                deps.add(lw)
            for rd in self.readers.get(x, ()):
                deps.add(rd)
        deps.discard(o.idx)
        o.deps = deps
        for x in r:
            self.readers.setdefault(x, []).append(o.idx)
        for x in w:
            self.last_w[x] = o.idx
            self.readers[x] = []
        if dma is not None:
            o.dma_key = dma
            self.dma_cnt[dma] = self.dma_cnt.get(dma, 0) + 1
            o.dma_val = 16 * self.dma_cnt[dma]
            self.last_dma[dma] = o.idx
        else:
            self.last_eng[eng] = o.idx
        self.ops.append(o)
        return o

    def barrier(self):
        deps = set(self.last_eng.values()) | set(v for k, v in self.last_dma.items() if k not in self.bg_keys)
        for e in ENGS:
            self.op(e, lambda eng: None, extra=deps)
        self.last_w = {}
        self.readers = {}

    def finalize(self):
        ops = self.ops

        def skip(D, o):
            return D.eng == "pe" and o.eng == "pe" and o.dma_key is None and D.dma_key is None

        needs_sig = set()
        for o in ops:
            for d in o.deps:
                Dd = ops[d]
                if Dd.dma_key is not None or skip(Dd, o):
                    continue
                needs_sig.add(d)
        cnt = {e: 0 for e in ENGS}
        for o in ops:
            if o.idx in needs_sig:
                cnt[o.eng] += 1
                o.sig = cnt[o.eng]
        waited = {e: {} for e in ENGS}
        for o in ops:
            wl = {}
            for d in o.deps:
                Dd = ops[d]
                if Dd.dma_key is not None:
                    k, v = ("dma", Dd.dma_key), Dd.dma_val
                else:
                    if skip(Dd, o):
                        continue
                    k, v = ("eng", Dd.eng), Dd.sig
                if v > wl.get(k, 0):
                    wl[k] = v
            for k, v in wl.items():
                if waited[o.eng].get(k, 0) >= v:
                    continue
                waited[o.eng][k] = v
                o.waits.append((k, v))

    def emit(self):
        nc = self.nc
        self.finalize()
        sems = {}
        for e in ENGS:
            sems[("eng", e)] = nc.alloc_semaphore(name=f"s_{e}")
        for k in self.dma_cnt:
            sems[("dma", k)] = nc.alloc_semaphore(name=f"d_{k}")
        by_eng = {e: [o for o in self.ops if o.eng == e] for e in ENGS}
        prog = self

        def run(engname, eng):
            for o in by_eng[engname]:
                for k, v in o.waits:
                    eng.wait_ge(sems[k], v)
                ins = o.fn(eng)
                if ins is None:
                    if o.sig:
                        eng.nop().then_inc(sems[("eng", engname)], 1)
                    continue
                if o.dma_key is not None:
                    ins.then_inc(sems[("dma", o.dma_key)], 16)
                elif o.sig:
                    ins.then_inc(sems[("eng", engname)], 1)
            if engname == "sp":
                for k, n in prog.dma_cnt.items():
                    eng.wait_ge(sems[("dma", k)], 16 * n)

        with nc.Block() as block:
            @block.tensor
            def _(e):
                run("pe", e)

            @block.scalar
            def _(e):
                run("act", e)

            @block.vector
            def _(e):
                run("dve", e)

            @block.gpsimd
            def _(e):
                run("pool", e)

            @block.sync
            def _(e):
                run("sp", e)


class Arena:
    def __init__(self, nc, words):
        self.t = nc.alloc_sbuf_tensor("arena", [128, words], F32)
        self.top = 0
        self.W = words
        self.peak = 0

    def alloc_at(self, off, shape, dt=F32):
        assert off >= self.top, (off, self.top)
        save = self.top
        self.top = off
        v = self.alloc(shape, dt)
        self.top = save
        return v

    def alloc(self, shape, dt=F32):
        shape = list(shape)
        n = int(np.prod(shape))
        words = n if dt in (F32, I32) else (n + 1) // 2
        words = (words + 7) // 8 * 8
        off = self.top
        self.top += words
        self.peak = max(self.peak, self.top)
        assert self.top <= self.W, f"arena overflow {self.top} > {self.W}"
        v = self.t[:, off:off + words]
        self.last_raw = v
        if dt != F32:
            v = v.bitcast(dt)
        v = v[:, 0:n]
        if len(shape) == 2:
            v = v.rearrange("p (a b) -> p a b", a=shape[0], b=shape[1])
        elif len(shape) == 3:
            v = v.rearrange("p (a b c) -> p a b c", a=shape[0], b=shape[1], c=shape[2])
        return v


def dap(t, off, pat):
    return bass.AP(tensor=t, offset=off, ap=[list(x) for x in pat])


def _t5_bucket(dist):
    dist = np.asarray(dist, dtype=np.int64)
    d = np.maximum(dist, 1).astype(np.float32)
    lp = 16 + (np.log(d / np.float32(16.0)) / np.float32(np.log(128.0 / 16.0)) * np.float32(16.0)).astype(np.int32)
    return np.where(dist < 16, dist, np.minimum(lp, 31))


def make_consts():
    c = np.zeros((128, NCST), np.float32)
    i = np.arange(128)
    c[:, C_ID:C_ID + 128] = np.eye(128)
    s_, t_ = np.meshgrid(i, i, indexing="ij")
    c[:, C_M2:C_M2 + 128] = ((s_ // 64 == t_ // 64) & (s_ <= t_)).astype(np.float32)
    c[:, C_ANTI:C_ANTI + 128] = np.eye(128)[::-1]
    c[:, C_LTRI:C_LTRI + 128] = (s_ < t_).astype(np.float32)
    past = np.zeros((8, 8, 8), np.float32)
    own = np.zeros((8, 8, 8), np.float32)
    for qb in range(8):
        for n in range(8):
            past[qb, :, n] = 0.0 if n < qb else -1e30
            own[qb, :, n] = 1.0 if n == qb else 0.0
    c[:, C_PAST:C_PAST + 512] = past.reshape(1, 512)
    c[:, C_OWN:C_OWN + 512] = own.reshape(1, 512)
    c[:, C_EOFF:C_EOFF + 32] = (np.arange(32) * CAP).astype(np.float32)[None, :]
    E = np.zeros((8, 8, 128), np.float32)
    for n in range(8):
        E[n, n, :] = 1.0
    c[0:8, C_E:C_E + 1024] = E.reshape(8, 1024)
    ohg = np.zeros((33, 2, 256), np.float32)
    for u in range(255):
        d = u - 127
        if d >= 0:
            ohg[_t5_bucket(d), 0, u] += 1.0
            ohg[31, 0, u] -= 1.0
        else:
            ohg[32, 0, u] = NEG
        dist = d + 128
        ohg[_t5_bucket(dist), 1, u] += 1.0
        ohg[31, 1, u] -= 1.0
    c[0:33, C_OHG:C_OHG + 512] = ohg.reshape(33, 512)
    return c


def build(debug=0, stage=99, maxphase=99):
    nc = bass.Bass("TRN2", target_bir_lowering=False)

    def din(name, shape, dt=F32):
        return nc.dram_tensor(name, list(shape), dt, kind="ExternalInput")

    x_d = din("x", [NTOK, D])
    win_d = din("w_in", [D, INC])
    bin_d = din("b_in", [INC])
    lbl_d = din("lb_logits", [2, 512])
    ng_d = din("hg_norm_g", [512])
    rb_d = din("rel_bias", [32, 8])
    wpa_d = din("w_proj_a", [512, D])
    wpb_d = din("w_proj_b", [512, D])
    wout_d = din("w_out", [D, D])
    ln1g_d = din("ln1_g", [D])
    ln1b_d = din("ln1_b", [D])
    wr_d = din("w_router", [D, 36])
    br_d = din("b_router", [36])
    wgu_d = din("w_gate_up", [NE, D, 1024])
    wdn_d = din("w_down", [NE, 512, D])
    ln2g_d = din("ln2_g", [D])
    ln2b_d = din("ln2_b", [D])
    cst_d = din("consts", [128, NCST])
    eind_d = din("eind", [8, S])
    y_d = nc.dram_tensor("y", [NTOK, D], F32, kind="ExternalOutput")

    def dscr(name, shape, dt):
        return nc.dram_tensor(name, list(shape), dt, kind="Internal")

    gd_d = dscr("gd", [8, 512], BF16)
    ohg_d = dscr("ohgT", [4, 128, S], BF16)
    omb_d = dscr("ombT", [8, 64, S], BF16)
    x1_d = dscr("x1s", [NTOK, D], F32)
    xbuf_d = dscr("xbuf", [NE * CAP + 128, D], BF16)
    ybuf_d = dscr("ybuf", [NE * CAP, D], F32)
    dbg = {}
    if debug:
        dbg["ohg"] = nc.dram_tensor("dbg_ohg", [4, 128, S], BF16, kind="ExternalOutput")
        dbg["omb"] = nc.dram_tensor("dbg_omb", [8, 64, S], BF16, kind="ExternalOutput")
        dbg["x1"] = nc.dram_tensor("dbg_x1", [NTOK, D], F32, kind="ExternalOutput")

    P = Prog(nc)
    A = Arena(nc, 47 * 1024)
    ps = [nc.alloc_psum_tensor(f"ps{i}", [128, 512], F32)[:, :] for i in range(8)]
    psb = [p.bitcast(BF16) for p in ps]
    PSN = [f"ps{i}" for i in range(8)]

    def dma(eng, out, in_, r=(), w=(), key=None, nonc=False):
        if nonc:
            return P.op(eng, lambda e: e.dma_start(out=out, in_=in_, allow_slow_non_contiguous=True), r=r, w=w, dma=key)
        return P.op(eng, lambda e: e.dma_start(out=out, in_=in_), r=r, w=w, dma=key)

    def mm(out, lhsT, rhs, start, stop, r, w):
        return P.op("pe", lambda e: e.matmul(out, lhsT=lhsT, rhs=rhs, start=start, stop=stop), r=r, w=w)

    def tr(out, in_, ident, r, w):
        return P.op("pe", lambda e: e.transpose(out=out, in_=in_, identity=ident), r=r, w=w)

    def act(out, in_, func, r, w, bias=None, scale=None, accum=None):
        kw = {}
        if bias is not None:
            kw["bias"] = bias
        if scale is not None:
            kw["scale"] = scale
        if accum is not None:
            kw["accum_out"] = accum
        return P.op("act", lambda e: e.activation(out=out, in_=in_, func=func, **kw), r=r, w=w)

    def tcopy(eng, out, in_, r, w):
        if eng == "act":
            return P.op(eng, lambda e: e.activation(out=out, in_=in_, func=AF.Copy), r=r, w=w)
        return P.op(eng, lambda e: e.tensor_copy(out=out, in_=in_), r=r, w=w)

    def tt(eng, out, in0, in1, op, r, w):
        return P.op(eng, lambda e: e.tensor_tensor(out=out, in0=in0, in1=in1, op=op), r=r, w=w)

    def ts(eng, out, in0, s1, s2, op0, op1, r, w):
        if op1 is None:
            return P.op(eng, lambda e: e.tensor_scalar(out=out, in0=in0, scalar1=s1, scalar2=None, op0=op0), r=r, w=w)
        return P.op(eng, lambda e: e.tensor_scalar(out=out, in0=in0, scalar1=s1, scalar2=s2, op0=op0, op1=op1), r=r, w=w)

    def stt(out, in0, scalar, in1, op0, op1, r, w):
        return P.op("dve", lambda e: e.scalar_tensor_tensor(out=out, in0=in0, scalar=scalar, in1=in1, op0=op0, op1=op1), r=r, w=w)

    def memset(eng, ap, val, w):
        return P.op(eng, lambda e: e.memset(ap, val), w=w)

    cst = A.alloc([NCST])
    dma("sp", cst, cst_d.ap(), w=["cst"], key="cst")
    identf = cst[:, C_ID:C_ID + 128]
    cb = A.alloc([4, 128], BF16)
    identb, antib, ltrib, onesb = cb[:, 0, :], cb[:, 1, :], cb[:, 2, :], cb[:, 3, :]
    tcopy("dve", identb, cst[:, C_ID:C_ID + 128], ["cst"], ["identb"])
    tcopy("dve", antib, cst[:, C_ANTI:C_ANTI + 128], ["cst"], ["antib"])
    tcopy("dve", ltrib, cst[:, C_LTRI:C_LTRI + 128], ["cst"], ["ltrib"])
    memset("dve", onesb, 1.0, ["onesb"])
    Eb = A.alloc([8, 128], BF16)
    memset("dve", Eb.rearrange("p a b -> p (a b)"), 0.0, ["Eb"])
    tcopy("dve", Eb[0:8], cst[0:8, C_E:C_E + 1024].rearrange("p (a b) -> p a b", a=8, b=128), ["cst"], ["Eb"])
    mask2x4 = A.alloc([4, 128])
    for h in range(4):
        tcopy("dve", mask2x4[:, h, :], cst[:, C_M2:C_M2 + 128], ["cst"], ["mask2x4"])
    resetm = A.alloc([512], BF16)
    memset("dve", resetm, 1.0, ["resetm"])
    memset("dve", resetm[:, 0:512:64], 0.0, ["resetm"])
    pastm = cst[:, C_PAST:C_PAST + 512].rearrange("p (q h n) -> p q h n", q=8, h=8, n=8)
    ownm = cst[:, C_OWN:C_OWN + 512].rearrange("p (q h n) -> p q h n", q=8, h=8, n=8)

    bcol = A.alloc([44])
    dma("sp", bcol, dap(bin_d, 0, [[1, 128], [128, 44]]), w=["bcol"], key="bcol", nonc=True)
    bcolq = A.alloc([4])
    ts("dve", bcolq, bcol[:, 16:20], 0.125, None, ALU.mult, None, ["bcol"], ["bcolq"])
    brow = A.alloc([1536], BF16)
    dma("pool", brow[0:1, 0:1024], dap(bin_d, 1024, [[0, 1], [1, 1024]]), w=["brow"], key="brow")
    dma("pool", brow[0:1, 1024:1536], dap(bin_d, 3072, [[0, 1], [1, 512]]), w=["brow"], key="brow")
    onesr = A.alloc([128], BF16)
    memset("dve", onesr[0:1, :], 1.0, ["onesr"])
    lbl = A.alloc([2, 4])
    dma("sp", lbl, dap(lbl_d, 0, [[1, 128], [512, 2], [128, 4]]), w=["lbl"], key="lbl", nonc=True)
    lb = A.alloc([4])
    oml = A.alloc([4])
    tt("dve", lb, lbl[:, 0, :], lbl[:, 1, :], ALU.subtract, ["lbl"], ["lb"])
    act(lb, lb, AF.Sigmoid, ["lb"], ["lb"])
    ts("dve", oml, lb, -1.0, 1.0, ALU.mult, ALU.add, ["lb"], ["oml"])
    ng_bc = A.alloc([512])
    dma("sp", ng_bc, dap(ng_d, 0, [[0, 128], [1, 512]]), w=["ng_bc"], key="ng_bc")
    rb31 = A.alloc([8])
    dma("sp", rb31, dap(rb_d, 31 * 8, [[0, 128], [1, 8]]), w=["rb31"], key="rb31")
    rbm = A.alloc([8, 8])
    tcopy("dve", rbm, rb31.unsqueeze(2).to_broadcast([128, 8, 8]), ["rb31"], ["rbm"])
    rbl = A.alloc([8])
    dma("sp", rbl[0:32, :], rb_d.ap(), w=["rbl"], key="rbl")
    memset("dve", rbl[32:33, :], 1.0, ["rbl1"])
    mm(ps[0][0:8, 0:512], rbl[0:33, 0:8], cst[0:33, C_OHG:C_OHG + 512], True, True, ["rbl", "rbl1", "cst"], ["ps0"])
    Gs = A.alloc([512], BF16)
    tcopy("dve", Gs[0:8, :], ps[0][0:8, 0:512], ["ps0"], ["Gs"])
    dma("sp", gd_d.ap(), Gs[0:8, :], r=["Gs"], w=["gd_d"], key="gd_w")
    Yc = A.alloc([16, 128], BF16)
    dma("sp", Yc, dap(gd_d, 0, [[1, 128], [256, 16], [1, 128]]), r=["gd_d"], w=["Yc"], key="Yc")
    wr = A.alloc([8, 36])
    dma("sp", wr, dap(wr_d, 0, [[36, 128], [128 * 36, 8], [1, 36]]), w=["wr"], key="wr")
    br_bc = A.alloc([36])
    dma("sp", br_bc, dap(br_d, 0, [[0, 128], [1, 36]]), w=["br_bc"], key="br_bc")
    carry = A.alloc([32])
    tcopy("dve", carry, cst[:, C_EOFF:C_EOFF + 32], ["cst"], ["carry"])
    dest_i = A.alloc([32, 2], I32)
    wts = A.alloc([32, 2])
    zrow = A.alloc([1024], BF16)
    memset("dve", A.last_raw, 0.0, ["zrow"])
    P.barrier()
    persist_top = A.top
    def start_zero_fill():
        P.bg_keys.add("zfill")
        zfill_ops = []
        nrow = NE * CAP + 128
        for r0 in range(0, nrow, 1024):
            nr = min(1024, nrow - r0)
            zfill_ops.append(dma("sp", dap(xbuf_d, r0 * D, [[D, 128], [128 * D, nr // 128], [1, D]]),
                                 zrow.unsqueeze(1).to_broadcast([128, nr // 128, 1024]), key="zfill").idx)
        return zfill_ops

    def load_w(dst, src_t, row0, col0, ncols, nk, rowstride, key, rname, kp=128):
        dma("pool", dst, dap(src_t, row0 * rowstride + col0, [[rowstride, kp], [kp * rowstride, nk], [1, ncols]]),
            w=[rname], key=key)

    for s in range(2 if stage >= 1 else 0):
        A.top = persist_top
        xT = A.alloc([8, S], BF16)
        seq_top = A.top
        xb = [A.alloc([1024], BF16) for _ in range(2)]
        wq = A.alloc_at(seq_top + 8320, [8, 512], BF16)
        wf = A.alloc_at(seq_top + 8320 + 2048, [8, 512], BF16)
        for j in range(16):
            b = j % 2
            if j == 4:
                load_w(wq, win_d, 0, 0, 512, 8, INC, "wq", "wq")
                load_w(wf, win_d, 0, 512, 512, 8, INC, "wf", "wf")
            dma("pool", xb[b], dap(x_d, (s * 16 + j) * 128 * D, [[D, 128], [1, D]]), w=[f"xb{b}"], key=f"xb{b}")
            for dk in range(8):
                tr(psb[b][:, dk * 128:(dk + 1) * 128], xb[b][:, dk * 128:(dk + 1) * 128], identb,
                   [f"xb{b}", "identb"], [PSN[b]])
            tcopy("act" if b else "dve", xT[:, :, j * 128:(j + 1) * 128],
                  psb[b].rearrange("p (a b) -> p a b", a=8, b=128), [PSN[b]], [f"xT{j}"])
        XT = lambda j0, n: [f"xT{j}" for j in range(j0, j0 + n)]
        P.barrier()
        if s == 0:
            zfill_ops = start_zero_fill()
        if maxphase <= 1:
            break
        A.top = seq_top

        qtT = A.alloc([4, S], BF16)
        ktT = A.alloc([4, S], BF16)
        dec = A.alloc([4, 32])
        h_top = A.top
        assert A.top == seq_top + 8320, A.top - seq_top
        A.top += 4096
        wi = A.alloc_at(seq_top + 19712, [8, 512], BF16)
        wg = A.alloc_at(seq_top + 19712 + 2048, [8, 512], BF16)
        load_w(wi, win_d, 0, 1024, 512, 8, INC, "wi", "wi")
        load_w(wg, win_d, 0, 1536, 512, 8, INC, "wg", "wg")
        sq2 = [A.alloc([512], BF16) for _ in range(2)]
        fb2 = [A.alloc([512]) for _ in range(2)]
        lfb2 = [A.alloc([512]) for _ in range(2)]
        bb2 = [A.alloc([512]) for _ in range(2)]
        kk2 = [A.alloc([512]) for _ in range(2)]
        eb2 = [A.alloc([512]) for _ in range(2)]
        enb2 = [A.alloc([512]) for _ in range(2)]
        its = [(h, tg) for h in range(4) for tg in range(4)]
        for r0 in range(0, 16, 2):
            rnd = []
            for u in range(2):
                h, tg = its[r0 + u]
                rnd.append((u, h, tg, slice(tg * 512, (tg + 1) * 512), 2 + u * 2, 3 + u * 2,
                            sq2[u], fb2[u], lfb2[u], bb2[u], kk2[u], eb2[u], enb2[u]))
            for (u, h, tg, tsl, bq, bf_, sq, fb, lfb, bb, kk, eb, enb) in rnd:
                for dk in range(8):
                    mm(ps[bq], wq[:, dk, h * 128:(h + 1) * 128], xT[:, dk, tsl], dk == 0, dk == 7,
                       ["wq"] + XT(tg * 4, 4), [PSN[bq]])
                for dk in range(8):
                    mm(ps[bf_], wf[:, dk, h * 128:(h + 1) * 128], xT[:, dk, tsl], dk == 0, dk == 7,
                       ["wf"] + XT(tg * 4, 4), [PSN[bf_]])
            for (u, h, tg, tsl, bq, bf_, sq, fb, lfb, bb, kk, eb, enb) in rnd:
                act(sq, ps[bq], AF.Silu, [PSN[bq], "bcol"], [f"sq{u}"], bias=bcol[:, h:h + 1])
                act(fb, ps[bf_], AF.Sigmoid, [PSN[bf_], "bcol"], [f"fb{u}"], bias=bcol[:, 4 + h:5 + h])
                ts("dve", fb, fb, oml[:, h:h + 1], lb[:, h:h + 1], ALU.mult, ALU.add, [f"fb{u}", "oml", "lb"], [f"fb{u}"])
            for (u, h, tg, tsl, bq, bf_, sq, fb, lfb, bb, kk, eb, enb) in rnd:
                act(lfb, fb, AF.Ln, [f"fb{u}"], [f"lfb{u}"])
                P.op("dve", lambda e, bb=bb, lfb=lfb: e.tensor_tensor_scan(out=bb, data0=resetm, data1=lfb, initial=0.0,
                                                                         op0=ALU.mult, op1=ALU.add),
                     r=["resetm", f"lfb{u}"], w=[f"bb{u}"])
                ts("pool", kk, fb, -1.0, 1.0, ALU.mult, ALU.add, [f"fb{u}"], [f"kk{u}"])
            for (u, h, tg, tsl, bq, bf_, sq, fb, lfb, bb, kk, eb, enb) in rnd:
                act(eb, bb, AF.Exp, [f"bb{u}"], [f"eb{u}"])
                act(enb, bb, AF.Exp, [f"bb{u}"], [f"enb{u}"], scale=-1.0)
                tt("dve", qtT[:, h, tsl], sq, eb, ALU.mult, [f"sq{u}", f"eb{u}"], [f"qt{h}"])
                tt("pool", ktT[:, h, tsl], kk, enb, ALU.mult, [f"kk{u}", f"enb{u}"], [f"kt{h}"])
                tcopy("dve", dec[:, h, tg * 8:(tg + 1) * 8], eb[:, 63:512:64], [f"eb{u}"], ["dec"])
        P.barrier()
        if maxphase <= 2:
            break
        A.top = h_top

        wmq = A.alloc_at(seq_top + 24576, [8, 512], BF16)
        wmk = A.alloc_at(seq_top + 24576 + 2048, [8, 512], BF16)
        wmv = A.alloc_at(seq_top + 24576 + 4096, [8, 512], BF16)
        load_w(wmq, win_d, 0, 2048, 512, 8, INC, "wmq", "wmq")
        load_w(wmk, win_d, 0, 2560, 512, 8, INC, "wmk", "wmk")
        load_w(wmv, win_d, 0, 3072, 512, 8, INC, "wmv", "wmv")
        St = A.alloc([4, 128])
        Sb = A.alloc([4, 128], BF16)
        Sbb = A.alloc([4, 128], BF16)
        memset("dve", St, 0.0, ["St"])
        memset("dve", Sb, 0.0, ["Sb"])
        vt = [A.alloc([512], BF16) for _ in range(2)]
        gt = [A.alloc([512], BF16) for _ in range(2)]
        gs = A.alloc([512])
        ktk = [A.alloc([4, 128], BF16) for _ in range(2)]
        scm = A.alloc([4, 128], BF16)
        Tb = A.alloc([4, 128])
        sqo = A.alloc([512])
        ssq = A.alloc([4])
        rs = A.alloc([4])
        otmp = A.alloc([4, 128])
        otok = A.alloc([4, 128], BF16)
        ohs = [A.alloc([4, 512], BF16) for _ in range(2)]
        def f_v(j):
            b = j % 2
            tl = slice(j * 128, (j + 1) * 128)
            mm(ps[0], onesr[0:1, :], brow[0:1, 0:512], True, False, ["onesr", "brow"], ["ps0"])
            for dk in range(8):
                mm(ps[0], xT[:, dk, tl], wi[:, dk, :], False, dk == 7, [f"xT{j}", "wi"], ["ps0"])
            tcopy("act", vt[b], ps[0], ["ps0"], [f"vt{b}"])

        def f_g(j):
            b = j % 2
            tl = slice(j * 128, (j + 1) * 128)
            mm(ps[1], onesr[0:1, :], brow[0:1, 512:1024], True, False, ["onesr", "brow"], ["ps1"])
            for dk in range(8):
                mm(ps[1], xT[:, dk, tl], wg[:, dk, :], False, dk == 7, [f"xT{j}", "wg"], ["ps1"])
            act(gs, ps[1], AF.Silu, ["ps1"], ["gs"])
            tt("pool", gt[b], gs, ng_bc, ALU.mult, ["gs", "ng_bc"], [f"gt{b}"])

        def f_k(j):
            b = j % 2
            tl = slice(j * 128, (j + 1) * 128)
            for h in range(4):
                tr(psb[6][:, h * 128:(h + 1) * 128], ktT[:, h, tl], identb, [f"kt{h}", "identb"], ["ps6"])
            tcopy("act", ktk[b], psb[6][:, 0:512].rearrange("p (a b) -> p a b", a=4, b=128), ["ps6"], [f"ktk{b}"])

        def b_a(j):
            b = j % 2
            tl = slice(j * 128, (j + 1) * 128)
            for h in range(4):
                mm(ps[2][:, h * 128:(h + 1) * 128], ktT[:, h, tl], qtT[:, h, tl], True, True,
                   [f"kt{h}", f"qt{h}"], ["ps2"])
            tt("dve", scm, ps[2].rearrange("p (a b) -> p a b", a=4, b=128), mask2x4, ALU.mult,
               ["ps2", "mask2x4"], ["scm"])
            for h in range(4):
                hs = slice(h * 128, (h + 1) * 128)
                mm(ps[4][:, hs], ktk[b][0:64, h, :], vt[b][0:64, hs], True, True, [f"ktk{b}", f"vt{b}"], ["ps4"])
            for h in range(4):
                hs = slice(h * 128, (h + 1) * 128)
                mm(ps[5][:, hs], ktk[b][64:128, h, :], vt[b][64:128, hs], True, True, [f"ktk{b}", f"vt{b}"], ["ps5"])
            dec0 = dec[:, :, 2 * j:2 * j + 1].to_broadcast([128, 4, 128])
            tt("dve", Tb, ps[4].rearrange("p (a b) -> p a b", a=4, b=128), St, ALU.add, ["ps4", "St"], ["Tb"])
            tt("dve", Sbb, Tb, dec0, ALU.mult, ["Tb", "dec"], ["Sbb"])
            tt("pool", St, Tb, dec0, ALU.mult, ["Tb", "dec"], ["St"])

        def b_b(j):
            b = j % 2
            t0 = j * 128
            dec1 = dec[:, :, 2 * j + 1:2 * j + 2].to_broadcast([128, 4, 128])
            for h in range(4):
                hs = slice(h * 128, (h + 1) * 128)
                mm(ps[3][:, hs], scm[:, h, :], vt[b][:, hs], True, False, ["scm", f"vt{b}"], ["ps3"])
                mm(ps[3][0:64, hs], qtT[:, h, t0:t0 + 64], Sb[:, h, :], False, True, [f"qt{h}", "Sb"], ["ps3"])
                mm(ps[3][64:128, hs], qtT[:, h, t0 + 64:t0 + 128], Sbb[:, h, :], False, True, [f"qt{h}", "Sbb"], ["ps3"])
            tt("dve", Tb, ps[5].rearrange("p (a b) -> p a b", a=4, b=128), St, ALU.add, ["ps5", "St"], ["Tb"])
            tt("dve", Sb, Tb, dec1, ALU.mult, ["Tb", "dec"], ["Sb"])
            tt("pool", St, Tb, dec1, ALU.mult, ["Tb", "dec"], ["St"])

        def b_c(j):
            b = j % 2
            act(sqo, ps[3], AF.Square, ["ps3"], ["sqo"])
            P.op("dve", lambda e: e.tensor_reduce(out=ssq, in_=sqo.rearrange("p (a b) -> p a b", a=4, b=128),
                                                  axis=AX.X, op=ALU.add), r=["sqo"], w=["ssq"])
            ts("dve", ssq, ssq, 1.0 / 128.0, EPS, ALU.mult, ALU.add, ["ssq"], ["ssq"])
            act(rs, ssq, AF.Ln, ["ssq"], ["rs"])
            act(rs, rs, AF.Exp, ["rs"], ["rs"], scale=-0.5)
            tt("dve", otmp, ps[3].rearrange("p (a b) -> p a b", a=4, b=128),
               rs.unsqueeze(2).to_broadcast([128, 4, 128]), ALU.mult, ["ps3", "rs"], ["otmp"])
            tt("pool", otok, otmp, gt[b].rearrange("p (a b) -> p a b", a=4, b=128), ALU.mult,
               ["otmp", f"gt{b}"], ["otok"])

        def b_d(j):
            for h in range(4):
                tr(psb[7][:, h * 128:(h + 1) * 128], otok[:, h, :], identb, ["otok", "identb"], ["ps7"])
            g4 = (j // 4) % 2
            tcopy("act", ohs[g4][:, :, (j % 4) * 128:(j % 4 + 1) * 128],
                  psb[7][:, 0:512].rearrange("p (a b) -> p a b", a=4, b=128), ["ps7"], [f"ohs{g4}"])
            if j % 4 == 3:
                tg = j // 4
                dma("sp", dap(ohg_d, tg * 512, [[S, 128], [128 * S, 4], [1, 512]]), ohs[g4],
                    r=[f"ohs{g4}"], key=f"ohs{g4}")
                if debug and s == 0:
                    dma("sp", dap(dbg["ohg"], tg * 512, [[S, 128], [128 * S, 4], [1, 512]]), ohs[g4],
                        r=[f"ohs{g4}"], key=f"dbgohs{g4}")

        f_v(0)
        f_g(0)
        f_k(0)
        for j in range(16):
            nx = j + 1 < 16
            b_a(j)
            if nx:
                f_v(j + 1)
                f_k(j + 1)
            if j >= 1:
                b_d(j - 1)
            b_b(j)
            if nx:
                f_g(j + 1)
            b_c(j)
        b_d(15)
        P.barrier()
        if maxphase <= 3:
            break
        A.top = seq_top

        mqE = A.alloc([4, S], BF16)
        memset("dve", A.last_raw[64:128], 0.0, ["mqz"])
        mqO = A.alloc([4, S], BF16)
        memset("dve", A.last_raw[0:64], 0.0, ["mqz"])
        mkE = A.alloc([4, S], BF16)
        memset("dve", A.last_raw[64:128], 0.0, ["mkz"])
        mkO = A.alloc([4, S], BF16)
        memset("dve", A.last_raw[0:64], 0.0, ["mkz"])
        for p in range(4):
            dma("pool", mkE[64:72, p, :], eind_d.ap(), r=["mkz"], w=[f"mkiE{p}"], key="mkzE")
            dma("pool", mkO[0:8, p, :], eind_d.ap(), r=["mkz"], w=[f"mkiO{p}"], key="mkzO")
        vaug = A.alloc([16, 8, 65], BF16)
        kmE = A.alloc([4, 8], BF16)
        kmO = A.alloc([4, 8], BF16)
        memset("dve", kmE.rearrange("p a b -> p (a b)"), 0.0, ["kmT"])
        memset("dve", kmO.rearrange("p a b -> p (a b)"), 0.0, ["kmT"])
        km32 = A.alloc([4, 8])
        m_top = A.top
        assert A.top <= seq_top + 24576, A.top - seq_top
        memset("dve", vaug.rearrange("p a b c -> p (a b c)"), 1.0, ["vaug"])
        it = 0
        for p in range(4):
            for tg in range(4):
                bq, bk = (it % 2) * 2, (it % 2) * 2 + 1
                it += 1
                tsl = slice(tg * 512, (tg + 1) * 512)
                for dk in range(8):
                    mm(ps[bq], wmq[:, dk, p * 128:(p + 1) * 128], xT[:, dk, tsl], dk == 0, dk == 7,
                       ["wmq"] + XT(tg * 4, 4), [PSN[bq]])
                for dk in range(8):
                    mm(ps[bk], wmk[:, dk, p * 128:(p + 1) * 128], xT[:, dk, tsl], dk == 0, dk == 7,
                       ["wmk"] + XT(tg * 4, 4), [PSN[bk]])
                act(mqE[0:64, p, tsl], ps[bq][0:64], AF.Identity, [PSN[bq], "bcolq", "mqz"], [f"mq{p}"], bias=bcolq[0:64, p:p + 1], scale=0.125)
                act(mqO[64:128, p, tsl], ps[bq][64:128], AF.Identity, [PSN[bq], "bcolq", "mqz"], [f"mq{p}"], bias=bcolq[64:128, p:p + 1], scale=0.125)
                ts("dve", mkE[0:64, p, tsl], ps[bk][0:64], bcol[0:64, 20 + p:21 + p], None, ALU.add, None, [PSN[bk], "bcol", "mkz"], [f"mk{p}"])
                ts("dve", mkO[64:128, p, tsl], ps[bk][64:128], bcol[64:128, 20 + p:21 + p], None, ALU.add, None, [PSN[bk], "bcol", "mkz"], [f"mk{p}"])
            P.op("dve", lambda e, p=p: e.tensor_reduce(out=km32[0:64, p, :], in_=mkE[0:64, p, :].rearrange("p (a b) -> p a b", a=8, b=256),
                                                       axis=AX.X, op=ALU.add), r=[f"mk{p}"], w=["km32"])
            P.op("dve", lambda e, p=p: e.tensor_reduce(out=km32[64:128, p, :], in_=mkO[64:128, p, :].rearrange("p (a b) -> p a b", a=8, b=256),
                                                       axis=AX.X, op=ALU.add), r=[f"mk{p}"], w=["km32"])
        ts("dve", kmE[0:64], km32[0:64], 1.0 / 256.0, None, ALU.mult, None, ["km32", "kmT"], ["kmT"])
        ts("dve", kmO[64:128], km32[64:128], 1.0 / 256.0, None, ALU.mult, None, ["km32", "kmT"], ["kmT"])
        gm = [A.alloc([8, 8]) for _ in range(2)]
        top8 = [A.alloc([8, 8]) for _ in range(2)]
        thr = [A.alloc([8]) for _ in range(2)]
        sel = [A.alloc([8, 8]) for _ in range(2)]
        Mtok = [A.alloc([8, 8], BF16) for _ in range(2)]
        PT = [A.alloc([2, 256], BF16) for _ in range(3)]
        osb = A.alloc([256])
        rden = A.alloc([256])
        onesf = A.alloc([64])
        memset("dve", onesf[64:65, :], 1.0, ["onesf"])
        oms = [A.alloc([8, 256], BF16) for _ in range(2)]

        def gate_front(qb, jls=(0, 1)):
            for jl in jls:
                jt = 2 * qb + jl
                tl = slice(jt * 128, (jt + 1) * 128)
                for h in range(8):
                    p_ = h // 2
                    mm(ps[6][:, jl * 64 + h * 8:jl * 64 + (h + 1) * 8], (mqO if h % 2 else mqE)[:, p_, tl], (kmO if h % 2 else kmE)[:, p_, :],
                       True, True, [f"mq{p_}", "kmT"], ["ps6"])
                tt("dve", gm[jl], ps[6][:, jl * 64:(jl + 1) * 64].rearrange("p (a b) -> p a b", a=8, b=8), pastm[:, qb], ALU.add,
                   ["ps6", "cst"], [f"gm{jl}"])
                for h in range(8):
                    P.op("dve", lambda e, h=h, jl=jl: e.max(out=top8[jl][:, h, :], in_=gm[jl][:, h, :]), r=[f"gm{jl}"], w=[f"top8{jl}"])
                ts("dve", thr[jl], top8[jl][:, :, 2], -1e29, None, ALU.max, None, [f"top8{jl}"], [f"thr{jl}"])
                tt("dve", sel[jl], gm[jl], thr[jl].unsqueeze(2).to_broadcast([128, 8, 8]), ALU.is_ge, [f"gm{jl}", f"thr{jl}"], [f"sel{jl}"])
                tt("dve", sel[jl], sel[jl], ownm[:, qb], ALU.max, [f"sel{jl}", "cst"], [f"sel{jl}"])
                ts("dve", sel[jl], sel[jl], -NEG, NEG, ALU.mult, ALU.add, [f"sel{jl}"], [f"sel{jl}"])
                tt("dve", Mtok[jl], sel[jl], rbm, ALU.add, [f"sel{jl}", "rbm"], [f"Mtok{jl}"])

        def gate_back(qb):
            for jl in range(2):
                qc = slice(qb * 256 + jl * 128, qb * 256 + (jl + 1) * 128)
                for h in range(8):
                    r0 = 0 if h % 2 else 64
                    tr(psb[7][r0:r0 + 8, h * 128:(h + 1) * 128], Mtok[jl][:, h, :], identb, [f"Mtok{jl}", "identb"], ["ps7"])
                p7 = psb[7].rearrange("p (a b c) -> p a b c", a=4, b=2, c=128)
                act(mqE[64:72, :, qc], p7[64:72, :, 0, :], AF.Copy, ["ps7"], [f"mq{p}" for p in range(4)])
                act(mqO[0:8, :, qc], p7[0:8, :, 1, :], AF.Copy, ["ps7"], [f"mq{p}" for p in range(4)])

        sc_state = {"i": 0}
        MKI = [f"mkiE{p}" for p in range(4)] + [f"mkiO{p}" for p in range(4)]

        def scores(qb, h, n):
            mb = qb % 2
            p_ = h // 2
            sb_ = sc_state["i"] % 3
            sc_state["i"] += 1
            psc = ps[sb_]
            qsel = mqO if h % 2 else mqE
            q_all = qsel[:, p_, qb * 256:(qb + 1) * 256]
            q_hi = qsel[:, p_, qb * 256 + 128:(qb + 1) * 256]
            own = (n == qb)
            for jj in range(2):
                kt_ = 2 * n + jj
                ktile = (mkO if h % 2 else mkE)[:, p_, kt_ * 128:(kt_ + 1) * 128]
                if own and jj == 1:
                    osl = slice(jj * 256 + 128, jj * 256 + 256)
                    mm(psc[:, osl], ktile, q_hi, True, False, [f"mk{p_}", f"mq{p_}"] + MKI, [PSN[sb_]])
                    mm(psc[:, osl], antib, Yc[:, h * 2 + 0, :], False, True, ["antib", "Yc"], [PSN[sb_]])
                else:
                    osl = slice(jj * 256, jj * 256 + 256)
                    corr = own or (n == qb - 1 and jj == 1)
                    mm(psc[:, osl], ktile, q_all, True, not corr, [f"mk{p_}", f"mq{p_}"] + MKI, [PSN[sb_]])
                    if own:
                        mm(psc[:, jj * 256:jj * 256 + 128], antib, Yc[:, h * 2 + 0, :], False, False, ["antib", "Yc"], [PSN[sb_]])
                        mm(psc[:, jj * 256 + 128:jj * 256 + 256], antib, Yc[:, h * 2 + 1, :], False, True, ["antib", "Yc"], [PSN[sb_]])
                    elif corr:
                        mm(psc[:, jj * 256:jj * 256 + 128], antib, Yc[:, h * 2 + 1, :], False, True, ["antib", "Yc"], [PSN[sb_]])
            return sb_

        def exp_pv(qb, h, n, sb_):
            po = 4 + h % 2
            psc = ps[sb_]
            pt = PT[sb_]
            first = (n == 0)
            if n == qb:
                act(pt[:, 0, :], psc[:, 0:256], AF.Exp, [PSN[sb_]], [f"PT{sb_}"])
                act(pt[:, 1, 128:256], psc[:, 384:512], AF.Exp, [PSN[sb_]], [f"PT{sb_}"])
                mm(ps[po][0:65, 0:256], vaug[:, 2 * n, h, :], pt[:, 0, :], first, False, [f"va{2 * n}", f"PT{sb_}"], [PSN[po]])
                mm(ps[po][0:65, 128:256], vaug[:, 2 * n + 1, h, :], pt[:, 1, 128:256], False, True,
                   [f"va{2 * n + 1}", f"PT{sb_}"], [PSN[po]])
            else:
                act(pt.rearrange("p a b -> p (a b)"), psc, AF.Exp, [PSN[sb_]], [f"PT{sb_}"])
                mm(ps[po][0:65, 0:256], vaug[:, 2 * n, h, :], pt[:, 0, :], first, False, [f"va{2 * n}", f"PT{sb_}"], [PSN[po]])
                mm(ps[po][0:65, 0:256], vaug[:, 2 * n + 1, h, :], pt[:, 1, :], False, False,
                   [f"va{2 * n + 1}", f"PT{sb_}"], [PSN[po]])

        def norm1(h):
            po = 4 + h % 2
            tcopy("act", osb[0:64, :], ps[po][0:64, 0:256], [PSN[po]], ["osb"])
            P.op("dve", lambda e, po=po: e.reciprocal(out=rden[64:65, :], in_=ps[po][64:65, 0:256]), r=[PSN[po]], w=["rden"])

        def norm2(qb, h):
            mb = qb % 2
            mm(ps[3][0:64, 0:256], onesf[64:65, 0:64], rden[64:65, :], True, True, ["onesf", "rden"], ["ps3"])
            tt("dve", oms[mb][0:64, h, :], osb[0:64, :], ps[3][0:64, 0:256], ALU.mult, ["osb", "ps3"], [f"oms{mb}"])

        gate_front(0)
        gate_back(0)
        for j in range(16):
            b = 4 + j % 2
            tl = slice(j * 128, (j + 1) * 128)
            mm(ps[b], onesr[0:1, :], brow[0:1, 1024:1536], True, False, ["onesr", "brow"], [PSN[b]])
            for dk in range(8):
                mm(ps[b], xT[:, dk, tl], wmv[:, dk, :], False, dk == 7, [f"xT{j}", "wmv"], [PSN[b]])
            tcopy("act" if j % 2 else "dve", vaug[:, j, :, 0:64], ps[b].rearrange("p (a b) -> p a b", a=8, b=64),
                  [PSN[b]], [f"va{j}", "vaug"])
        assert A.top <= seq_top + 24576, A.top - seq_top
        for qb in range(8):
            mb = qb % 2
            blocks = [(h, n) for h in range(8) for n in range(qb + 1)]
            pend = None
            sbs = {0: scores(qb, *blocks[0]), 1: scores(qb, *blocks[1])}
            for i, (h, n) in enumerate(blocks):
                if i + 2 < len(blocks):
                    sbs[i + 2] = scores(qb, *blocks[i + 2])
                exp_pv(qb, h, n, sbs[i])
                if pend is not None:
                    norm2(qb, pend)
                    pend = None
                if n == qb:
                    norm1(h)
                    pend = h
                    if qb + 1 < 8 and h in (1, 4):
                        gate_front(qb + 1, (0,) if h == 1 else (1,))
            norm2(qb, pend)
            if qb + 1 < 8:
                gate_back(qb + 1)
            dma("sp", dap(omb_d, qb * 256, [[S, 64], [64 * S, 8], [1, 256]]), oms[mb][0:64],
                r=[f"oms{mb}"], key=f"oms{mb}")
            if debug and s == 0:
                dma("sp", dap(dbg["omb"], qb * 256, [[S, 64], [64 * S, 8], [1, 256]]), oms[mb][0:64],
                    r=[f"oms{mb}"], key=f"dbgoms{mb}")
        P.barrier()
        if maxphase <= 4:
            break
        A.top = seq_top

        mT = A.alloc([8, S], BF16)
        xa_top = A.top
        wga = A.alloc([8, 1024], BF16)
        wgb = A.alloc([8, 1024], BF16)
        wpa = A.alloc([4, 1024], BF16)
        wpb = A.alloc([4, 1024], BF16)
        load_w(wga, win_d, 0, 3584, 1024, 8, INC, "wA", "wga")
        load_w(wgb, win_d, 0, 4608, 1024, 8, INC, "wB", "wgb")
        load_w(wpa, wpa_d, 0, 0, 1024, 4, D, "wC", "wpa")
        load_w(wpb, wpb_d, 0, 0, 1024, 4, D, "wD", "wpb")
        ohg_g = [A.alloc([4, 512], BF16) for _ in range(2)]
        omb_g = [A.alloc([4, 512], BF16) for _ in range(2)]
        sga = A.alloc([512])
        sgb = A.alloc([512])
        t1 = A.alloc([512])
        t2 = A.alloc([512])
        it = 0
        for tg in range(4):
            gb_ = tg % 2
            tsl = slice(tg * 512, (tg + 1) * 512)
            dma("sp", ohg_g[gb_], dap(ohg_d, tg * 512, [[S, 128], [128 * S, 4], [1, 512]]), w=[f"ohg_g{gb_}"], key=f"ohg_g{gb_}")
            dma("sp", omb_g[gb_], dap(omb_d, tg * 512, [[S, 128], [128 * S, 4], [1, 512]]), w=[f"omb_g{gb_}"], key=f"omb_g{gb_}")
            for c in range(8):
                o4 = (it % 2) * 4
                it += 1
                csl = slice(c * 128, (c + 1) * 128)
                for dk in range(8):
                    mm(ps[o4], wga[:, dk, csl], xT[:, dk, tsl], dk == 0, dk == 7, ["wga"] + XT(tg * 4, 4), [PSN[o4]])
                for dk in range(8):
                    mm(ps[o4 + 1], wgb[:, dk, csl], xT[:, dk, tsl], dk == 0, dk == 7, ["wgb"] + XT(tg * 4, 4), [PSN[o4 + 1]])
                for h in range(4):
                    mm(ps[o4 + 2], wpa[:, h, csl], ohg_g[gb_][:, h, :], h == 0, h == 3, ["wpa", f"ohg_g{gb_}"], [PSN[o4 + 2]])
                for h in range(4):
                    mm(ps[o4 + 3], wpb[:, h, csl], omb_g[gb_][:, h, :], h == 0, h == 3, ["wpb", f"omb_g{gb_}"], [PSN[o4 + 3]])
                act(sga, ps[o4], AF.Sigmoid, [PSN[o4], "bcol"], ["sga"], bias=bcol[:, 28 + c:29 + c])
                act(sgb, ps[o4 + 1], AF.Sigmoid, [PSN[o4 + 1], "bcol"], ["sgb"], bias=bcol[:, 36 + c:37 + c])
                tt("dve", t1, sga, ps[o4 + 2], ALU.mult, ["sga", PSN[o4 + 2]], ["t1"])
                tt("dve", t2, sgb, ps[o4 + 3], ALU.mult, ["sgb", PSN[o4 + 3]], ["t2"])
                tt("pool", mT[:, c, tsl], t1, t2, ALU.add, ["t1", "t2"], [f"mT{tg}"])
        P.barrier()
        if maxphase <= 5:
            break
        A.top = xa_top

        wo = A.alloc([8, 1024], BF16)
        load_w(wo, wout_d, 0, 0, 1024, 8, D, "wA", "wo")
        if s == 1:
            wgu0 = A.alloc_at(A.W - 6144, [8, 1024], BF16)
            wdn0 = A.alloc_at(A.W - 2048, [4, 1024], BF16)
            load_w(wgu0, wgu_d, 0, 0, 1024, 8, 1024, "wgu0", "wgu0")
            load_w(wdn0, wdn_d, 0, 0, 1024, 4, D, "wdn0", "wdn0")
        l1g = A.alloc([1024])
        l1b = A.alloc([1024])
        dma("sp", l1g, dap(ln1g_d, 0, [[0, 128], [1, D]]), w=["l1g"], key="l1g")
        dma("sp", l1b, dap(ln1b_d, 0, [[0, 128], [1, D]]), w=["l1b"], key="l1b")
        xin = [A.alloc([1024]) for _ in range(2)]
        rr = A.alloc([1024])
        st = A.alloc([12])
        mv = A.alloc([2])
        rsd = A.alloc([1])
        nmr = A.alloc([1])
        x1 = [A.alloc([1024]) for _ in range(3)]
        x1b = [A.alloc([1024], BF16) for _ in range(4)]
        x1T_ = [A.alloc([8, 128]) for _ in range(2)]
        lg = A.alloc([36])
        sm = A.alloc([16])
        ohg4 = A.alloc([4])
        pen = A.alloc([4])
        em = A.alloc([4, 8])
        t8 = A.alloc([8])
        oh1_ = [A.alloc([32]) for _ in range(2)]
        oh2_ = [A.alloc([32]) for _ in range(2)]
        cnt_ = [A.alloc([32], BF16) for _ in range(2)]
        slot = A.alloc([32])
        tmp32 = A.alloc([32])
        dfl = A.alloc([2])
        def xb_A_mm(j):
            b = j % 2
            tile = s * 16 + j
            tl = slice(j * 128, (j + 1) * 128)
            dma("sp", xin[b], dap(x_d, tile * 128 * D, [[D, 128], [1, D]]), w=[f"xin{b}"], key=f"xin{b}")
            for half in range(2):
                for dk in range(8):
                    mm(ps[half], mT[:, dk, tl], wo[:, dk, half * 512:(half + 1) * 512], dk == 0, dk == 7,
                       [f"mT{j // 4}", "wo"], [PSN[half]])

        def xb_A_rest(j):
            b = j % 2
            tile = s * 16 + j
            for half in range(2):
                hsl = slice(half * 512, (half + 1) * 512)
                stt(rr[:, hsl], xin[b][:, hsl], ALPHA, ps[half], ALU.mult, ALU.add, [f"xin{b}", PSN[half]], ["rr"])
            P.op("dve", lambda e: e.bn_stats(out=st[:, 0:6], in_=rr[:, 0:512]), r=["rr"], w=["st"])
            P.op("dve", lambda e: e.bn_stats(out=st[:, 6:12], in_=rr[:, 512:1024]), r=["rr"], w=["st"])
            P.op("dve", lambda e: e.bn_aggr(out=mv, in_=st), r=["st"], w=["mv"])
            ts("dve", rsd, mv[:, 1:2], EPS, None, ALU.add, None, ["mv"], ["rsd"])
            act(rsd, rsd, AF.Ln, ["rsd"], ["rsd"])
            act(rsd, rsd, AF.Exp, ["rsd"], ["rsd"], scale=-0.5)
            stt(nmr, mv[:, 0:1], -1.0, rsd[:, 0:1], ALU.mult, ALU.mult, ["mv", "rsd"], ["nmr"])
            act(rr, rr, AF.Identity, ["rr", "rsd", "nmr"], ["rr"], bias=nmr[:, 0:1], scale=rsd[:, 0:1])
            tt("pool", rr, rr, l1g, ALU.mult, ["rr", "l1g"], ["rr"])
            tt("pool", x1[j % 3], rr, l1b, ALU.add, ["rr", "l1b"], [f"x1{j % 3}"])
            dma("sp", dap(x1_d, tile * 128 * D, [[D, 128], [1, D]]), x1[j % 3], r=[f"x1{j % 3}"], key=f"x1o{j % 3}")
            if debug:
                dma("sp", dap(dbg["x1"], tile * 128 * D, [[D, 128], [1, D]]), x1[j % 3], r=[f"x1{j % 3}"], key=f"dbgx1{j % 3}")
            tcopy("act", x1b[j % 4], x1[j % 3], [f"x1{j % 3}"], [f"x1b{j % 4}"])

        def xb_B0(j):
            b = j % 2
            x1T = x1T_[b]
            for dk in range(8):
                tr(ps[2 + dk // 4][:, (dk % 4) * 128:(dk % 4 + 1) * 128], x1[j % 3][:, dk * 128:(dk + 1) * 128], identf,
                   [f"x1{j % 3}", "cst"], [PSN[2 + dk // 4]])
            tcopy("act", x1T[:, 0:4, :], ps[2].rearrange("p (a b) -> p a b", a=4, b=128), ["ps2"], [f"x1T{b}"])
            tcopy("act", x1T[:, 4:8, :], ps[3].rearrange("p (a b) -> p a b", a=4, b=128), ["ps3"], [f"x1T{b}"])

        def xb_B1(j):
            b = j % 2
            tile = s * 16 + j
            x1T = x1T_[b]
            oh1, oh2, cnt = oh1_[b], oh2_[b], cnt_[b]
            for dk in range(8):
                mm(ps[4][:, 0:36], x1T[:, dk, :], wr[:, dk, :], dk == 0, dk == 7, [f"x1T{b}", "wr"], ["ps4"])
            tt("dve", lg, ps[4][:, 0:36], br_bc, ALU.add, ["ps4", "br_bc"], ["lg"])
            gmax, ngmax, sg_, pg_, v21, e21, den_, w1_, w2_ = (sm[:, i:i + 1] for i in range(9))
            P.op("dve", lambda e, gmax=gmax: e.tensor_reduce(out=gmax, in_=lg[:, 0:4], axis=AX.X, op=ALU.max), r=["lg"], w=["sm0"])
            ts("dve", ohg4, lg[:, 0:4], gmax, None, ALU.is_equal, None, ["lg", "sm0"], ["ohg4"])
            ts("dve", ngmax, gmax, -1.0, None, ALU.mult, None, ["sm0"], ["sm1"])
            act(pen, lg[:, 0:4], AF.Exp, ["lg", "sm1"], ["pen", "sm2"], bias=ngmax, accum=sg_)
            P.op("dve", lambda e, pg_=pg_, sg_=sg_: e.reciprocal(out=pg_, in_=sg_), r=["sm2"], w=["sm3"])
            ts("dve", pen, ohg4, 1e30, -1e30, ALU.mult, ALU.add, ["ohg4", "pen"], ["pen"])
            tt("dve", em, lg[:, 4:36].rearrange("p (a b) -> p a b", a=4, b=8), pen.unsqueeze(2).to_broadcast([128, 4, 8]),
               ALU.add, ["lg", "pen"], ["em"])
            emf = em.rearrange("p a b -> p (a b)")
            P.op("dve", lambda e, emf=emf: e.max(out=t8, in_=emf), r=["em"], w=["t8"])
            ts("dve", oh1, emf, t8[:, 0:1], None, ALU.is_equal, None, ["em", "t8"], [f"oh1{b}"])
            ts("dve", oh2, emf, t8[:, 1:2], None, ALU.is_equal, None, ["em", "t8"], [f"oh2{b}"])
            tt("dve", v21, t8[:, 1:2], t8[:, 0:1], ALU.subtract, ["t8"], ["sm4"])
            act(e21, v21, AF.Exp, ["sm4"], ["sm5"])
            ts("dve", den_, e21, 1.0, None, ALU.add, None, ["sm5"], ["sm6"])
            P.op("dve", lambda e, w1_=w1_, den_=den_: e.reciprocal(out=w1_, in_=den_), r=["sm6"], w=["sm7"])
            tt("dve", w2_, e21, w1_, ALU.mult, ["sm5", "sm7"], ["sm8"])
            tt("dve", wts[:, tile, 0:1], w1_, pg_, ALU.mult, ["sm7", "sm3"], [f"wts{tile}a"])
            tt("dve", wts[:, tile, 1:2], w2_, pg_, ALU.mult, ["sm8", "sm3"], [f"wts{tile}b"])
            tt("dve", cnt, oh1, oh2, ALU.add, [f"oh1{b}", f"oh2{b}"], [f"cnt{b}"])

        def xb_B2(j):
            b = j % 2
            tile = s * 16 + j
            oh1, oh2, cnt = oh1_[b], oh2_[b], cnt_[b]
            mm(ps[5][:, 0:32], ltrib, cnt, True, True, ["ltrib", f"cnt{b}"], ["ps5"])
            mm(ps[5][:, 32:64], onesb, cnt, True, True, ["onesb", f"cnt{b}"], ["ps5"])
            tt("dve", slot, ps[5][:, 0:32], carry, ALU.add, ["ps5", "carry"], ["slot"])
            tt("dve", carry, ps[5][:, 32:64], carry, ALU.add, ["ps5", "carry"], ["carry"])
            for k, oh in ((0, oh1), (1, oh2)):
                tt("dve", tmp32, oh, slot, ALU.mult, [f"oh{k + 1}{b}", "slot"], ["tmp32"])
                P.op("dve", lambda e, k=k: e.tensor_reduce(out=dfl[:, k:k + 1], in_=tmp32, axis=AX.X, op=ALU.add),
                     r=["tmp32"], w=["dfl"])
            tcopy("dve", dest_i[:, tile, :], dfl, ["dfl"], [f"dest{tile}"])
            for k in range(2):
                P.op("pool", lambda e, k=k, tile=tile, b=b: e.indirect_dma_start(
                    out=xbuf_d.ap(), out_offset=bass.IndirectOffsetOnAxis(ap=dest_i[:, tile, k:k + 1], axis=0),
                    in_=x1b[j % 4], in_offset=None), r=[f"x1b{j % 4}", f"dest{tile}"], dma=f"scat{j % 4}", extra=zfill_ops)

        for j0 in range(2):
            xb_A_mm(j0)
            xb_A_rest(j0)
        xb_B0(0)
        for j in range(16):
            xb_B1(j)
            if j + 2 < 16:
                xb_A_mm(j + 2)
            if j + 1 < 16:
                xb_B0(j + 1)
            if j + 2 < 16:
                xb_A_rest(j + 2)
            if j >= 1:
                xb_B2(j - 1)
        xb_B2(15)
        P.barrier()

    if stage >= 2:
        A.top = persist_top
        wgu = [wgu0, A.alloc([8, 1024], BF16)]
        wdn = [wdn0, A.alloc([4, 1024], BF16)]
        xe = [A.alloc([3, 1024], BF16) for _ in range(2)]
        xTe = [A.alloc([8, CAP], BF16) for _ in range(2)]
        hact = A.alloc([4, CAP], BF16)
        sgs = [A.alloc([CAP]) for _ in range(2)]
        ysb = [A.alloc([1024]) for _ in range(4)]
        yi = 0
        dma("sp", xe[0], dap(xbuf_d, 0, [[D, 128], [128 * D, 3], [1, D]]), w=["xe0"], key="xe0")
        for e_ in range(NE):
            b = e_ % 2
            if e_ > 0:
                load_w(wgu[b], wgu_d, e_ * D, 0, 1024, 8, 1024, f"wgu{b}", f"wgu{b}")
                load_w(wdn[b], wdn_d, e_ * 512, 0, 1024, 4, D, f"wdn{b}", f"wdn{b}")
            if e_ + 1 < NE:
                dma("sp", xe[1 - b], dap(xbuf_d, (e_ + 1) * CAP * D, [[D, 128], [128 * D, 3], [1, D]]),
                    w=[f"xe{1 - b}"], key=f"xe{1 - b}")
            for r_ in range(3):
                tb = r_ % 2
                for dk in range(8):
                    tr(psb[tb][:, dk * 128:(dk + 1) * 128], xe[b][:, r_, dk * 128:(dk + 1) * 128], identb,
                       [f"xe{b}", "identb"], [PSN[tb]])
                tcopy("act" if r_ % 2 else "dve", xTe[b][:, :, r_ * 128:(r_ + 1) * 128],
                      psb[tb].rearrange("p (a b) -> p a b", a=8, b=128), [PSN[tb]], [f"xTe{b}"])
            for fc in range(4):
                pg_b, pu_b = 2 + (fc % 2) * 2, 3 + (fc % 2) * 2
                for dk in range(8):
                    mm(ps[pg_b][:, 0:CAP], wgu[b][:, dk, fc * 128:(fc + 1) * 128], xTe[b][:, dk, :], dk == 0, dk == 7,
                       [f"wgu{b}", f"xTe{b}"], [PSN[pg_b]])
                for dk in range(8):
                    mm(ps[pu_b][:, 0:CAP], wgu[b][:, dk, 512 + fc * 128:512 + (fc + 1) * 128], xTe[b][:, dk, :], dk == 0, dk == 7,
                       [f"wgu{b}", f"xTe{b}"], [PSN[pu_b]])
                act(sgs[fc % 2], ps[pg_b][:, 0:CAP], AF.Silu, [PSN[pg_b]], [f"sgs{fc % 2}"])
                tt("dve", hact[:, fc, :], sgs[fc % 2], ps[pu_b][:, 0:CAP], ALU.mult, [f"sgs{fc % 2}", PSN[pu_b]], ["hact"])
            for r_ in range(3):
                yb = yi % 4
                yi += 1
                for half in range(2):
                    pb = 6 + half
                    for fk in range(4):
                        mm(ps[pb], hact[:, fk, r_ * 128:(r_ + 1) * 128], wdn[b][:, fk, half * 512:(half + 1) * 512],
                           fk == 0, fk == 3, ["hact", f"wdn{b}"], [PSN[pb]])
                tcopy("act", ysb[yb][:, 0:512], ps[6], ["ps6"], [f"ysb{yb}"])
                tcopy("dve", ysb[yb][:, 512:1024], ps[7], ["ps7"], [f"ysb{yb}"])
                dma("sp", dap(ybuf_d, (e_ * CAP + r_ * 128) * D, [[D, 128], [1, D]]), ysb[yb],
                    r=[f"ysb{yb}"], key=f"ysb{yb}")
        P.barrier()

        A.top = persist_top
        l2g = A.alloc([1024])
        l2b = A.alloc([1024])
        dma("sp", l2g, dap(ln2g_d, 0, [[0, 128], [1, D]]), w=["l2g"], key="l2g")
        dma("sp", l2b, dap(ln2b_d, 0, [[0, 128], [1, D]]), w=["l2b"], key="l2b")
        y1 = [A.alloc([1024]) for _ in range(3)]
        y2 = [A.alloc([1024]) for _ in range(3)]
        xs = [A.alloc([1024]) for _ in range(3)]
        zz = A.alloc([1024])
        zn = [A.alloc([1024]) for _ in range(2)]
        ot = [A.alloc([1024]) for _ in range(2)]
        st2 = A.alloc([12])
        mv2 = A.alloc([2])
        rsd2 = A.alloc([1])
        nmr2 = A.alloc([1])
        def c_issue(tile):
            b = tile % 3
            for k, yk in ((0, y1[b]), (1, y2[b])):
                P.op("pool", lambda e, k=k, tile=tile, yk=yk: e.indirect_dma_start(
                    out=yk, out_offset=None, in_=ybuf_d.ap(),
                    in_offset=bass.IndirectOffsetOnAxis(ap=dest_i[:, tile, k:k + 1], axis=0)),
                    r=[f"dest{tile}"], w=[f"y{k}{b}"], dma=f"gy{k}{b}")
            dma("sp", xs[b], dap(x1_d, tile * 128 * D, [[D, 128], [1, D]]), w=[f"xs{b}"], key=f"xs{b}")

        zz2 = [zz, A.alloc([1024])]
        st3 = [st2, A.alloc([12])]
        mv3 = [mv2, A.alloc([2])]
        rsd3 = [rsd2, A.alloc([1])]
        nmr3 = [nmr2, A.alloc([1])]

        def c_front(tile):
            b = tile % 2
            c = tile % 3
            z_, st_, mv_, rs_, nm_ = zz2[b], st3[b], mv3[b], rsd3[b], nmr3[b]
            stt(z_, y1[c], wts[:, tile, 0:1], xs[c], ALU.mult, ALU.add, [f"y0{c}", f"xs{c}"], [f"zz{b}"])
            stt(z_, y2[c], wts[:, tile, 1:2], z_, ALU.mult, ALU.add, [f"y1{c}", f"zz{b}"], [f"zz{b}"])
            P.op("dve", lambda e, z_=z_, st_=st_: e.bn_stats(out=st_[:, 0:6], in_=z_[:, 0:512]), r=[f"zz{b}"], w=[f"st{b}"])
            P.op("dve", lambda e, z_=z_, st_=st_: e.bn_stats(out=st_[:, 6:12], in_=z_[:, 512:1024]), r=[f"zz{b}"], w=[f"st{b}"])
            P.op("dve", lambda e, st_=st_, mv_=mv_: e.bn_aggr(out=mv_, in_=st_), r=[f"st{b}"], w=[f"mv{b}"])
            ts("dve", rs_, mv_[:, 1:2], EPS, None, ALU.add, None, [f"mv{b}"], [f"rsd{b}"])
            act(rs_, rs_, AF.Ln, [f"rsd{b}"], [f"rsd{b}"])
            act(rs_, rs_, AF.Exp, [f"rsd{b}"], [f"rsd{b}"], scale=-0.5)
            stt(nm_, mv_[:, 0:1], -1.0, rs_[:, 0:1], ALU.mult, ALU.mult, [f"mv{b}", f"rsd{b}"], [f"nmr{b}"])
            act(zn[b], z_, AF.Identity, [f"zz{b}", f"rsd{b}", f"nmr{b}"], [f"zn{b}"], bias=nm_[:, 0:1], scale=rs_[:, 0:1])
            tt("pool", zn[b], zn[b], l2g, ALU.mult, [f"zn{b}", "l2g"], [f"zn{b}"])

        def c_scale(tile):
            c = tile % 3
            act(xs[c], xs[c], AF.Copy, [f"xs{c}"], [f"xs{c}"], scale=ALPHA)

        def c_back(tile):
            b = tile % 2
            tt("dve", ot[b], zn[b], l2b, ALU.add, [f"zn{b}", "l2b"], [f"ot{b}"])
            dma("sp", dap(y_d, tile * 128 * D, [[D, 128], [1, D]]), ot[b], r=[f"ot{b}"], key=f"ot{b}")

        c_issue(0)
        c_issue(1)
        c_issue(2)
        c_scale(0)
        c_scale(1)
        c_front(0)
        for tile in range(32):
            if tile + 3 < 32:
                c_issue(tile + 3)
            if tile + 2 < 32:
                c_scale(tile + 2)
            if tile + 1 < 32:
                c_front(tile + 1)
            c_back(tile)
    P.emit()
    return nc, A.peak


_CACHE = {}


def kernel(x, w_in, b_in, lb_logits, hg_norm_g, rel_bias, w_proj_a, w_proj_b, w_out, ln1_g, ln1_b,
           w_group, b_group, w_expert, b_expert, w_gate_up, w_down, ln2_g, ln2_b):
    f = lambda a: np.ascontiguousarray(np.asarray(a, dtype=np.float32))
    if "nc" not in _CACHE:
        _CACHE["nc"] = build()[0]
    nc = _CACHE["nc"]
    x = f(x)
    shared = {
        "w_in": f(w_in[0]), "b_in": f(b_in[0]), "lb_logits": f(lb_logits), "hg_norm_g": f(hg_norm_g[0]),
        "rel_bias": f(rel_bias), "w_proj_a": f(w_proj_a[0]), "w_proj_b": f(w_proj_b[0]), "w_out": f(w_out[0]),
        "ln1_g": f(ln1_g[0]), "ln1_b": f(ln1_b[0]),
        "w_router": f(np.concatenate([np.asarray(w_group[0]), np.asarray(w_expert[0])], axis=1)),
        "b_router": f(np.concatenate([np.asarray(b_group[0]), np.asarray(b_expert[0])], axis=0)),
        "w_gate_up": f(w_gate_up[0]), "w_down": f(w_down[0]), "ln2_g": f(ln2_g[0]), "ln2_b": f(ln2_b[0]),
        "consts": make_consts(),
        "eind": np.ascontiguousarray((np.arange(S)[None, :] // 256 == np.arange(8)[:, None]).astype(np.float32)),
    }
    in_maps = []
    for c in range(NCORES):
        m = dict(shared)
        m["x"] = np.ascontiguousarray(x[2 * c:2 * c + 2].reshape(NTOK, D))
        in_maps.append(m)
    res = run_bass_kernel_spmd(nc, in_maps, core_ids=list(range(NCORES)))
    out = np.concatenate([np.asarray(r["y"]).reshape(2, S, D) for r in res.results], axis=0)
    return out.astype(np.float32)
```
